# Optimizing a Trainium2 kernel written in Bass

```python
import math
import jax, jax.numpy as jnp
from jax import lax
import numpy as np

D_MODEL = 1024
BATCH = 8
SEQ = 4096
DEPTH = 4

GDN_HEADS = 4
GDN_DK = 128
GDN_DV = 128
GDN_CONV = 4
GDN_CHUNK = 64
SWA_GROUPS = ((128, 1), (512, 4), (2048, 16))
SWA_HEADS = 4
SWA_DH = 64
ROPE_THETA = 10000.0
GLA_HEADS = 4
GLA_DK = 64
GLA_DV = 128
GLA_RANK = 16
GLA_TAU = 16.0
GLA_CHUNK = 64
MEM_LEN = 256
XA_HEADS = 4
XA_DH = 128
N_EXPERTS = 32
TOP_K = 4
D_EXPERT = 1024
SWIGLU_ALPHA = 1.702
SWIGLU_LIMIT = 7.0
MOE_BLOCK = 128
RMS_EPS = 1e-6
N_BRANCH = 3

GDN_QK_W = GDN_HEADS * GDN_DK
GDN_V_W = GDN_HEADS * GDN_DV
SWA_W = len(SWA_GROUPS) * SWA_HEADS * SWA_DH
SWA_OUT_W = SWA_HEADS * SWA_DH
GLA_QK_W = GLA_HEADS * GLA_DK
GLA_V_W = GLA_HEADS * GLA_DV
IN_SIZES = (GDN_QK_W, GDN_QK_W, GDN_V_W, GDN_V_W, GDN_HEADS, GDN_HEADS,
            SWA_W, SWA_W, SWA_W,
            GLA_QK_W, GLA_QK_W, GLA_V_W, GLA_V_W, GLA_RANK,
            N_BRANCH * D_MODEL)
N_IN = sum(IN_SIZES)
IN_OFFSETS = tuple(sum(IN_SIZES[:i + 1]) for i in range(len(IN_SIZES) - 1))

kernel_name = "hybrid_gdn_dilswa_gla_moe_block"


def rms_norm(x, gain):
    xf = x.astype(jnp.float32)
    y = xf * lax.rsqrt(jnp.mean(xf * xf, axis=-1, keepdims=True) + RMS_EPS)
    return (y * gain.astype(jnp.float32)).astype(x.dtype)


def l2norm(x):
    return x * lax.rsqrt(jnp.sum(x * x, axis=-1, keepdims=True) + 1e-6)


def rope(x, cos, sin):
    xf = x.astype(jnp.float32)
    x1, x2 = jnp.split(xf, 2, axis=-1)
    return jnp.concatenate([x1 * cos - x2 * sin, x2 * cos + x1 * sin], axis=-1).astype(x.dtype)


def causal_depthwise_conv(x, w):
    K, C = w.shape
    return lax.conv_general_dilated(x, w[:, None, :].astype(x.dtype), window_strides=(1,),
                                    padding=((K - 1, 0),), dimension_numbers=("NWC", "WIO", "NWC"),
                                    feature_group_count=C)


def to_heads(t, n_heads):
    b, s = t.shape[:2]
    return t.reshape(b, s, n_heads, -1).transpose(0, 2, 1, 3)


def gated_delta_rule(q, k, v, g, beta):
    out_dtype = v.dtype
    f32 = jnp.float32
    q, k, v, g, beta = (t.astype(f32) for t in (q, k, v, g, beta))
    Bz, H, S, dk = q.shape
    dv = v.shape[-1]
    C = GDN_CHUNK
    N = S // C
    q = l2norm(q) * dk ** -0.5
    k = l2norm(k)
    chunk = lambda t: t.reshape(Bz, H, N, C, *t.shape[3:])
    q, k, v, g, beta = (chunk(t) for t in (q, k, v, g, beta))
    g = jnp.cumsum(g, axis=-1)
    incl = jnp.tril(jnp.ones((C, C), bool))
    strict = jnp.tril(jnp.ones((C, C), bool), -1)
    decay = jnp.exp(jnp.where(incl, g[..., :, None] - g[..., None, :], -jnp.inf))
    k_beta = k * beta[..., None]
    lower = jnp.where(strict, jnp.einsum("bhnid,bhnjd->bhnij", k_beta, k) * decay, 0.0)
    tmat = lower + jnp.eye(C, dtype=f32)
    solve = lambda a, b: lax.linalg.triangular_solve(a, b, left_side=True, lower=True, unit_diagonal=True)
    u = solve(tmat, v * beta[..., None])
    w = solve(tmat, k_beta * jnp.exp(g)[..., None])
    intra = jnp.where(incl, jnp.einsum("bhnid,bhnjd->bhnij", q, k) * decay, 0.0)
    q_dec = q * jnp.exp(g)[..., None]
    g_last = g[..., -1]
    k_dec = k * jnp.exp(g_last[..., None] - g)[..., None]

    def step(state, inp):
        q_c, w_c, u_c, a_c, k_c, gl = inp
        v_new = u_c - jnp.einsum("bhcd,bhde->bhce", w_c, state)
        o = jnp.einsum("bhcd,bhde->bhce", q_c, state) + jnp.einsum("bhij,bhje->bhie", a_c, v_new)
        state = state * jnp.exp(gl)[..., None, None] + jnp.einsum("bhcd,bhce->bhde", k_c, v_new)
        return state, o

    xs = tuple(jnp.moveaxis(t, 2, 0) for t in (q_dec, w, u, intra, k_dec, g_last))
    _, o = lax.scan(step, jnp.zeros((Bz, H, dk, dv), f32), xs)
    return jnp.moveaxis(o, 0, 2).reshape(Bz, H, S, dv).astype(out_dtype)


def gla_chunked(q, k, v, log_a):
    out_dtype = v.dtype
    f32 = jnp.float32
    q, k, v, log_a = (t.astype(f32) for t in (q, k, v, log_a))
    Bz, H, S, dk = q.shape
    dv = v.shape[-1]
    C = GLA_CHUNK
    N = S // C
    q = q * dk ** -0.5
    chunk = lambda t: jnp.moveaxis(t.reshape(Bz, H, N, C, t.shape[-1]), 2, 0)
    incl = jnp.tril(jnp.ones((C, C), bool))[:, :, None]

    def step(state, inp):
        q_c, k_c, v_c, la_c = inp
        b = jnp.cumsum(la_c, axis=-2)
        b_last = b[..., -1:, :]
        decay = jnp.exp(jnp.where(incl, b[..., :, None, :] - b[..., None, :, :], -jnp.inf))
        scores = jnp.einsum("bhid,bhjd,bhijd->bhij", q_c, k_c, decay)
        o = (jnp.einsum("bhid,bhde->bhie", q_c * jnp.exp(b), state)
             + jnp.einsum("bhij,bhje->bhie", scores, v_c))
        state = (state * jnp.exp(b_last)[..., 0, :, None]
                 + jnp.einsum("bhjd,bhje->bhde", k_c * jnp.exp(b_last - b), v_c))
        return state, o

    _, o = lax.scan(step, jnp.zeros((Bz, H, dk, dv), f32), (chunk(q), chunk(k), chunk(v), chunk(log_a)))
    return jnp.moveaxis(o, 0, 2).reshape(Bz, H, S, dv).astype(out_dtype)


def dilated_window_attention(q, k, v, dilation, n_back):
    Bz, S, H, dh = q.shape
    L = S // dilation
    c = n_back
    nb = -(-L // c)
    Lp = nb * c
    regroup = lambda t: t.reshape(Bz, L, dilation, H, dh).transpose(0, 2, 3, 1, 4).astype(jnp.float32)
    qs, ks, vs = regroup(q), regroup(k), regroup(v)
    qb = jnp.pad(qs, ((0, 0), (0, 0), (0, 0), (0, Lp - L), (0, 0))).reshape(Bz, dilation, H, nb, c, dh)

    def kv_blocks(t):
        tp = jnp.pad(t, ((0, 0), (0, 0), (0, 0), (c, Lp - L), (0, 0)))
        prev = tp[..., :Lp, :].reshape(Bz, dilation, H, nb, c, dh)
        cur = tp[..., c:, :].reshape(Bz, dilation, H, nb, c, dh)
        return jnp.concatenate([prev, cur], axis=-2)

    kb, vb = kv_blocks(ks), kv_blocks(vs)
    qi = jnp.arange(nb)[:, None, None] * c + jnp.arange(c)[None, :, None]
    ki = jnp.arange(nb)[:, None, None] * c - c + jnp.arange(2 * c)[None, None, :]
    dist = qi - ki
    valid = (dist >= 0) & (dist <= n_back) & (ki >= 0)
    s = jnp.einsum("brhnqd,brhnkd->brhnqk", qb, kb) * dh ** -0.5
    s = jnp.where(valid, s, -jnp.inf)
    m = jnp.max(s, axis=-1, keepdims=True)
    p = jnp.exp(s - m)
    l = jnp.sum(p, axis=-1, keepdims=True)
    o = jnp.einsum("brhnqk,brhnkd->brhnqd", p / l, vb)
    lse = (m + jnp.log(l))[..., 0]
    o = o.reshape(Bz, dilation, H, Lp, dh)[:, :, :, :L].transpose(0, 3, 1, 2, 4).reshape(Bz, S, H, dh)
    lse = lse.reshape(Bz, dilation, H, Lp)[:, :, :, :L].transpose(0, 3, 1, 2).reshape(Bz, S, H)
    return o, lse


def hybrid_mixer(h, cos, sin, w_in, gate_bias, gdn_conv, gdn_a_log, gdn_dt_bias, gdn_norm,
                 swa_q_norm, swa_k_norm, gla_gate_up, gla_gate_bias, gla_norm,
                 w_branch_a, w_branch_b, w_branch_c, w_mix_out):
    Bz, S, _ = h.shape
    proj = h @ w_in
    (a_q, a_k, a_v, a_z, a_alpha, a_beta, b_q, b_k, b_v,
     c_q, c_k, c_v, c_r, c_low, gates) = jnp.split(proj, IN_OFFSETS, axis=-1)

    qkv = jax.nn.silu(causal_depthwise_conv(jnp.concatenate([a_q, a_k, a_v], axis=-1), gdn_conv))
    a_q, a_k, a_v = jnp.split(qkv, (GDN_QK_W, 2 * GDN_QK_W), axis=-1)
    beta = jax.nn.sigmoid(a_beta.astype(jnp.float32))
    g = -jnp.exp(gdn_a_log) * jax.nn.softplus(a_alpha.astype(jnp.float32) + gdn_dt_bias)
    o_a = gated_delta_rule(to_heads(a_q, GDN_HEADS), to_heads(a_k, GDN_HEADS), to_heads(a_v, GDN_HEADS),
                           g.transpose(0, 2, 1), beta.transpose(0, 2, 1)).transpose(0, 2, 1, 3)
    z = a_z.reshape(Bz, S, GDN_HEADS, GDN_DV)
    y_a = (rms_norm(o_a, gdn_norm) * jax.nn.silu(z)).reshape(Bz, S, GDN_V_W) @ w_branch_a

    shp = (Bz, S, len(SWA_GROUPS), SWA_HEADS, SWA_DH)
    qb = rope(rms_norm(b_q.reshape(shp), swa_q_norm), cos, sin)
    kb = rope(rms_norm(b_k.reshape(shp), swa_k_norm), cos, sin)
    vb = b_v.reshape(shp)
    outs, lses = [], []
    for gi, (window, dil) in enumerate(SWA_GROUPS):
        o_g, lse_g = dilated_window_attention(qb[:, :, gi], kb[:, :, gi], vb[:, :, gi], dil, window // dil)
        outs.append(o_g)
        lses.append(lse_g)
    wts = jax.nn.softmax(jnp.stack(lses, axis=0), axis=0)
    o_b = jnp.einsum("gbsh,gbshd->bshd", wts, jnp.stack(outs, axis=0)).astype(h.dtype)
    y_b = o_b.reshape(Bz, S, SWA_OUT_W) @ w_branch_b

    log_a = jax.nn.log_sigmoid((c_low @ gla_gate_up + gla_gate_bias).astype(jnp.float32)) / GLA_TAU
    o_c = gla_chunked(to_heads(c_q, GLA_HEADS), to_heads(c_k, GLA_HEADS), to_heads(c_v, GLA_HEADS),
                      to_heads(log_a, GLA_HEADS)).transpose(0, 2, 1, 3)
    r = c_r.reshape(Bz, S, GLA_HEADS, GLA_DV)
    y_c = (rms_norm(o_c, gla_norm) * jax.nn.silu(r)).reshape(Bz, S, GLA_V_W) @ w_branch_c

    gt = jax.nn.sigmoid(gates.reshape(Bz, S, N_BRANCH, D_MODEL) + gate_bias)
    y = gt[:, :, 0] * y_a + gt[:, :, 1] * y_b + gt[:, :, 2] * y_c
    return y @ w_mix_out


def memory_cross_attention(h, m, wq, wkv, q_gain, k_gain, wo):
    Bz, S, _ = h.shape
    M = m.shape[1]
    q = rms_norm((h @ wq).reshape(Bz, S, XA_HEADS, XA_DH), q_gain)
    kv = (m @ wkv).reshape(Bz, M, 2, XA_HEADS, XA_DH)
    k = rms_norm(kv[:, :, 0], k_gain)
    v = kv[:, :, 1]
    s = jnp.einsum("bshd,bmhd->bhsm", q.astype(jnp.float32), k.astype(jnp.float32)) * XA_DH ** -0.5
    p = jax.nn.softmax(s, axis=-1)
    o = jnp.einsum("bhsm,bmhd->bshd", p, v.astype(jnp.float32)).astype(h.dtype)
    return o.reshape(Bz, S, XA_HEADS * XA_DH) @ wo


def moe_ffn(h, router_w, router_b, w_in, b_in, w_out, b_out):
    Bz, S, D = h.shape
    T = Bz * S
    A = T * TOP_K
    xf = h.reshape(T, D)
    logits = (xf @ router_w + router_b).astype(jnp.float32)
    top_val, top_idx = lax.top_k(logits, TOP_K)
    gates = jax.nn.softmax(top_val, axis=-1).astype(h.dtype)
    e_flat = top_idx.reshape(A)
    order = jnp.argsort(e_flat)
    e_sorted = e_flat[order]
    tok_sorted = (order // TOP_K).astype(jnp.int32)
    gate_sorted = gates.reshape(A)[order]
    counts = jnp.bincount(e_flat, length=N_EXPERTS)
    padded = (counts + MOE_BLOCK - 1) // MOE_BLOCK * MOE_BLOCK
    starts = jnp.cumsum(counts) - counts
    pad_ends = jnp.cumsum(padded)
    pad_starts = pad_ends - padded
    dest = pad_starts[e_sorted] + jnp.arange(A) - starts[e_sorted]
    P = A + N_EXPERTS * MOE_BLOCK
    NB = P // MOE_BLOCK
    tok_buf = jnp.full((P,), T, jnp.int32).at[dest].set(tok_sorted)
    gate_buf = jnp.zeros((P,), h.dtype).at[dest].set(gate_sorted)
    block_expert = jnp.minimum(jnp.searchsorted(pad_ends, jnp.arange(NB) * MOE_BLOCK, side="right"),
                               N_EXPERTS - 1)
    xb = jnp.concatenate([xf, jnp.zeros((1, D), xf.dtype)], axis=0)[tok_buf].reshape(NB, MOE_BLOCK, D)

    def expert_block(args):
        xblk, e = args
        hh = xblk @ w_in[e] + b_in[e]
        glu = jnp.minimum(hh[:, :D_EXPERT], SWIGLU_LIMIT)
        lin = jnp.clip(hh[:, D_EXPERT:], -SWIGLU_LIMIT, SWIGLU_LIMIT)
        act = glu * jax.nn.sigmoid(SWIGLU_ALPHA * glu) * (lin + 1.0)
        return act @ w_out[e] + b_out[e]

    yb = lax.map(expert_block, (xb, block_expert)).reshape(P, D)
    y = jnp.zeros((T + 1, D), h.dtype).at[tok_buf].add(yb * gate_buf[:, None])
    return y[:T].reshape(Bz, S, D)


def setup_inputs(seed: int = 0) -> dict:
    key = jax.random.key(seed)
    ks = jax.random.split(key, 36)
    L, D = DEPTH, D_MODEL
    res = (3.0 * DEPTH) ** -0.5

    def nrm(k, shape, scale):
        return jax.random.normal(k, shape, jnp.float32) * scale

    def gain(k, shape):
        return 1.0 + nrm(k, shape, 0.05)

    dt = jnp.exp(jax.random.uniform(ks[6], (L, GDN_HEADS), jnp.float32, math.log(1e-3), math.log(1e-1)))
    start = jax.random.randint(ks[2], (BATCH, 1), 0, 4096, jnp.int32)
    return {
        "x": nrm(ks[0], (BATCH, SEQ, D), 1.0),
        "mem": nrm(ks[1], (BATCH, MEM_LEN, D), 1.0),
        "positions": start + jnp.arange(SEQ, dtype=jnp.int32)[None, :],
        "norm_mix": gain(ks[3], (L, D)),
        "w_in": nrm(ks[4], (L, D, N_IN), D ** -0.5),
        "gate_bias": nrm(ks[5], (L, N_BRANCH, D), 0.1),
        "gdn_conv": nrm(ks[7], (L, GDN_CONV, 2 * GDN_QK_W + GDN_V_W), GDN_CONV ** -0.5),
        "gdn_a_log": jnp.log(jax.random.uniform(ks[8], (L, GDN_HEADS), jnp.float32, 1.0, 16.0)),
        "gdn_dt_bias": dt + jnp.log(-jnp.expm1(-dt)),
        "gdn_norm": gain(ks[9], (L, GDN_DV)),
        "swa_q_norm": gain(ks[10], (L, SWA_DH)),
        "swa_k_norm": gain(ks[11], (L, SWA_DH)),
        "gla_gate_up": nrm(ks[12], (L, GLA_RANK, GLA_QK_W), GLA_RANK ** -0.5),
        "gla_gate_bias": nrm(ks[13], (L, GLA_QK_W), 0.5),
        "gla_norm": gain(ks[14], (L, GLA_DV)),
        "w_branch_a": nrm(ks[15], (L, GDN_V_W, D), GDN_V_W ** -0.5),
        "w_branch_b": nrm(ks[16], (L, SWA_OUT_W, D), SWA_OUT_W ** -0.5),
        "w_branch_c": nrm(ks[17], (L, GLA_V_W, D), GLA_V_W ** -0.5),
        "w_mix_out": nrm(ks[18], (L, D, D), D ** -0.5 * res),
        "norm_cross": gain(ks[19], (L, D)),
        "norm_mem": gain(ks[20], (L, D)),
        "xa_wq": nrm(ks[21], (L, D, XA_HEADS * XA_DH), D ** -0.5),
        "xa_wkv": nrm(ks[22], (L, D, 2 * XA_HEADS * XA_DH), D ** -0.5),
        "xa_q_norm": gain(ks[23], (L, XA_DH)),
        "xa_k_norm": gain(ks[24], (L, XA_DH)),
        "xa_wo": nrm(ks[25], (L, XA_HEADS * XA_DH, D), (XA_HEADS * XA_DH) ** -0.5 * res),
        "norm_ffn": gain(ks[26], (L, D)),
        "router_w": nrm(ks[27], (L, D, N_EXPERTS), D ** -0.5),
        "router_b": nrm(ks[28], (L, N_EXPERTS), 0.01),
        "moe_w_in": nrm(ks[29], (L, N_EXPERTS, D, 2 * D_EXPERT), D ** -0.5),
        "moe_b_in": nrm(ks[30], (L, N_EXPERTS, 2 * D_EXPERT), 0.02),
        "moe_w_out": nrm(ks[31], (L, N_EXPERTS, D_EXPERT, D), D_EXPERT ** -0.5 * res),
        "moe_b_out": nrm(ks[32], (L, N_EXPERTS, D), 0.02),
    }


def reference(x, mem, positions, norm_mix, w_in, gate_bias, gdn_conv, gdn_a_log, gdn_dt_bias, gdn_norm,
              swa_q_norm, swa_k_norm, gla_gate_up, gla_gate_bias, gla_norm, w_branch_a, w_branch_b,
              w_branch_c, w_mix_out, norm_cross, norm_mem, xa_wq, xa_wkv, xa_q_norm, xa_k_norm, xa_wo,
              norm_ffn, router_w, router_b, moe_w_in, moe_b_in, moe_w_out, moe_b_out):
    inv_freq = ROPE_THETA ** (-jnp.arange(0, SWA_DH, 2, dtype=jnp.float32) / SWA_DH)
    ang = positions.astype(jnp.float32)[..., None] * inv_freq
    cos = jnp.cos(ang)[:, :, None, None, :]
    sin = jnp.sin(ang)[:, :, None, None, :]
    for l in range(DEPTH):
        h = rms_norm(x, norm_mix[l])
        x = x + hybrid_mixer(h, cos, sin, w_in[l], gate_bias[l], gdn_conv[l], gdn_a_log[l], gdn_dt_bias[l],
                             gdn_norm[l], swa_q_norm[l], swa_k_norm[l], gla_gate_up[l], gla_gate_bias[l],
                             gla_norm[l], w_branch_a[l], w_branch_b[l], w_branch_c[l], w_mix_out[l])
        h = rms_norm(x, norm_cross[l])
        m = rms_norm(mem, norm_mem[l])
        x = x + memory_cross_attention(h, m, xa_wq[l], xa_wkv[l], xa_q_norm[l], xa_k_norm[l], xa_wo[l])
        h = rms_norm(x, norm_ffn[l])
        x = x + moe_ffn(h, router_w[l], router_b[l], moe_w_in[l], moe_b_in[l], moe_w_out[l], moe_b_out[l])
    return x
```

```python
import numpy as np
from contextlib import ExitStack
import concourse.bass as bass
import concourse.mybir as mybir
from concourse.bass_utils import run_bass_kernel_spmd

F32 = mybir.dt.float32
BF16 = mybir.dt.bfloat16
I32 = mybir.dt.int32
AF = mybir.ActivationFunctionType
ALU = mybir.AluOpType
AX = mybir.AxisListType

D = 1024
S = 4096
NT = S // 128
DEPTH = 4
N_IN = 8984
MEM = 256
NE = 32
CAP = 640
NCAPT = CAP // 128
XB_ROWS = NE * CAP
EPS = 1e-6


class Buf:
    __slots__ = ("name", "writer", "readers")

    def __init__(self, name=""):
        self.name = name
        self.writer = None
        self.readers = {}


class FW:
    def __init__(self, nc, es, n_dma_sems=10):
        self.nc = nc
        self.es = es
        self.eng = {"pe": nc.tensor, "act": nc.scalar, "dve": nc.vector, "pool": nc.gpsimd, "sp": nc.sync}
        self.sem = {}
        self.cnt = {}
        for k in ("pe", "act", "dve", "pool"):
            self.sem[k] = es.enter_context(nc.semaphore("S_" + k))
            self.cnt[k] = 0
        self.dq = {}
        for q in ("sp", "act", "pool"):
            ring = []
            for i in range(n_dma_sems):
                key = f"d_{q}{i}"
                self.sem[key] = es.enter_context(nc.semaphore("S_" + key))
                self.cnt[key] = 0
                ring.append(key)
            self.dq[q] = [ring, 0]
        self.known = {k: {} for k in ("pe", "act", "dve", "pool", "sp")}
        self.n_inst = 0
        self.n_wait = 0

    def _wait(self, stream, dep):
        key, val = dep
        if stream == "pe" and key == "pe":
            return
        if self.known[stream].get(key, 0) >= val:
            return
        self.eng[stream].wait_ge(self.sem[key], val)
        self.known[stream][key] = val
        self.n_wait += 1

    def _deps(self, stream, reads, writes):
        for b in reads:
            if b.writer is not None:
                self._wait(stream, b.writer)
        for b in writes:
            if b.writer is not None:
                self._wait(stream, b.writer)
            for k, v in b.readers.items():
                self._wait(stream, (k, v))

    def _commit(self, done, reads, writes):
        k, v = done
        for b in writes:
            b.writer = done
            b.readers = {}
        for b in reads:
            if b.readers.get(k, 0) < v:
                b.readers[k] = v

    def op(self, stream, fn, reads=(), writes=(), inc=True):
        self._deps(stream, reads, writes)
        ins = fn()
        self.n_inst += 1
        if inc:
            self.cnt[stream] += 1
            ins.then_inc(self.sem[stream], 1)
            done = (stream, self.cnt[stream])
        else:
            done = (stream, self.cnt[stream] + 1)
        self._commit(done, reads, writes)
        return ins

    def _dma_common(self, q, issue, reads, writes):
        ring, idx = self.dq[q]
        key = ring[idx % len(ring)]
        self.dq[q][1] = idx + 1
        if self.cnt[key] > 0:
            self._wait(q, (key, self.cnt[key]))
        self._deps(q, reads, writes)
        ins = issue()
        self.cnt[key] += 16
        ins.then_inc(self.sem[key], 16)
        self.n_inst += 1
        done = (key, self.cnt[key])
        self._commit(done, reads, writes)
        return done

    def dma(self, q, out, in_, reads=(), writes=(), **kw):
        return self._dma_common(q, lambda: self.eng[q].dma_start(out=out, in_=in_, **kw), reads, writes)

    def idma(self, out, out_off, in_, in_off, reads=(), writes=(), **kw):
        return self._dma_common(
            "pool", lambda: self.nc.gpsimd.indirect_dma_start(out, out_off, in_, in_off, **kw), reads, writes)

    def barrier(self):
        for stream in ("pe", "act", "dve", "pool", "sp"):
            for key, c in self.cnt.items():
                if c > 0:
                    self._wait(stream, (key, c))


class T:
    __slots__ = ("t", "b", "ps")

    def __init__(self, t, name="", b=None, ps=False):
        self.t = t
        self.b = b if b is not None else Buf(name)
        self.ps = ps


class Ring:
    def __init__(self, items):
        self.items = items
        self.i = 0

    def next(self):
        it = self.items[self.i % len(self.items)]
        self.i += 1
        return it


def _consts():
    i = np.arange(128)
    same = (i[:, None] // 64) == (i[None, :] // 64)
    c = {}
    c["ident"] = np.eye(128, dtype=np.float32)
    c["ones"] = np.ones((128, 128), np.float32)
    c["mbt"] = (same & (i[:, None] <= i[None, :])).astype(np.float32)
    c["mrev"] = (same & (i[:, None] > i[None, :])).astype(np.float32)
    c["sel0"] = np.zeros((128, 128), np.float32); c["sel0"][:64, :] = 1.0
    c["sel1"] = np.zeros((128, 128), np.float32); c["sel1"][64:, :] = 1.0
    c["selc"] = same.astype(np.float32)
    c["bigls"] = np.where(same & (i[None, :] < i[:, None]), 0.0, 1e4).astype(np.float32)
    c["negu"] = np.where(same & (i[:, None] <= i[None, :]), 0.0, -1e4).astype(np.float32)
    c["lt"] = (i[:, None] < i[None, :]).astype(np.float32)
    names = list(c.keys())
    arr = np.stack([c[n] for n in names], axis=1)
    m = np.zeros((128, 2, 256), np.float32)
    m[:, 0, :128] = (i[:, None] >= i[None, :]); m[:, 0, 128:] = (i[:, None] <= i[None, :])
    m[:, 1, 128:] = (i[:, None] <= i[None, :])
    misc = np.zeros((128, 128), np.float32)
    misc[:, 0:32] = (10000.0 ** (-np.arange(0, 64, 2, dtype=np.float32) / 64))[None, :]
    misc[:, 32:64] = (np.arange(32, dtype=np.float32) * CAP)[None, :]
    misc[:, 64] = (i // 64 == 0); misc[:, 65] = (i // 64 == 1)
    return names, np.ascontiguousarray(arr), m, misc


CNAMES, CARR, SWAMASK, MISC = _consts()
NCONST = len(CNAMES)

OFF = {}
_o = 0
for _n, _w in (("a_q", 512), ("a_k", 512), ("a_v", 512), ("a_z", 512), ("a_ab", 8), ("b_q", 768), ("b_k", 768),
               ("b_v", 768), ("c_q", 256), ("c_k", 256), ("c_v", 512), ("c_r", 512), ("c_low", 16), ("gates", 3072)):
    OFF[_n] = _o
    _o += _w
assert _o == N_IN


class Prog:
    def __init__(self, n_layers=DEPTH, debug=(), stop_after=None):
        self.L = n_layers
        self.debug = set(debug)
        self.stop_after = stop_after
        self.nc = nc = bass.Bass("TRN2", target_bir_lowering=False)
        self.es = ExitStack()
        self.fw = FW(nc, self.es)
        self.dram = {}
        self.dbuf = {}
        self.uid = 0
        self._cc = {}

    def din(self, name, shape, dt=F32):
        self.dram[name] = self.nc.dram_tensor(name, list(shape), dt, kind="ExternalInput").ap()
        self.dbuf[name] = Buf(name)
        return self.dram[name]

    def dscratch(self, name, shape, dt=F32):
        kind = "ExternalOutput" if name in self.debug else "Internal"
        self.dram[name] = self.nc.dram_tensor(name, list(shape), dt, kind=kind).ap()
        self.dbuf[name] = Buf(name)
        return self.dram[name]

    def sb(self, es, name, shape, dt=F32):
        self.uid += 1
        return T(es.enter_context(self.nc.sbuf_tensor(f"{name}_{self.uid}", list(shape), dt)), name)

    def sbring(self, es, name, shape, dt, n):
        return Ring([self.sb(es, f"{name}{i}", shape, dt) for i in range(n)])

    @staticmethod
    def _b(xs):
        return [x.b if isinstance(x, T) else x for x in xs]

    def mm(self, out, lhsT, rhs, r, w, start=True, stop=True):
        nc = self.nc
        return self.fw.op("pe", lambda: nc.tensor.matmul(out, lhsT=lhsT, rhs=rhs, start=start, stop=stop),
                          self._b(r), self._b(w), inc=stop)

    def tr(self, out, in_, ident, r, w, inc=True):
        nc = self.nc
        return self.fw.op("pe", lambda: nc.tensor.transpose(out=out, in_=in_, identity=ident),
                          self._b(r), self._b(w), inc=inc)

    @staticmethod
    def _psw(r, w):
        return list(w) + [x for x in r if isinstance(x, T) and x.ps]

    def act(self, out, in_, func, r, w, **kw):
        nc = self.nc
        return self.fw.op("act", lambda: nc.scalar.activation(out=out, in_=in_, func=func, **kw),
                          self._b(r), self._b(self._psw(r, w)))

    def v(self, eng, method, r, w, *a, **kw):
        e = self.nc.vector if eng == "dve" else self.nc.gpsimd
        return self.fw.op(eng, lambda: getattr(e, method)(*a, **kw), self._b(r), self._b(self._psw(r, w)))

    def dma(self, q, out, in_, r, w, **kw):
        return self.fw.dma(q, out, in_, self._b(r), self._b(w), **kw)

    def rsqrt(self, out, in_, addc, r, w):
        self.act(out, in_, AF.Sqrt, r, w, bias=self.constcol(addc))
        self.v("dve", "reciprocal", w, w, out=out, in_=out)

    def constcol(self, val):
        key = float(val)
        if key not in self._cc:
            t = self.sb(self.es, "cc", [128, 1])
            self.v("pool", "memset", [], [t], t.t[:], key)
            self._cc[key] = t
        return self._cc[key].t[:, 0:1]


def bcast_rows(ap, n, parts=128):
    return bass.AP(ap.tensor, ap.offset, [[0, parts], [1, n]])


def build_program(n_layers=DEPTH, debug=(), stop_after=None, moe_decl=NE):
    P = Prog(n_layers, debug, stop_after)
    nc, fw = P.nc, P.fw
    L = n_layers
    x_in = P.din("x", [S, D])
    mem_in = P.din("mem", [MEM, D])
    pos_in = P.din("pos", [128, NT], I32)
    carr_in = P.din("carr", [128, NCONST, 128])
    swam_in = P.din("swamask", [128, 2, 256])
    misc_in = P.din("misc", [128, 128])
    W = {}
    for name, shape in (
        ("norm_mix", [L, D]), ("w_in", [L, D, N_IN]), ("gate_bias", [L, 3 * D]), ("gdn_convT", [L, 1536, 4]),
        ("gdn_a_log", [L, 4]), ("gdn_dt_bias", [L, 4]), ("gdn_norm", [L, 128]), ("swa_q_norm", [L, 64]),
        ("swa_k_norm", [L, 64]), ("gla_gate_up", [L, 16, 256]), ("gla_gate_bias", [L, 256]),
        ("gla_norm", [L, 128]), ("w_branch_a", [L, 512, D]), ("w_branch_b", [L, 256, D]),
        ("w_branch_c", [L, 512, D]), ("w_mix_out", [L, D, D]), ("norm_cross", [L, D]), ("norm_mem", [L, D]),
        ("xa_wq", [L, D, 512]), ("xa_wkv", [L, D, 1024]), ("xa_q_norm", [L, 128]), ("xa_k_norm", [L, 128]),
        ("xa_wo", [L, 512, D]), ("norm_ffn", [L, D]), ("router_w", [L, D, NE]), ("router_b", [L, NE]),
        ("moe_w_in", [L, moe_decl, D, 2 * D]), ("moe_b_in", [L, 128, NE, 16]), ("moe_w_out", [L, moe_decl, D, D]),
        ("moe_b_out", [L, NE, D]),
    ):
        W[name] = P.din(name, shape)
    y = P.nc.dram_tensor("y", [S, D], F32, kind="ExternalOutput").ap()
    P.dram["y"] = y
    P.dbuf["y"] = Buf("y")
    GQKV = P.dscratch("GQKV", [12, 128, S])
    ZS = P.dscratch("ZS", [S, 512], BF16)
    SQ = P.dscratch("SQ", [S, 768], BF16)
    SK = P.dscratch("SK", [S, 768], BF16)
    SV = P.dscratch("SV", [S, 768], BF16)
    CQ = P.dscratch("CQ", [S, 256])
    CK = P.dscratch("CK", [S, 256])
    CV = P.dscratch("CV", [S, 512])
    RS = P.dscratch("RS", [S, 512], BF16)
    LA = P.dscratch("LA", [S, 256])
    GT = P.dscratch("GT", [S, 3 * D], BF16)
    GA = P.dscratch("GA", [S, 512], BF16)
    GC = P.dscratch("GC", [S, 512], BF16)
    NUM = P.dscratch("NUM", [3, S, 260])
    H3 = P.dscratch("H3", [S, D], BF16)
    XB = P.dscratch("XB", [XB_ROWS, D], BF16)
    YB = P.dscratch("YB", [XB_ROWS, D])
    if "HT" in P.debug:
        P.dscratch("HT", [128, 8, S], BF16)
    db = P.dbuf

    es = P.es
    cst = P.sb(es, "cst", [128, NCONST, 128])
    identb = P.sb(es, "identb", [128, 128], BF16)
    onesb = P.sb(es, "onesb", [128, 128], BF16)
    swam = P.sb(es, "swam", [128, 2, 256], BF16)
    misc = P.sb(es, "misc", [128, 128])
    cosT = P.sb(es, "cosT", [128, NT, 32])
    sinT = P.sb(es, "sinT", [128, NT, 32])
    gbs = P.sb(es, "gbs", [128, NT, 8])
    C = {n: cst.t[:, i, :] for i, n in enumerate(CNAMES)}
    ident = C["ident"]
    PS = [T(es.enter_context(nc.psum_tensor(f"ps{i}", [128, 512], F32)), f"ps{i}", ps=True) for i in range(8)]
    psr = Ring(PS)

    for cv in (1.0, D * EPS, 64 * EPS, 128 * EPS, 1e-6):
        P.constcol(cv)
    P.dma("sp", cst.t[:], carr_in, [db["carr"]], [cst])
    P.dma("sp", misc.t[:], misc_in, [db["misc"]], [misc])
    P.dma("pool", swam.t[:], swam_in, [db["swamask"]], [swam])
    P.dma("pool", identb.t[:], carr_in[:, CNAMES.index("ident"), :], [db["carr"]], [identb])
    P.dma("pool", onesb.t[:], carr_in[:, CNAMES.index("ones"), :], [db["carr"]], [onesb])

    with ExitStack() as s0:
        posi = P.sb(s0, "posi", [128, NT], I32)
        posf = P.sb(s0, "posf", [128, NT])
        ang = P.sb(s0, "ang", [128, NT, 32])
        red = P.sb(s0, "red", [128, NT, 32])
        P.dma("sp", posi.t[:], pos_in, [db["pos"]], [posi])
        P.v("dve", "tensor_copy", [posi], [posf], out=posf.t[:], in_=posi.t[:])
        TWO_PI = 2.0 * np.pi
        for t in range(NT):
            P.v("dve", "tensor_scalar", [posf, misc], [ang], out=ang.t[:, t, :], in0=misc.t[:, 0:32],
                scalar1=posf.t[:, t:t + 1], scalar2=None, op0=ALU.mult)
        ki = P.sb(s0, "ki", [128, NT, 32], I32)
        kf = P.sb(s0, "kf", [128, NT, 32])
        for dst, shift in ((sinT, 0.0), (cosT, 0.5 * np.pi)):
            P.v("dve", "tensor_scalar", [ang], [red], out=red.t[:], in0=ang.t[:], scalar1=float(shift),
                scalar2=float(1.0 / TWO_PI), op0=ALU.add, op1=ALU.mult)
            P.v("dve", "tensor_copy", [red], [ki], out=ki.t[:], in_=red.t[:])
            P.v("dve", "tensor_copy", [ki], [kf], out=kf.t[:], in_=ki.t[:])
            P.v("dve", "tensor_scalar", [ang], [red], out=red.t[:], in0=ang.t[:], scalar1=float(shift),
                scalar2=None, op0=ALU.add)
            P.v("dve", "scalar_tensor_tensor", [kf, red], [red], out=red.t[:], in0=kf.t[:], scalar=float(-TWO_PI),
                in1=red.t[:], op0=ALU.mult, op1=ALU.add)
            P.v("dve", "tensor_scalar", [red], [kf], out=kf.t[:], in0=red.t[:], scalar1=float(np.pi),
                scalar2=float(-TWO_PI), op0=ALU.is_gt, op1=ALU.mult)
            P.v("dve", "tensor_tensor", [red, kf], [red], out=red.t[:], in0=red.t[:], in1=kf.t[:], op=ALU.add)
            P.v("dve", "tensor_scalar", [red], [red], out=red.t[:], in0=red.t[:], scalar1=float(-np.pi),
                scalar2=float(np.pi), op0=ALU.max, op1=ALU.min)
            P.act(dst.t[:], red.t[:], AF.Sin, [red], [dst])
        fw.barrier()

    ctx = dict(P=P, W=W, C=C, cst=cst, identb=identb, onesb=onesb, swam=swam, misc=misc, cosT=cosT, sinT=sinT,
               gbs=gbs, psr=psr, PS=PS, y=y, x_in=x_in, mem_in=mem_in)
    for l in range(L):
        xsrc, xb = (x_in, db["x"]) if l == 0 else (y, db["y"])
        phase_A(ctx, l, xsrc, xb)
        if stop_after == ("A", l):
            break
        phase_G(ctx, l)
        if stop_after == ("G", l):
            break
        phase_B(ctx, l)
        if stop_after == ("B", l):
            break
        phase_O(ctx, l, xsrc, xb)
        if stop_after == ("O", l):
            break
        phase_X(ctx, l)
        if stop_after == ("X", l):
            break
        phase_M(ctx, l)
        if stop_after == ("M", l):
            break
    fw.barrier()
    es.close()
    return P


def rms_rows(P, s, xt, rows, ncols, scratch, tag):
    ssq = P.sb(s, "ssq" + tag, [128, 1])
    P.act(scratch.t[:rows, :ncols], xt.t[:rows, :ncols], AF.Square, [xt], [scratch, ssq], accum_out=ssq.t[:rows, :])
    P.rsqrt(ssq.t[:rows, :], ssq.t[:rows, :], ncols * EPS, [ssq], [ssq])
    return ssq


def norm_transpose(ctx, s, src_ap, src_buf, gain_ap, gain_buf, hT, out_rows=None, f32T=None, h3_dst=None):
    P = ctx["P"]; nc = P.nc
    psr = ctx["psr"]; identb = ctx["identb"]
    ntile = src_ap.shape[0] // 128
    with ExitStack() as s1:
        g32 = P.sb(s1, "g32", [128, D])
        P.dma("sp", g32.t[:], bcast_rows(gain_ap, D), [gain_buf], [g32])
        P.v("dve", "tensor_scalar", [g32], [g32], out=g32.t[:], in0=g32.t[:], scalar1=float(np.sqrt(D)), scalar2=None,
            op0=ALU.mult)
        xr = P.sbring(s1, "xr", [128, D], F32, 2)
        jr = P.sbring(s1, "junk", [128, D], F32, 2)
        hr = P.sbring(s1, "hr", [128, D], BF16, 2)
        hfr = P.sbring(s1, "hfr", [128, D], F32, 2) if f32T is not None else None
        sr = Ring([P.sb(s1, f"ssqA{i}", [128, 1]) for i in range(4)])
        for t in range(ntile):
            xt = xr.next(); jk = jr.next(); hb = hr.next(); ssq = sr.next()
            P.dma("sp", xt.t[:], src_ap[t * 128:(t + 1) * 128, :], [src_buf], [xt])
            P.act(jk.t[:], xt.t[:], AF.Square, [xt], [jk, ssq], accum_out=ssq.t[:])
            P.rsqrt(ssq.t[:], ssq.t[:], D * EPS, [ssq], [ssq])
            P.act(jk.t[:], xt.t[:], AF.Copy, [xt, ssq], [jk], scale=ssq.t[:, 0:1])
            if hfr is not None:
                hf = hfr.next()
                P.v("dve", "tensor_tensor", [jk, g32], [hf], out=hf.t[:], in0=jk.t[:], in1=g32.t[:], op=ALU.mult)
                P.v("pool", "tensor_copy", [hf], [hb], out=hb.t[:], in_=hf.t[:])
            else:
                P.v("dve", "tensor_tensor", [jk, g32], [hb], out=hb.t[:], in0=jk.t[:], in1=g32.t[:], op=ALU.mult)
            if h3_dst is not None:
                P.dma("act", h3_dst[0][t * 128:(t + 1) * 128, :], hb.t[:], [hb], [h3_dst[1]])
            ps = psr.next()
            pv = ps.t[:].bitcast(BF16)
            for k in range(8):
                P.tr(pv[:, k * 128:(k + 1) * 128], hb.t[:, k * 128:(k + 1) * 128], identb.t[:], [hb, identb], [ps],
                     inc=(k == 7))
            P.act(hT.t[:, :, t * 128:(t + 1) * 128], pv.rearrange("p (k n) -> p k n", k=8), AF.Copy, [ps], [hT])
            if f32T is not None:
                for half in range(2):
                    ps2 = psr.next()
                    for k in range(4):
                        kk = half * 4 + k
                        P.tr(ps2.t[:, k * 128:(k + 1) * 128], hf.t[:, kk * 128:(kk + 1) * 128], ctx["C"]["ident"],
                             [hf, ctx["cst"]], [ps2], inc=(k == 3))
                    P.v("dve", "tensor_copy", [ps2], [f32T], out=f32T.t[:, half * 4:(half + 1) * 4, t * 128:(t + 1) * 128],
                        in_=ps2.t[:].rearrange("p (k n) -> p k n", k=4))
        P.fw.barrier()


def phase_A(ctx, l, xsrc, xbuf):
    P = ctx["P"]; nc = P.nc; fw = P.fw; W = ctx["W"]; db = P.dbuf; dr = P.dram
    psr = ctx["psr"]; C = ctx["C"]; cst = ctx["cst"]; onesb = ctx["onesb"]; gbs = ctx["gbs"]
    cosT, sinT = ctx["cosT"], ctx["sinT"]
    w_in = W["w_in"][l]
    with ExitStack() as s:
        hT = P.sb(s, "hT", [128, 8, S], BF16)
        norm_transpose(ctx, s, xsrc, xbuf, W["norm_mix"][l], db["norm_mix"], hT)
        if "HT" in P.debug:
            P.dma("sp", dr["HT"], hT.t[:], [hT], [db["HT"]])
        if P.stop_after == ("A0", l):
            fw.barrier()
            return
        wr = P.sbring(s, "wt", [128, 8, 512], BF16, 2)

        def load_w(c0, n):
            wt = wr.next()
            P.dma("pool", wt.t[:, :, :n], w_in[:, c0:c0 + n].rearrange("(k p) n -> p k n", p=128), [db["w_in"]], [wt])
            return wt

        def tjob(c0, n, epi, extra=None):
            wt = load_w(c0, n)
            for t in range(NT):
                ps = psr.next()
                for k in range(8):
                    P.mm(ps.t[:, :n], hT.t[:, k, t * 128:(t + 1) * 128], wt.t[:, k, :n], [hT, wt], [ps],
                         start=(k == 0), stop=(k == 7 and extra is None))
                if extra is not None:
                    extra(ps, n)
                epi(t, ps, n)

        def fjob(c0, n, epi):
            wt = load_w(c0, n)
            for tb in range(S // 512):
                ps = psr.next()
                for k in range(8):
                    P.mm(ps.t[:n, :], wt.t[:, k, :n], hT.t[:, k, tb * 512:(tb + 1) * 512], [hT, wt], [ps],
                         start=(k == 0), stop=(k == 7))
                epi(tb, ps, n)

        with ExitStack() as s2:
          if 'SKIPGDN' not in P.debug:
              cw = P.sb(s2, "cw", [128, 12, 4])
              P.dma("sp", cw.t[:], W["gdn_convT"][l].rearrange("(b p) j -> p b j", p=128), [db["gdn_convT"]], [cw])
              prer = P.sbring(s2, "pre", [128, S + 3], F32, 2)
              accr = P.sbring(s2, "acc", [128, S], F32, 2)
              sqr = P.sbring(s2, "sq", [128, 512], F32, 2)
              rnr = P.sbring(s2, "rn", [128, 512], F32, 2)
              for fb in range(12):
                  pre = prer.next(); acc = accr.next()
                  P.v("pool", "memset", [], [pre], pre.t[:, 0:3], 0.0)

                  def epi(tb, ps, n, pre=pre):
                      P.act(pre.t[:, 3 + tb * 512:3 + (tb + 1) * 512], ps.t[:, :], AF.Copy, [ps], [pre])
                  fjob(OFF["a_q"] + fb * 128, 128, epi)
                  eng = "dve"
                  P.v(eng, "tensor_scalar", [pre, cw], [acc], out=acc.t[:], in0=pre.t[:, 3:3 + S],
                      scalar1=cw.t[:, fb, 3:4], scalar2=None, op0=ALU.mult)
                  for j in range(3):
                      P.v(eng, "scalar_tensor_tensor", [pre, cw, acc], [acc], out=acc.t[:], in0=pre.t[:, j:j + S],
                          scalar=cw.t[:, fb, j:j + 1], in1=acc.t[:], op0=ALU.mult, op1=ALU.add)
                  P.act(acc.t[:], acc.t[:], AF.Silu, [acc], [acc])
                  if fb < 8:
                      qs = (128.0 ** -0.5) if fb < 4 else 1.0
                      for tb in range(S // 512):
                          sq = sqr.next(); rn = rnr.next(); ps = psr.next()
                          sl = slice(tb * 512, (tb + 1) * 512)
                          P.act(sq.t[:], acc.t[:, sl], AF.Square, [acc], [sq])
                          P.mm(ps.t[:], C["ones"], sq.t[:], [cst, sq], [ps])
                          P.rsqrt(rn.t[:], ps.t[:], 1e-6, [ps], [rn])
                          P.v("dve", "scalar_tensor_tensor", [acc, rn], [acc], out=acc.t[:, sl], in0=acc.t[:, sl],
                              scalar=float(qs), in1=rn.t[:], op0=ALU.mult, op1=ALU.mult)
                  P.dma("sp", dr["GQKV"][fb], acc.t[:], [acc], [db["GQKV"]])
          fw.barrier()

        with ExitStack() as s2:
            o16r = P.sbring(s2, "o16", [128, 512], BF16, 3)
            o32r = P.sbring(s2, "o32", [128, 512], F32, 3)

            def store(dst, c0, dt16, func=AF.Copy):
                def epi(t, ps, n):
                    o = (o16r if dt16 else o32r).next()
                    P.act(o.t[:, :n], ps.t[:, :n], func, [ps], [o])
                    P.dma("sp", dr[dst][t * 128:(t + 1) * 128, c0:c0 + n], o.t[:, :n], [o], [db[dst]])
                return epi

            if "ONLYCQ" in P.debug:
                tjob(OFF["c_q"], 256, store("CQ", 0, False))
                fw.barrier()
                return
            tjob(OFF["a_z"], 512, store("ZS", 0, True, AF.Silu))
            par = P.sb(s2, "par", [128, 8])
            P.dma("sp", par.t[:, 0:4], bcast_rows(W["gdn_a_log"][l], 4), [db["gdn_a_log"]], [par])
            P.dma("sp", par.t[:, 4:8], bcast_rows(W["gdn_dt_bias"][l], 4), [db["gdn_dt_bias"]], [par])
            P.act(par.t[:, 0:4], par.t[:, 0:4], AF.Exp, [par], [par])
            t8r = P.sbring(s2, "t8", [128, 8], F32, 2)

            def epi_ab(t, ps, n):
                t8 = t8r.next()
                P.v("dve", "tensor_tensor", [ps, par], [t8], out=t8.t[:, 0:4], in0=ps.t[:, 0:4], in1=par.t[:, 4:8],
                    op=ALU.add)
                P.act(t8.t[:, 0:4], t8.t[:, 0:4], AF.Exp, [t8], [t8])
                P.act(t8.t[:, 0:4], t8.t[:, 0:4], AF.Ln, [t8], [t8], bias=P.constcol(1.0))
                P.v("dve", "scalar_tensor_tensor", [t8, par], [gbs], out=gbs.t[:, t, 0:4], in0=t8.t[:, 0:4],
                    scalar=-1.0, in1=par.t[:, 0:4], op0=ALU.mult, op1=ALU.mult)
                P.act(gbs.t[:, t, 4:8], ps.t[:, 4:8], AF.Sigmoid, [ps], [gbs])
            tjob(OFF["a_ab"], 8, epi_ab)

            gq = P.sb(s2, "gq", [128, 2, 64])
            P.dma("sp", gq.t[:, 0, :], bcast_rows(W["swa_q_norm"][l], 64), [db["swa_q_norm"]], [gq])
            P.dma("sp", gq.t[:, 1, :], bcast_rows(W["swa_k_norm"][l], 64), [db["swa_k_norm"]], [gq])
            P.v("dve", "tensor_scalar", [gq], [gq], out=gq.t[:], in0=gq.t[:], scalar1=8.0, scalar2=None, op0=ALU.mult)
            sqr = P.sbring(s2, "sq2", [128, 512], F32, 2)
            xnr = P.sbring(s2, "xn", [128, 512], F32, 2)
            tmr = P.sbring(s2, "tm", [128, 8, 32], F32, 2)
            ssr = P.sbring(s2, "ss", [128, 8], F32, 2)

            def qk_epi(dst, c0, which):
                def epi(t, ps, n):
                    nh = n // 64
                    sq = sqr.next(); xn = xnr.next(); ss = ssr.next(); o = o16r.next(); tm = tmr.next()
                    P.act(sq.t[:, :n], ps.t[:, :n], AF.Square, [ps], [sq])
                    P.v("dve", "tensor_reduce", [sq], [ss], out=ss.t[:, :nh],
                        in_=sq.t[:, :n].rearrange("p (h d) -> p h d", d=64), axis=AX.X, op=ALU.add)
                    P.rsqrt(ss.t[:, :nh], ss.t[:, :nh], 64 * EPS, [ss], [ss])
                    x3 = xn.t[:, :n].rearrange("p (h d) -> p h d", d=64)
                    P.v("dve", "tensor_tensor", [ps, ss], [xn], out=x3, in0=ps.t[:, :n].rearrange("p (h d) -> p h d", d=64),
                        in1=ss.t[:, :nh].unsqueeze(2).to_broadcast([128, nh, 64]), op=ALU.mult)
                    P.v("pool", "tensor_tensor", [xn, gq], [xn], out=x3, in0=x3,
                        in1=gq.t[:, which:which + 1, :].to_broadcast([128, nh, 64]), op=ALU.mult)
                    o3 = o.t[:, :n].rearrange("p (h d) -> p h d", d=64)
                    cb = cosT.t[:, t:t + 1, :].to_broadcast([128, nh, 32])
                    sb_ = sinT.t[:, t:t + 1, :].to_broadcast([128, nh, 32])
                    x1 = x3[:, :, 0:32]; x2 = x3[:, :, 32:64]
                    P.v("dve", "tensor_tensor", [xn, sinT], [tm], out=tm.t[:, :nh, :], in0=x2, in1=sb_, op=ALU.mult)
                    P.v("pool", "tensor_tensor", [xn, cosT], [sq], out=sq.t[:, :nh * 32].rearrange("p (h d) -> p h d", d=32),
                        in0=x1, in1=cb, op=ALU.mult)
                    P.v("dve", "tensor_tensor", [sq, tm], [o], out=o3[:, :, 0:32],
                        in0=sq.t[:, :nh * 32].rearrange("p (h d) -> p h d", d=32), in1=tm.t[:, :nh, :], op=ALU.subtract)
                    P.v("dve", "tensor_tensor", [xn, sinT], [tm], out=tm.t[:, :nh, :], in0=x1, in1=sb_, op=ALU.mult)
                    P.v("pool", "tensor_tensor", [xn, cosT], [sq], out=sq.t[:, :nh * 32].rearrange("p (h d) -> p h d", d=32),
                        in0=x2, in1=cb, op=ALU.mult)
                    P.v("dve", "tensor_tensor", [sq, tm], [o], out=o3[:, :, 32:64],
                        in0=sq.t[:, :nh * 32].rearrange("p (h d) -> p h d", d=32), in1=tm.t[:, :nh, :], op=ALU.add)
                    P.dma("sp", dr[dst][t * 128:(t + 1) * 128, c0:c0 + n], o.t[:, :n], [o], [db[dst]])
                return epi

            tjob(OFF["b_q"], 512, qk_epi("SQ", 0, 0))
            tjob(OFF["b_q"] + 512, 256, qk_epi("SQ", 512, 0))
            tjob(OFF["b_k"], 512, qk_epi("SK", 0, 1))
            tjob(OFF["b_k"] + 512, 256, qk_epi("SK", 512, 1))
            tjob(OFF["b_v"], 512, store("SV", 0, True))
            tjob(OFF["b_v"] + 512, 256, store("SV", 512, True))
            tjob(OFF["c_q"], 256, store("CQ", 0, False))
            tjob(OFF["c_k"], 256, store("CK", 0, False))
            tjob(OFF["c_v"], 512, store("CV", 0, False))
            tjob(OFF["c_r"], 512, store("RS", 0, True, AF.Silu))
            clT = P.sb(s2, "clT", [32, S])
            gu = P.sb(s2, "gu", [32, 256])
            P.v("pool", "memset", [], [clT], clT.t[:], 1.0)
            P.dma("sp", gu.t[0:16, :], W["gla_gate_up"][l], [db["gla_gate_up"]], [gu])
            P.dma("sp", gu.t[16:17, :], W["gla_gate_bias"][l:l + 1, :], [db["gla_gate_bias"]], [gu])

            def epi_cl(tb, ps, n):
                P.act(clT.t[0:16, tb * 512:(tb + 1) * 512], ps.t[0:16, :], AF.Copy, [ps], [clT])
            fjob(OFF["c_low"], 16, epi_cl)
            for t in range(NT):
                ps = psr.next(); o = o32r.next()
                P.mm(ps.t[:, :256], clT.t[0:17, t * 128:(t + 1) * 128], gu.t[0:17, :], [clT, gu], [ps])
                P.act(o.t[:, :256], ps.t[:, :256], AF.Exp, [ps], [o], scale=-1.0)
                P.act(o.t[:, :256], o.t[:, :256], AF.Ln, [o], [o], bias=P.constcol(1.0))
                P.v("dve", "tensor_scalar", [o], [o], out=o.t[:, :256], in0=o.t[:, :256], scalar1=-1.0 / 16.0,
                    scalar2=None, op0=ALU.mult)
                P.dma("sp", dr["LA"][t * 128:(t + 1) * 128, :], o.t[:, :256], [o], [db["LA"]])
            gbias = P.sb(s2, "gbias", [1, 3 * D], BF16)
            P.dma("pool", gbias.t[:], W["gate_bias"][l:l + 1, :], [db["gate_bias"]], [gbias])
            for j in range(6):
                def extra(ps, n, j=j):
                    P.mm(ps.t[:, :n], onesb.t[0:1, :], gbias.t[0:1, j * 512:(j + 1) * 512], [onesb, gbias], [ps],
                         start=False, stop=True)
                tjob(OFF["gates"] + j * 512, 512, store("GT", j * 512, True, AF.Sigmoid), extra=extra)
        fw.barrier()


def head_norm_gate(P, s, rings, o_t, gain, gate, dst, dst_buf, t):
    jk = rings["jk"].next(); ss = rings["ss"].next(); ob = rings["ob"].next()
    P.act(jk.t[:], o_t.t[:], AF.Square, [o_t], [jk])
    P.v("dve", "tensor_reduce", [jk], [ss], out=ss.t[:], in_=jk.t[:].rearrange("p (h d) -> p h d", d=128), axis=AX.X,
        op=ALU.add)
    P.rsqrt(ss.t[:], ss.t[:], 128 * EPS, [ss], [ss])
    o3 = o_t.t[:].rearrange("p (h d) -> p h d", d=128)
    j3 = jk.t[:].rearrange("p (h d) -> p h d", d=128)
    P.v("dve", "tensor_tensor", [o_t, ss], [jk], out=j3, in0=o3, in1=ss.t[:].unsqueeze(2).to_broadcast([128, 4, 128]),
        op=ALU.mult)
    P.v("pool", "tensor_tensor", [jk, gain], [jk], out=j3, in0=j3, in1=gain.t[:].unsqueeze(1).to_broadcast([128, 4, 128]),
        op=ALU.mult)
    P.v("dve", "tensor_tensor", [jk, gate], [ob], out=ob.t[:], in0=jk.t[:], in1=gate.t[:], op=ALU.mult)
    P.dma("sp", dst[t * 128:(t + 1) * 128, :], ob.t[:], [ob], [dst_buf])


def phase_G(ctx, l):
    P = ctx["P"]; nc = P.nc; fw = P.fw; W = ctx["W"]; db = P.dbuf; dr = P.dram
    C = ctx["C"]; cst = ctx["cst"]; gbs = ctx["gbs"]; misc = ctx["misc"]; PS = ctx["PS"]
    ident = C["ident"]
    with ExitStack() as s:
        PQ = Ring([T(PS[b].t[:, 0:128], b=PS[b].b, ps=True) for b in range(6)])
        PW = Ring([PS[6], PS[7]])
        f128 = lambda nm, n=1: P.sbring(s, nm, [128, 128], F32, n)
        Sg = [P.sb(s, f"Sg{h}", [128, 128]) for h in range(4)]
        Sc = [P.sb(s, f"Sc{h}", [128, 128]) for h in range(4)]
        for h in range(4):
            P.v("pool", "memset", [], [Sg[h]], Sg[h].t[:], 0.0)
            P.v("pool", "memset", [], [Sc[h]], Sc[h].t[:], 0.0)
        gn_a = P.sb(s, "gn_a", [128, 128]); gn_c = P.sb(s, "gn_c", [128, 128])
        for g_, nm in ((gn_a, "gdn_norm"), (gn_c, "gla_norm")):
            P.dma("sp", g_.t[:], bcast_rows(W[nm][l], 128), [db[nm]], [g_])
            P.v("dve", "tensor_scalar", [g_], [g_], out=g_.t[:], in0=g_.t[:], scalar1=float(np.sqrt(128.0)), scalar2=None,
                op0=ALU.mult)
        qkvr = P.sbring(s, "qkv", [128, 12, 128], F32, 2)
        zr = P.sbring(s, "zt", [128, 512], BF16, 2)
        rsr = P.sbring(s, "rst", [128, 512], BF16, 2)
        lar = P.sbring(s, "la", [128, 256], F32, 2)
        cqr = P.sbring(s, "cq", [128, 256], F32, 2)
        ckr = P.sbring(s, "ck", [128, 256], F32, 2)
        cvr = P.sbring(s, "cv", [128, 512], F32, 2)
        gsr = P.sbring(s, "gs", [128, 16], F32, 2)
        exr = P.sbring(s, "ex", [128, 12], F32, 2)
        smr = P.sbring(s, "sm", [128, 16], F32, 2)
        dgr = f128("dg", 2); tlr = f128("tl", 2); tur = f128("tu", 2); dlr = f128("dl", 2); dur = f128("du", 2)
        xmr = f128("xm", 3); xtr = f128("xt", 3); aqr = f128("aq", 2); rtr = f128("rt", 2)
        bvr = f128("bv", 2); kdr = f128("kd", 2); r2r = f128("r2", 2); vnr = f128("vn", 2); tqr = f128("tq", 2)
        oar = P.sbring(s, "oa", [128, 512], F32, 2)
        ocr = P.sbring(s, "oc", [128, 512], F32, 2)
        rings = dict(jk=P.sbring(s, "jkh", [128, 512], F32, 2), ss=P.sbring(s, "ssh", [128, 4], F32, 2),
                     ob=P.sbring(s, "obh", [128, 512], BF16, 2))
        ebr = P.sbring(s, "eb", [128, 256], F32, 2); enr = P.sbring(s, "enb", [128, 256], F32, 2)
        err = P.sbring(s, "erev", [128, 256], F32, 2)
        qtr = P.sbring(s, "qt", [128, 256], F32, 2); ktr = P.sbring(s, "kt", [128, 256], F32, 2)
        kcr = P.sbring(s, "kdc", [128, 256], F32, 2)
        qTr = P.sbring(s, "qtT", [128, 2, 128], F32, 2); kTr = P.sbring(s, "ktT", [128, 2, 128], F32, 2)
        eblr = P.sbring(s, "ebl", [128, 4], F32, 2)
        scr = f128("scT", 2)

        for t in range(P.gnt if hasattr(P, 'gnt') else NT):
            sl = slice(t * 128, (t + 1) * 128)
            qkv = qkvr.next(); zt = zr.next(); rst = rsr.next(); la = lar.next(); cq = cqr.next(); ck = ckr.next()
            cv = cvr.next()
            for g3 in range(3):
                P.dma("sp", qkv.t[:, 4 * g3:4 * g3 + 4, :], dr["GQKV"][4 * g3:4 * g3 + 4, :, sl].rearrange("f p n -> p f n"),
                      [db["GQKV"]], [qkv])
            P.dma("act", zt.t[:], dr["ZS"][sl, :], [db["ZS"]], [zt])
            P.dma("act", rst.t[:], dr["RS"][sl, :], [db["RS"]], [rst])
            P.dma("sp", la.t[:], dr["LA"][sl, :], [db["LA"]], [la])
            P.dma("sp", cq.t[:], dr["CQ"][sl, :], [db["CQ"]], [cq])
            P.dma("act", ck.t[:], dr["CK"][sl, :], [db["CK"]], [ck])
            P.dma("act", cv.t[:], dr["CV"][sl, :], [db["CV"]], [cv])
            if 'NOGDN' not in P.debug:
                gs = gsr.next(); ex = exr.next(); sm = smr.next()
                pg = PQ.next()
                for i, nm in enumerate(("mbt", "sel0", "sel1", "selc")):
                    P.mm(pg.t[:, i * 4:(i + 1) * 4], C[nm], gbs.t[:, t, 0:4], [cst, gbs], [pg])
                P.v("dve", "tensor_copy", [pg], [gs], out=gs.t[:], in_=pg.t[:, 0:16])
                P.act(ex.t[:], gs.t[:, 0:12], AF.Exp, [gs], [ex])
                P.v("dve", "tensor_tensor", [gs], [sm], out=sm.t[:, 12:16], in0=gs.t[:, 12:16], in1=gs.t[:, 0:4],
                    op=ALU.subtract)
                P.act(sm.t[:, 0:4], sm.t[:, 12:16], AF.Exp, [sm], [sm])
                P.v("dve", "tensor_scalar", [gbs], [sm], out=sm.t[:, 4:8], in0=gbs.t[:, t, 4:8], scalar1=-1.0, scalar2=None,
                    op0=ALU.mult)
                P.v("dve", "tensor_tensor", [sm, ex], [sm], out=sm.t[:, 8:12], in0=sm.t[:, 4:8], in1=ex.t[:, 0:4],
                    op=ALU.mult)
                oa = oar.next()
                for h in range(4):
                    qT = qkv.t[:, h, :]; kT = qkv.t[:, 4 + h, :]; vT = qkv.t[:, 8 + h, :]
                    gc_h = gs.t[:, h:h + 1]
                    dg = dgr.next(); tl = tlr.next(); tu = tur.next(); dl = dlr.next(); du = dur.next()
                    P.v("dve", "tensor_scalar", [cst, gs], [dg], out=dg.t[:], in0=ident, scalar1=gc_h, scalar2=None,
                        op0=ALU.mult)
                    pB = PQ.next()
                    P.mm(pB.t[:], C["ones"], dg.t[:], [cst, dg], [pB])
                    P.v("dve", "scalar_tensor_tensor", [pB, gs, cst], [tl], out=tl.t[:], in0=pB.t[:], scalar=gc_h,
                        in1=C["bigls"], op0=ALU.subtract, op1=ALU.max)
                    P.act(dl.t[:], tl.t[:], AF.Exp, [tl], [dl], scale=-1.0)
                    P.v("dve", "scalar_tensor_tensor", [pB, gs, cst], [tu], out=tu.t[:], in0=pB.t[:], scalar=gc_h,
                        in1=C["negu"], op0=ALU.subtract, op1=ALU.min)
                    P.act(du.t[:], tu.t[:], AF.Exp, [tu], [du])
                    pKK = PQ.next(); pKQ = PQ.next()
                    P.mm(pKK.t[:], kT, kT, [qkv], [pKK])
                    P.mm(pKQ.t[:], kT, qT, [qkv], [pKQ])
                    xm = xmr.next(); xt = xtr.next(); aq = aqr.next(); rt = rtr.next()
                    P.v("dve", "scalar_tensor_tensor", [pKK, sm, dl], [xm], out=xm.t[:], in0=pKK.t[:], scalar=sm.t[:, 4 + h:5 + h],
                        in1=dl.t[:], op0=ALU.mult, op1=ALU.mult)
                    P.v("dve", "tensor_tensor", [pKQ, du], [aq], out=aq.t[:], in0=pKQ.t[:], in1=du.t[:], op=ALU.mult)
                    pXT = PQ.next()
                    P.tr(pXT.t[:], xm.t[:], ident, [xm, cst], [pXT])
                    P.act(xt.t[:], pXT.t[:], AF.Copy, [pXT], [xt])
                    P.v("dve", "tensor_tensor", [pXT, cst], [rt], out=rt.t[:], in0=pXT.t[:], in1=ident, op=ALU.add)
                    Pm, PTm = xm, xt
                    for lev in range(5):
                        p1 = PQ.next()
                        P.mm(p1.t[:], PTm.t[:], Pm.t[:], [PTm, Pm], [p1])
                        if lev < 4:
                            p2 = PQ.next()
                            P.mm(p2.t[:], Pm.t[:], PTm.t[:], [PTm, Pm], [p2])
                        n1 = xmr.next()
                        P.act(n1.t[:], p1.t[:], AF.Copy, [p1], [n1])
                        if lev < 4:
                            n2 = xtr.next()
                            P.v("dve", "tensor_copy", [p2], [n2], out=n2.t[:], in_=p2.t[:])
                        p3 = PQ.next()
                        P.mm(p3.t[:], n1.t[:], rt.t[:], [n1, rt], [p3])
                        P.v("dve", "tensor_tensor", [p3, rt], [rt], out=rt.t[:], in0=rt.t[:], in1=p3.t[:], op=ALU.add)
                        Pm = n1
                        if lev < 4:
                            PTm = n2
                    bv = bvr.next(); kd = kdr.next()
                    pV = PQ.next(); pK = PQ.next()
                    P.tr(pV.t[:], vT, ident, [qkv, cst], [pV])
                    P.act(bv.t[:], pV.t[:], AF.Copy, [pV, gbs], [bv], scale=gbs.t[:, t, 4 + h:5 + h])
                    P.tr(pK.t[:], kT, ident, [qkv, cst], [pK])
                    P.act(kd.t[:], pK.t[:], AF.Copy, [pK, sm], [kd], scale=sm.t[:, h:h + 1])
                    r2 = r2r.next(); vn = vnr.next(); tq = tqr.next()
                    for c in range(2):
                        r = slice(64 * c, 64 * c + 64)
                        pKS = PQ.next()
                        P.mm(pKS.t[:], kT, Sg[h].t[:], [qkv, Sg[h]], [pKS])
                        P.v("dve", "scalar_tensor_tensor", [pKS, sm, bv], [r2], out=r2.t[r, :], in0=pKS.t[r, :],
                            scalar=sm.t[r, 8 + h:9 + h], in1=bv.t[r, :], op0=ALU.mult, op1=ALU.add)
                        pVN = PQ.next()
                        P.mm(pVN.t[:], rt.t[r, :], r2.t[r, :], [rt, r2], [pVN])
                        P.act(vn.t[r, :], pVN.t[r, :], AF.Copy, [pVN], [vn])
                        pQS = PQ.next()
                        P.mm(pQS.t[:], qT, Sg[h].t[:], [qkv, Sg[h]], [pQS])
                        P.act(tq.t[r, :], pQS.t[r, :], AF.Copy, [pQS, ex], [tq], scale=ex.t[r, h:h + 1])
                        pAV = PQ.next()
                        P.mm(pAV.t[:], aq.t[r, :], vn.t[r, :], [aq, vn], [pAV])
                        P.v("dve", "tensor_tensor", [pAV, tq], [oa], out=oa.t[r, h * 128:(h + 1) * 128], in0=pAV.t[r, :],
                            in1=tq.t[r, :], op=ALU.add)
                        pSU = PQ.next()
                        P.mm(pSU.t[:], kd.t[r, :], vn.t[r, :], [kd, vn], [pSU])
                        egl = ex.t[:, 4 + 4 * c + h:5 + 4 * c + h]
                        P.v("dve", "scalar_tensor_tensor", [Sg[h], ex, pSU], [Sg[h]], out=Sg[h].t[:], in0=Sg[h].t[:], scalar=egl,
                            in1=pSU.t[:], op0=ALU.mult, op1=ALU.add)
                head_norm_gate(P, s, rings, oa, gn_a, zt, dr["GA"], db["GA"], t)
            if 'NOGLA' not in P.debug:
                eb = ebr.next(); enb = enr.next(); erev = err.next(); qt = qtr.next(); kt = ktr.next(); kdc = kcr.next()
                pb = PW.next()
                P.mm(pb.t[:, 0:256], C["mbt"], la.t[:], [cst, la], [pb])
                P.mm(pb.t[:, 256:512], C["mrev"], la.t[:], [cst, la], [pb])
                P.act(eb.t[:], pb.t[:, 0:256], AF.Exp, [pb], [eb])
                P.act(enb.t[:], pb.t[:, 0:256], AF.Exp, [pb], [enb], scale=-1.0)
                P.act(erev.t[:], pb.t[:, 256:512], AF.Exp, [pb], [erev])
                P.v("dve", "scalar_tensor_tensor", [cq, eb], [qt], out=qt.t[:], in0=cq.t[:], scalar=0.125, in1=eb.t[:],
                    op0=ALU.mult, op1=ALU.mult)
                P.v("pool", "tensor_tensor", [ck, enb], [kt], out=kt.t[:], in0=ck.t[:], in1=enb.t[:], op=ALU.mult)
                P.v("pool", "tensor_tensor", [ck, erev], [kdc], out=kdc.t[:], in0=ck.t[:], in1=erev.t[:], op=ALU.mult)
                qtT = qTr.next(); ktT = kTr.next(); ebl = eblr.next()
                pe_ = PQ.next()
                for p in range(2):
                    pq_ = PQ.next(); pk_ = PQ.next()
                    P.tr(pq_.t[:], qt.t[:, p * 128:(p + 1) * 128], ident, [qt, cst], [pq_])
                    P.act(qtT.t[:, p, :], pq_.t[:], AF.Copy, [pq_], [qtT])
                    P.tr(pk_.t[:], kt.t[:, p * 128:(p + 1) * 128], ident, [kt, cst], [pk_])
                    P.v("dve", "tensor_copy", [pk_], [ktT], out=ktT.t[:, p, :], in_=pk_.t[:])
                    P.mm(pe_.t[:, 2 * p:2 * p + 2], la.t[:, p * 128:(p + 1) * 128], misc.t[:, 64:66], [la, misc], [pe_])
                P.act(ebl.t[:], pe_.t[:, 0:4], AF.Exp, [pe_], [ebl])
                oc = ocr.next()
                for h in range(4):
                    p = h // 2; o = 64 * (h % 2); fo = slice(o, o + 64)
                    scT = scr.next()
                    pS = PQ.next()
                    P.mm(pS.t[:], ktT.t[fo, p, :], qtT.t[fo, p, :], [ktT, qtT], [pS])
                    P.v("dve", "tensor_tensor", [pS, cst], [scT], out=scT.t[:], in0=pS.t[:], in1=C["mbt"], op=ALU.mult)
                    for c in range(2):
                        r = slice(64 * c, 64 * c + 64)
                        pO = PQ.next()
                        P.mm(pO.t[:], qtT.t[:, p, :], Sc[h].t[:, :], [qtT, Sc[h]], [pO], start=True, stop=False)
                        P.mm(pO.t[:], scT.t[:, :], cv.t[:, h * 128:(h + 1) * 128], [scT, cv], [pO], start=False, stop=True)
                        P.act(oc.t[r, h * 128:(h + 1) * 128], pO.t[r, :], AF.Copy, [pO], [oc])
                        pSU = PQ.next()
                        P.mm(pSU.t[:], kdc.t[r, p * 128:(p + 1) * 128], cv.t[r, h * 128:(h + 1) * 128], [kdc, cv], [pSU])
                        P.v("dve", "scalar_tensor_tensor", [Sc[h], ebl, pSU], [Sc[h]], out=Sc[h].t[fo, :], in0=Sc[h].t[fo, :],
                            scalar=ebl.t[fo, 2 * p + c:2 * p + c + 1], in1=pSU.t[fo, :], op0=ALU.mult, op1=ALU.add)
                head_norm_gate(P, s, rings, oc, gn_c, rst, dr["GC"], db["GC"], t)
        fw.barrier()


def phase_B(ctx, l):
    P = ctx["P"]; nc = P.nc; fw = P.fw; db = P.dbuf; dr = P.dram
    identb = ctx["identb"]; swam = ctx["swam"]; PS = ctx["PS"]
    psr = ctx["psr"]
    with ExitStack() as s:
        qr = P.sbring(s, "bq", [128, 256], BF16, 2)
        kr = P.sbring(s, "bk", [128, 256], BF16, 2)
        vr = P.sbring(s, "bv", [128, 256], BF16, 2)
        ver = P.sbring(s, "vext", [128, 4, 65], BF16, 3)
        qkr = P.sbring(s, "qkT", [128, 4, 128], BF16, 3)
        per = P.sbring(s, "pexp", [128, 256], BF16, 4)
        pmr = P.sbring(s, "pm", [128, 256], BF16, 4)
        nor = P.sbring(s, "numsb", [128, 260], F32, 2)
        for it in ver.items:
            P.v("pool", "memset", [], [it], it.t[:], 1.0)
        for it in qkr.items:
            P.v("pool", "memset", [], [it], it.t[:], 0.0)
        prev_qk = qkr.items[-1]; prev_ve = ver.items[-1]
        for g, dil in enumerate((1, 4, 16)):
            Lg = S // dil
            for res in range(dil):
                for n in range(Lg // 128):
                    row0 = res + dil * 128 * n
                    def rows(name, width, c0):
                        a = dr[name]
                        return bass.AP(a.tensor, a.offset + row0 * width + c0, [[dil * width, 128], [1, 256 if width == 768 else 260]])
                    qt = qr.next(); kt = kr.next(); vt = vr.next(); ve = ver.next(); qk = qkr.next()
                    P.dma("sp", qt.t[:], rows("SQ", 768, g * 256), [db["SQ"]], [qt])
                    P.dma("act", kt.t[:], rows("SK", 768, g * 256), [db["SK"]], [kt])
                    P.dma("sp", vt.t[:], rows("SV", 768, g * 256), [db["SV"]], [vt])
                    P.v("pool", "tensor_copy", [vt], [ve], out=ve.t[:, :, 0:64], in_=vt.t[:].rearrange("p (h d) -> p h d", d=64))
                    pt = psr.next()
                    pv = pt.t[:].bitcast(BF16)
                    for i, src in enumerate((qt, qt, kt, kt)):
                        P.tr(pv[:, i * 128:(i + 1) * 128], src.t[:, (i % 2) * 128:(i % 2 + 1) * 128], identb.t[:], [src, identb], [pt],
                             inc=(i == 3))
                    P.act(qk.t[:], pv[:, 0:512].rearrange("p (a n) -> p a n", a=4), AF.Copy, [pt], [qk])
                    pms = []
                    for h in range(4):
                        p = h // 2; fo = slice(64 * (h % 2), 64 * (h % 2) + 64)
                        sc = psr.next()
                        P.mm(sc.t[:, 0:128], prev_qk.t[fo, 2 + p, :], qk.t[fo, p, :], [prev_qk, qk], [sc])
                        P.mm(sc.t[:, 128:256], qk.t[fo, 2 + p, :], qk.t[fo, p, :], [qk], [sc])
                        pe = per.next(); pm = pmr.next()
                        P.act(pe.t[:], sc.t[:, 0:256], AF.Exp, [sc], [pe], scale=0.125)
                        P.v("dve" if h % 2 == 0 else "pool", "tensor_tensor", [pe, swam], [pm], out=pm.t[:], in0=pe.t[:],
                            in1=swam.t[:, 1 if n == 0 else 0, :], op=ALU.mult)
                        pms.append(pm)
                    nu = psr.next()
                    for h in range(4):
                        P.mm(nu.t[:, h * 65:(h + 1) * 65], pms[h].t[:, 0:128], prev_ve.t[:, h, :], [pms[h], prev_ve], [nu],
                             start=True, stop=False)
                        P.mm(nu.t[:, h * 65:(h + 1) * 65], pms[h].t[:, 128:256], ve.t[:, h, :], [pms[h], ve], [nu],
                             start=False, stop=True)
                    no = nor.next()
                    P.act(no.t[:], nu.t[:, 0:260], AF.Copy, [nu], [no])
                    a = dr["NUM"]
                    dst = bass.AP(a.tensor, a.offset + (g * S + row0) * 260, [[dil * 260, 128], [1, 260]])
                    P.dma("sp", dst, no.t[:], [no], [db["NUM"]])
                    prev_qk = qk; prev_ve = ve
        fw.barrier()


def phase_O(ctx, l, xsrc, xbuf):
    P = ctx["P"]; nc = P.nc; fw = P.fw; W = ctx["W"]; db = P.dbuf; dr = P.dram
    identb = ctx["identb"]; psr = ctx["psr"]; y = ctx["y"]
    with ExitStack() as s:
        WA = P.sb(s, "WA", [128, 4, D], BF16); WB = P.sb(s, "WB", [128, 2, D], BF16)
        WC = P.sb(s, "WC", [128, 4, D], BF16); WM = P.sb(s, "WM", [128, 8, D], BF16)
        for wt, nm in ((WA, "w_branch_a"), (WB, "w_branch_b"), (WC, "w_branch_c"), (WM, "w_mix_out")):
            P.dma("pool", wt.t[:], W[nm][l].rearrange("(k p) n -> p k n", p=128), [db[nm]], [wt])
        gar = P.sbring(s, "ga", [128, 512], BF16, 2); gcr = P.sbring(s, "gc", [128, 512], BF16, 2)
        nmr = P.sbring(s, "nm", [128, 3, 260], F32, 2)
        gtr = P.sbring(s, "gt", [128, 3 * D], BF16, 2)
        xr = P.sbring(s, "xo", [128, D], F32, 2)
        rdr = P.sbring(s, "rden", [128, 4], F32, 2)
        obr = P.sbring(s, "obb", [128, 256], BF16, 2)
        aTr = P.sbring(s, "aT", [128, 8, 128], BF16, 2); bTr = P.sbring(s, "bT", [128, 2, 128], BF16, 2)
        t1r = P.sbring(s, "t1", [128, 512], F32, 2); t2r = P.sbring(s, "t2", [128, 512], F32, 2)
        ybr = P.sbring(s, "yb", [128, D], BF16, 2); yTr = P.sbring(s, "yT", [128, 8, 128], BF16, 2)
        for t in range(NT):
            sl = slice(t * 128, (t + 1) * 128)
            ga = gar.next(); gc = gcr.next(); nm = nmr.next(); gt = gtr.next(); xt = xr.next()
            P.dma("sp", ga.t[:], dr["GA"][sl, :], [db["GA"]], [ga])
            P.dma("act", gc.t[:], dr["GC"][sl, :], [db["GC"]], [gc])
            P.dma("sp", nm.t[:], dr["NUM"][:, sl, :].rearrange("g p n -> p g n"), [db["NUM"]], [nm])
            P.dma("act", gt.t[:], dr["GT"][sl, :], [db["GT"]], [gt])
            P.dma("sp", xt.t[:], xsrc[sl, :], [xbuf], [xt])
            P.v("dve", "tensor_tensor", [nm], [nm], out=nm.t[:, 0, :], in0=nm.t[:, 0, :], in1=nm.t[:, 1, :], op=ALU.add)
            P.v("dve", "tensor_tensor", [nm], [nm], out=nm.t[:, 0, :], in0=nm.t[:, 0, :], in1=nm.t[:, 2, :], op=ALU.add)
            rd = rdr.next(); ob = obr.next()
            n3 = nm.t[:, 0, :].rearrange("p (h d) -> p h d", d=65)
            P.v("dve", "reciprocal", [nm], [rd], out=rd.t[:].unsqueeze(2), in_=n3[:, :, 64:65])
            P.v("dve", "tensor_tensor", [nm, rd], [ob], out=ob.t[:].rearrange("p (h d) -> p h d", d=64), in0=n3[:, :, 0:64],
                in1=rd.t[:].unsqueeze(2).to_broadcast([128, 4, 64]), op=ALU.mult)
            aT = aTr.next(); bT = bTr.next()
            pa_ = psr.next(); pva = pa_.t[:].bitcast(BF16)
            for k in range(8):
                src = ga if k < 4 else gc
                P.tr(pva[:, k * 128:(k + 1) * 128], src.t[:, (k % 4) * 128:(k % 4 + 1) * 128], identb.t[:], [src, identb], [pa_],
                     inc=(k == 7))
            P.act(aT.t[:], pva.rearrange("p (k n) -> p k n", k=8), AF.Copy, [pa_], [aT])
            pb_ = psr.next(); pvb = pb_.t[:].bitcast(BF16)
            for k in range(2):
                P.tr(pvb[:, k * 128:(k + 1) * 128], ob.t[:, k * 128:(k + 1) * 128], identb.t[:], [ob, identb], [pb_], inc=(k == 1))
            P.v("dve", "tensor_copy", [pb_], [bT], out=bT.t[:], in_=pvb[:, 0:256].rearrange("p (k n) -> p k n", k=2))
            yb = ybr.next()
            for half in range(2):
                cs = slice(half * 512, (half + 1) * 512)
                pA = psr.next(); pB = psr.next(); pC = psr.next()
                for k in range(4):
                    P.mm(pA.t[:], aT.t[:, k, :], WA.t[:, k, cs], [aT, WA], [pA], start=(k == 0), stop=(k == 3))
                for k in range(2):
                    P.mm(pB.t[:], bT.t[:, k, :], WB.t[:, k, cs], [bT, WB], [pB], start=(k == 0), stop=(k == 1))
                for k in range(4):
                    P.mm(pC.t[:], aT.t[:, 4 + k, :], WC.t[:, k, cs], [aT, WC], [pC], start=(k == 0), stop=(k == 3))
                t1 = t1r.next(); t2 = t2r.next()
                P.v("dve", "tensor_tensor", [pA, gt], [t1], out=t1.t[:], in0=pA.t[:], in1=gt.t[:, half * 512:(half + 1) * 512],
                    op=ALU.mult)
                P.v("dve", "tensor_tensor", [pB, gt], [t2], out=t2.t[:], in0=pB.t[:], in1=gt.t[:, D + half * 512:D + (half + 1) * 512],
                    op=ALU.mult)
                P.v("pool", "tensor_tensor", [t1, t2], [t1], out=t1.t[:], in0=t1.t[:], in1=t2.t[:], op=ALU.add)
                P.v("dve", "tensor_tensor", [pC, gt], [t2], out=t2.t[:], in0=pC.t[:],
                    in1=gt.t[:, 2 * D + half * 512:2 * D + (half + 1) * 512], op=ALU.mult)
                P.v("pool", "tensor_tensor", [t1, t2], [yb], out=yb.t[:, cs], in0=t1.t[:], in1=t2.t[:], op=ALU.add)
            yT = yTr.next()
            py = psr.next(); pvy = py.t[:].bitcast(BF16)
            for k in range(8):
                P.tr(pvy[:, k * 128:(k + 1) * 128], yb.t[:, k * 128:(k + 1) * 128], identb.t[:], [yb, identb], [py], inc=(k == 7))
            P.act(yT.t[:], pvy.rearrange("p (k n) -> p k n", k=8), AF.Copy, [py], [yT])
            for half in range(2):
                cs = slice(half * 512, (half + 1) * 512)
                po = psr.next()
                for k in range(8):
                    P.mm(po.t[:], yT.t[:, k, :], WM.t[:, k, cs], [yT, WM], [po], start=(k == 0), stop=(k == 7))
                P.v("dve", "tensor_tensor", [po, xt], [xt], out=xt.t[:, cs], in0=po.t[:], in1=xt.t[:, cs], op=ALU.add)
            P.dma("sp", y[sl, :], xt.t[:], [xt], [db["y"]])
        fw.barrier()


def head_rms(P, rings, src_ps, gain, out_bf):
    jk = rings["jk"].next(); ss = rings["ss"].next()
    P.act(jk.t[:], src_ps.t[:], AF.Square, [src_ps], [jk])
    P.v("dve", "tensor_reduce", [jk], [ss], out=ss.t[:], in_=jk.t[:].rearrange("p (h d) -> p h d", d=128), axis=AX.X,
        op=ALU.add)
    P.rsqrt(ss.t[:], ss.t[:], 128 * EPS, [ss], [ss])
    j3 = jk.t[:].rearrange("p (h d) -> p h d", d=128)
    P.v("dve", "tensor_tensor", [src_ps, ss], [jk], out=j3, in0=src_ps.t[:].rearrange("p (h d) -> p h d", d=128),
        in1=ss.t[:].unsqueeze(2).to_broadcast([128, 4, 128]), op=ALU.mult)
    P.v("pool", "tensor_tensor", [jk, gain], [out_bf], out=out_bf.t[:].rearrange("p (h d) -> p h d", d=128), in0=j3,
        in1=gain.t[:].unsqueeze(1).to_broadcast([128, 4, 128]), op=ALU.mult)


def phase_X(ctx, l):
    P = ctx["P"]; nc = P.nc; fw = P.fw; W = ctx["W"]; db = P.dbuf; dr = P.dram
    identb = ctx["identb"]; psr = ctx["psr"]; y = ctx["y"]
    with ExitStack() as s:
        hT = P.sb(s, "hTx", [128, 8, S], BF16)
        mT = P.sb(s, "mT", [128, 8, MEM], BF16)
        norm_transpose(ctx, s, y, db["y"], W["norm_cross"][l], db["norm_cross"], hT)
        norm_transpose(ctx, s, ctx["mem_in"], db["mem"], W["norm_mem"][l], db["norm_mem"], mT)
        WQ = P.sb(s, "WQ", [128, 8, 512], BF16); WKV = P.sb(s, "WKV", [128, 8, D], BF16); WO = P.sb(s, "WO", [128, 4, D], BF16)
        for wt, nm in ((WQ, "xa_wq"), (WKV, "xa_wkv"), (WO, "xa_wo")):
            P.dma("pool", wt.t[:], W[nm][l].rearrange("(k p) n -> p k n", p=128), [db[nm]], [wt])
        gq = P.sb(s, "xgq", [128, 128]); gk = P.sb(s, "xgk", [128, 128])
        for g_, nm in ((gq, "xa_q_norm"), (gk, "xa_k_norm")):
            P.dma("sp", g_.t[:], bcast_rows(W[nm][l], 128), [db[nm]], [g_])
            P.v("dve", "tensor_scalar", [g_], [g_], out=g_.t[:], in0=g_.t[:], scalar1=float(np.sqrt(128.0)), scalar2=None,
                op0=ALU.mult)
        rings = dict(jk=P.sbring(s, "jkx", [128, 512], F32, 2), ss=P.sbring(s, "ssx", [128, 4], F32, 2))
        kT = P.sb(s, "kTx", [128, 4, MEM], BF16)
        vext = P.sb(s, "vxx", [128, 2, 4, 129], BF16)
        P.v("pool", "memset", [], [vext], vext.t[:], 1.0)
        khr = P.sbring(s, "khat", [128, 512], BF16, 2)
        for mt in range(2):
            pk = psr.next(); pv_ = psr.next()
            for k in range(8):
                P.mm(pk.t[:], mT.t[:, k, mt * 128:(mt + 1) * 128], WKV.t[:, k, 0:512], [mT, WKV], [pk], start=(k == 0), stop=(k == 7))
            for k in range(8):
                P.mm(pv_.t[:], mT.t[:, k, mt * 128:(mt + 1) * 128], WKV.t[:, k, 512:1024], [mT, WKV], [pv_], start=(k == 0),
                     stop=(k == 7))
            kh = khr.next()
            head_rms(P, rings, pk, gk, kh)
            P.act(vext.t[:, mt, :, 0:128], pv_.t[:].rearrange("p (h d) -> p h d", d=128), AF.Copy, [pv_], [vext])
            pt = psr.next(); pvt = pt.t[:].bitcast(BF16)
            for h in range(4):
                P.tr(pvt[:, h * 128:(h + 1) * 128], kh.t[:, h * 128:(h + 1) * 128], identb.t[:], [kh, identb], [pt], inc=(h == 3))
            P.act(kT.t[:, :, mt * 128:(mt + 1) * 128], pvt[:, 0:512].rearrange("p (h n) -> p h n", h=4), AF.Copy, [pt], [kT])
        qhr = P.sbring(s, "qhat", [128, 512], BF16, 2)
        qTr = P.sbring(s, "qTx", [128, 4, 128], BF16, 2)
        per = P.sbring(s, "pex", [128, 256], BF16, 4)
        nsr = P.sbring(s, "nsx", [128, 4, 129], F32, 2)
        rdr = P.sbring(s, "rdx", [128, 4], F32, 2)
        obr = P.sbring(s, "obx", [128, 512], BF16, 2)
        oTr = P.sbring(s, "oTx", [128, 4, 128], BF16, 2)
        xr = P.sbring(s, "xx", [128, D], F32, 2)
        for t in range(NT):
            sl = slice(t * 128, (t + 1) * 128)
            xt = xr.next()
            P.dma("sp", xt.t[:], y[sl, :], [db["y"]], [xt])
            pq = psr.next()
            for k in range(8):
                P.mm(pq.t[:], hT.t[:, k, sl], WQ.t[:, k, :], [hT, WQ], [pq], start=(k == 0), stop=(k == 7))
            qh = qhr.next(); qT = qTr.next()
            head_rms(P, rings, pq, gq, qh)
            pt = psr.next(); pvt = pt.t[:].bitcast(BF16)
            for h in range(4):
                P.tr(pvt[:, h * 128:(h + 1) * 128], qh.t[:, h * 128:(h + 1) * 128], identb.t[:], [qh, identb], [pt], inc=(h == 3))
            P.act(qT.t[:], pvt[:, 0:512].rearrange("p (h n) -> p h n", h=4), AF.Copy, [pt], [qT])
            pes = []
            for h in range(4):
                sc = psr.next()
                for mt in range(2):
                    P.mm(sc.t[:, mt * 128:(mt + 1) * 128], kT.t[:, h, mt * 128:(mt + 1) * 128], qT.t[:, h, :], [kT, qT], [sc])
                pe = per.next()
                P.act(pe.t[:], sc.t[:, 0:256], AF.Exp, [sc], [pe], scale=float(128.0 ** -0.5))
                pes.append(pe)
            ns = nsr.next()
            for hp in range(2):
                nu = psr.next()
                for hh in range(2):
                    h = hp * 2 + hh
                    for mt in range(2):
                        P.mm(nu.t[:, hh * 129:(hh + 1) * 129], pes[h].t[:, mt * 128:(mt + 1) * 128], vext.t[:, mt, h, :],
                             [pes[h], vext], [nu], start=(mt == 0), stop=(mt == 1))
                P.act(ns.t[:, hp * 2:hp * 2 + 2, :], nu.t[:, 0:258].rearrange("p (h d) -> p h d", d=129), AF.Copy, [nu], [ns])
            rd = rdr.next(); ob = obr.next(); oT = oTr.next()
            P.v("dve", "reciprocal", [ns], [rd], out=rd.t[:].unsqueeze(2), in_=ns.t[:, :, 128:129])
            P.v("dve", "tensor_tensor", [ns, rd], [ob], out=ob.t[:].rearrange("p (h d) -> p h d", d=128), in0=ns.t[:, :, 0:128],
                in1=rd.t[:].unsqueeze(2).to_broadcast([128, 4, 128]), op=ALU.mult)
            pt2 = psr.next(); pvt2 = pt2.t[:].bitcast(BF16)
            for h in range(4):
                P.tr(pvt2[:, h * 128:(h + 1) * 128], ob.t[:, h * 128:(h + 1) * 128], identb.t[:], [ob, identb], [pt2], inc=(h == 3))
            P.act(oT.t[:], pvt2[:, 0:512].rearrange("p (h n) -> p h n", h=4), AF.Copy, [pt2], [oT])
            for half in range(2):
                cs = slice(half * 512, (half + 1) * 512)
                po = psr.next()
                for k in range(4):
                    P.mm(po.t[:], oT.t[:, k, :], WO.t[:, k, cs], [oT, WO], [po], start=(k == 0), stop=(k == 3))
                P.v("dve", "tensor_tensor", [po, xt], [xt], out=xt.t[:, cs], in0=po.t[:], in1=xt.t[:, cs], op=ALU.add)
            P.dma("sp", y[sl, :], xt.t[:], [xt], [db["y"]])
        fw.barrier()


def phase_M(ctx, l):
    P = ctx["P"]; nc = P.nc; fw = P.fw; W = ctx["W"]; db = P.dbuf; dr = P.dram
    identb = ctx["identb"]; onesb = ctx["onesb"]; psr = ctx["psr"]; y = ctx["y"]; C = ctx["C"]; cst = ctx["cst"]
    misc = ctx["misc"]
    XBd = dr["XB"]; YBd = dr["YB"]
    if "breg" not in ctx:
        ctx["breg"] = nc.gpsimd.to_reg(XB_ROWS - 1)
    breg = ctx["breg"]
    with ExitStack() as s:
        DST = P.sb(s, "DST", [128, NT, 4], I32)
        GTE = P.sb(s, "GTE", [128, NT, 4])
        with ExitStack() as s1:
            g32 = P.sb(s1, "g32m", [128, D])
            P.dma("sp", g32.t[:], bcast_rows(W["norm_ffn"][l], D), [db["norm_ffn"]], [g32])
            P.v("dve", "tensor_scalar", [g32], [g32], out=g32.t[:], in0=g32.t[:], scalar1=float(np.sqrt(D)), scalar2=None,
                op0=ALU.mult)
            RW = P.sb(s1, "RW", [128, 8, NE])
            P.dma("sp", RW.t[:], W["router_w"][l].rearrange("(k p) e -> p k e", p=128), [db["router_w"]], [RW])
            rb = P.sb(s1, "rb", [1, NE])
            P.dma("sp", rb.t[:], W["router_b"][l:l + 1, :], [db["router_b"]], [rb])
            ltb = P.sb(s1, "ltb", [128, 128], BF16)
            P.dma("pool", ltb.t[:], P.dram["carr"][:, CNAMES.index("lt"), :], [db["carr"]], [ltb])
            carry = P.sb(s1, "carry", [128, NE])
            P.v("dve", "memset", [], [carry], carry.t[:], 0.0)
            xr = P.sbring(s1, "xm", [128, D], F32, 2); jr = P.sbring(s1, "jm", [128, D], F32, 2)
            hfr = P.sbring(s1, "hfm", [128, D], F32, 2); hbr = P.sbring(s1, "hbm", [128, D], BF16, 3)
            sqr = Ring([P.sb(s1, f"ssqm{i}", [128, 1]) for i in range(3)])
            hTr = P.sbring(s1, "hTf", [128, 8, 128], F32, 2)
            lgr = P.sbring(s1, "lg", [128, NE], F32, 2); t8r = P.sbring(s1, "top8", [128, 8], F32, 2)
            smr = P.sbring(s1, "smm", [128, 16], F32, 2)
            mkr = P.sbring(s1, "mk", [128, NE], F32, 2); mbr = P.sbring(s1, "mkb", [128, NE], BF16, 2)
            pfr = P.sbring(s1, "posf", [128, NE], F32, 2); ovr = P.sbring(s1, "ovf", [128, NE], F32, 2)
            ohr = P.sbring(s1, "oh", [128, NE], F32, 2)
            for t in range(NT):
                sl = slice(t * 128, (t + 1) * 128)
                xt = xr.next(); jk = jr.next(); hf = hfr.next(); hb = hbr.next(); ssq = sqr.next()
                P.dma("sp", xt.t[:], y[sl, :], [db["y"]], [xt])
                P.act(jk.t[:], xt.t[:], AF.Square, [xt], [jk, ssq], accum_out=ssq.t[:])
                P.rsqrt(ssq.t[:], ssq.t[:], D * EPS, [ssq], [ssq])
                P.act(jk.t[:], xt.t[:], AF.Copy, [xt, ssq], [jk], scale=ssq.t[:, 0:1])
                P.v("dve", "tensor_tensor", [jk, g32], [hf], out=hf.t[:], in0=jk.t[:], in1=g32.t[:], op=ALU.mult)
                P.v("pool", "tensor_copy", [hf], [hb], out=hb.t[:], in_=hf.t[:])
                hTf = hTr.next()
                for half in range(2):
                    ps2 = psr.next()
                    for k in range(4):
                        kk = half * 4 + k
                        P.tr(ps2.t[:, k * 128:(k + 1) * 128], hf.t[:, kk * 128:(kk + 1) * 128], C["ident"], [hf, cst], [ps2],
                             inc=(k == 3))
                    P.act(hTf.t[:, half * 4:(half + 1) * 4, :], ps2.t[:].rearrange("p (k n) -> p k n", k=4), AF.Copy, [ps2], [hTf])
                pl = psr.next()
                for k in range(8):
                    P.mm(pl.t[:, 0:NE], hTf.t[:, k, :], RW.t[:, k, :], [hTf, RW], [pl], start=(k == 0), stop=False)
                P.mm(pl.t[:, 0:NE], C["ones"][0:1, :], rb.t[0:1, :], [cst, rb], [pl], start=False, stop=True)
                lg = lgr.next(); t8 = t8r.next(); sm = smr.next(); mk = mkr.next(); mkb = mbr.next()
                P.act(lg.t[:], pl.t[:, 0:NE], AF.Copy, [pl], [lg])
                P.v("dve", "max", [lg], [t8], out=t8.t[:], in_=lg.t[:])
                P.v("dve", "tensor_scalar", [t8], [sm], out=sm.t[:, 0:1], in0=t8.t[:, 0:1], scalar1=-1.0, scalar2=None, op0=ALU.mult)
                P.act(sm.t[:, 4:8], t8.t[:, 0:4], AF.Exp, [t8, sm], [sm], bias=sm.t[:, 0:1], accum_out=sm.t[:, 1:2])
                P.v("dve", "reciprocal", [sm], [sm], out=sm.t[:, 2:3], in_=sm.t[:, 1:2])
                P.v("dve", "tensor_scalar", [lg, t8], [mk], out=mk.t[:], in0=lg.t[:], scalar1=t8.t[:, 3:4], scalar2=None, op0=ALU.is_ge)
                P.v("pool", "tensor_copy", [mk], [mkb], out=mkb.t[:], in_=mk.t[:])
                pp = psr.next()
                P.mm(pp.t[:, 0:NE], ltb.t[:], mkb.t[:], [ltb, mkb], [pp])
                P.mm(pp.t[:, NE:2 * NE], onesb.t[:], mkb.t[:], [onesb, mkb], [pp])
                posf = pfr.next(); ovf = ovr.next()
                P.v("dve", "tensor_tensor", [pp, carry], [posf], out=posf.t[:], in0=pp.t[:, 0:NE], in1=carry.t[:], op=ALU.add)
                P.v("dve", "tensor_tensor", [pp, carry], [carry], out=carry.t[:], in0=pp.t[:, NE:2 * NE], in1=carry.t[:], op=ALU.add)
                P.v("dve", "tensor_scalar", [posf], [ovf], out=ovf.t[:], in0=posf.t[:], scalar1=float(CAP), scalar2=1e7,
                    op0=ALU.is_ge, op1=ALU.mult)
                P.v("dve", "tensor_tensor", [posf, misc], [posf], out=posf.t[:], in0=posf.t[:], in1=misc.t[:, 32:64], op=ALU.add)
                P.v("dve", "tensor_tensor", [posf, ovf], [posf], out=posf.t[:], in0=posf.t[:], in1=ovf.t[:], op=ALU.add)
                for j in range(4):
                    oh = ohr.next()
                    P.v("dve", "tensor_scalar", [lg, t8], [oh], out=oh.t[:], in0=lg.t[:], scalar1=t8.t[:, j:j + 1], scalar2=None,
                        op0=ALU.is_equal)
                    P.v("dve", "tensor_tensor", [oh, posf], [oh], out=oh.t[:], in0=oh.t[:], in1=posf.t[:], op=ALU.mult)
                    P.v("dve", "tensor_reduce", [oh], [sm], out=sm.t[:, 8 + j:9 + j], in_=oh.t[:], axis=AX.X, op=ALU.add)
                P.v("dve", "tensor_scalar", [sm], [sm], out=sm.t[:, 12:16], in0=sm.t[:, 8:12], scalar1=float(XB_ROWS), scalar2=None,
                    op0=ALU.is_lt)
                P.v("dve", "scalar_tensor_tensor", [sm], [GTE], out=GTE.t[:, t, :], in0=sm.t[:, 4:8], scalar=sm.t[:, 2:3],
                    in1=sm.t[:, 12:16], op0=ALU.mult, op1=ALU.mult)
                P.v("dve", "tensor_copy", [sm], [DST], out=DST.t[:, t, :], in_=sm.t[:, 8:12])
                for j in range(4):
                    fw.idma(XBd, bass.IndirectOffsetOnAxis(ap=DST.t[:, t, j:j + 1], axis=0), hb.t[:, :], None,
                            reads=[hb.b, DST.b], writes=[db["XB"]], bounds_check=breg, oob_is_err=False)
            fw.barrier()
        with ExitStack() as s2:
            bins = P.sb(s2, "bins", [128, NE, 16])
            P.dma("sp", bins.t[:], W["moe_b_in"][l], [db["moe_b_in"]], [bins])
            WIr = P.sbring(s2, "WI", [128, 8, 2 * D], BF16, 2)
            WOr = P.sbring(s2, "WO2", [128, 8, D], BF16, 2)
            bor = P.sbring(s2, "bo", [1, D], BF16, 2)
            xbr = P.sbring(s2, "xbt", [128, D], BF16, 3)
            xTr = P.sbring(s2, "xTe", [128, 8, CAP], BF16, 2)
            aTr = P.sbring(s2, "actT", [128, 8, CAP], BF16, 2)
            gr = P.sbring(s2, "eg", [128, CAP], F32, 2); sgr = P.sbring(s2, "esg", [128, CAP], F32, 2)
            lr = P.sbring(s2, "el", [128, CAP], F32, 2)
            yor = P.sbring(s2, "yo", [128, D], F32, 2)
            for e in range(NE):
                WI = WIr.next(); WO2 = WOr.next(); bo = bor.next()
                for kq in range(4):
                    P.dma("pool", WI.t[:, 2 * kq:2 * kq + 2, :],
                          W["moe_w_in"][l, e, kq * 256:(kq + 1) * 256, :].rearrange("(k p) n -> p k n", p=128), [db["moe_w_in"]], [WI])
                for kq in range(2):
                    P.dma("pool", WO2.t[:, 4 * kq:4 * kq + 4, :],
                          W["moe_w_out"][l, e, kq * 512:(kq + 1) * 512, :].rearrange("(k p) n -> p k n", p=128), [db["moe_w_out"]], [WO2])
                P.dma("pool", bo.t[:], W["moe_b_out"][l, e:e + 1, :], [db["moe_b_out"]], [bo])
                xT = xTr.next(); aT = aTr.next()
                for ct in range(NCAPT):
                    xb_ = xbr.next()
                    r0 = e * CAP + ct * 128
                    P.dma("sp", xb_.t[:], XBd[r0:r0 + 128, :], [db["XB"]], [xb_])
                    pt = psr.next(); pvt = pt.t[:].bitcast(BF16)
                    for k in range(8):
                        P.tr(pvt[:, k * 128:(k + 1) * 128], xb_.t[:, k * 128:(k + 1) * 128], identb.t[:], [xb_, identb], [pt], inc=(k == 7))
                    if ct % 2 == 0:
                        P.act(xT.t[:, :, ct * 128:(ct + 1) * 128], pvt.rearrange("p (k n) -> p k n", k=8), AF.Copy, [pt], [xT])
                    else:
                        P.v("dve", "tensor_copy", [pt], [xT], out=xT.t[:, :, ct * 128:(ct + 1) * 128],
                            in_=pvt.rearrange("p (k n) -> p k n", k=8))
                for fb in range(8):
                    pg0 = psr.next(); pl0 = psr.next(); prm = psr.next()
                    for (pt_, col, f0, n0, n1) in ((pg0, 0, fb, 0, 512), (pl0, 0, fb + 8, 0, 512), (prm, 0, fb, 512, CAP),
                                                    (prm, 128, fb + 8, 512, CAP)):
                        for k in range(8):
                            P.mm(pt_.t[:, col:col + (n1 - n0)], WI.t[:, k, f0 * 128:(f0 + 1) * 128], xT.t[:, k, n0:n1], [WI, xT], [pt_],
                                 start=(k == 0), stop=(k == 7))
                    g_ = gr.next(); sg = sgr.next(); l_ = lr.next()
                    bg = bins.t[:, e, fb:fb + 1]; bl = bins.t[:, e, fb + 8:fb + 9]
                    for (src, c0, c1, d0) in ((pg0, 0, 512, 0), (prm, 0, CAP - 512, 512)):
                        P.v("dve", "tensor_scalar", [src, bins], [g_], out=g_.t[:, d0:d0 + (c1 - c0)], in0=src.t[:, c0:c1], scalar1=bg,
                            scalar2=7.0, op0=ALU.add, op1=ALU.min)
                    for (src, c0, c1, d0) in ((pl0, 0, 512, 0), (prm, 128, 128 + CAP - 512, 512)):
                        P.v("dve", "tensor_scalar", [src, bins], [l_], out=l_.t[:, d0:d0 + (c1 - c0)], in0=src.t[:, c0:c1], scalar1=bl,
                            scalar2=7.0, op0=ALU.add, op1=ALU.min)
                    P.act(sg.t[:], g_.t[:], AF.Sigmoid, [g_], [sg], scale=1.702)
                    P.v("dve", "tensor_scalar", [l_], [l_], out=l_.t[:], in0=l_.t[:], scalar1=-7.0, scalar2=1.0, op0=ALU.max, op1=ALU.add)
                    P.v("dve", "tensor_tensor", [g_, sg], [g_], out=g_.t[:], in0=g_.t[:], in1=sg.t[:], op=ALU.mult)
                    P.v("dve", "tensor_tensor", [g_, l_], [aT], out=aT.t[:, fb, :], in0=g_.t[:], in1=l_.t[:], op=ALU.mult)
                for ct in range(NCAPT):
                    yo = yor.next()
                    for half in range(2):
                        cs = slice(half * 512, (half + 1) * 512)
                        po = psr.next()
                        for k in range(8):
                            P.mm(po.t[:], aT.t[:, k, ct * 128:(ct + 1) * 128], WO2.t[:, k, cs], [aT, WO2], [po], start=(k == 0), stop=False)
                        P.mm(po.t[:], onesb.t[0:1, :], bo.t[0:1, cs], [onesb, bo], [po], start=False, stop=True)
                        P.act(yo.t[:, cs], po.t[:], AF.Copy, [po], [yo])
                    r0 = e * CAP + ct * 128
                    P.dma("sp", YBd[r0:r0 + 128, :], yo.t[:], [yo], [db["YB"]])
            fw.barrier()
        with ExitStack() as s3:
            xr = P.sbring(s3, "xc", [128, D], F32, 2)
            gr_ = P.sbring(s3, "gth", [128, D], F32, 4)
            for it in gr_.items:
                P.v("dve", "memset", [], [it], it.t[:], 0.0)
            for t in range(NT):
                sl = slice(t * 128, (t + 1) * 128)
                xt = xr.next()
                P.dma("sp", xt.t[:], y[sl, :], [db["y"]], [xt])
                for j in range(4):
                    gt_ = gr_.next()
                    fw.idma(gt_.t[:, :], None, YBd, bass.IndirectOffsetOnAxis(ap=DST.t[:, t, j:j + 1], axis=0),
                            reads=[db["YB"], DST.b], writes=[gt_.b], bounds_check=breg, oob_is_err=False)
                    P.v("dve", "scalar_tensor_tensor", [gt_, GTE, xt], [xt], out=xt.t[:], in0=gt_.t[:], scalar=GTE.t[:, t, j:j + 1],
                        in1=xt.t[:], op0=ALU.mult, op1=ALU.add)
                P.dma("sp", y[sl, :], xt.t[:], [xt], [db["y"]])
            fw.barrier()


def make_in_maps(inputs, n_layers=DEPTH, cores=range(8)):
    f = lambda a: np.ascontiguousarray(np.asarray(a))
    shared = {"carr": CARR, "swamask": SWAMASK, "misc": MISC}
    for k, v in inputs.items():
        if k in ("x", "mem", "positions"):
            continue
        a = np.asarray(v)[:n_layers]
        if k == "gdn_conv":
            shared["gdn_convT"] = f(a.transpose(0, 2, 1))
        elif k == "moe_b_in":
            shared[k] = f(a.reshape(n_layers, NE, 16, 128).transpose(0, 3, 1, 2))
        elif k == "gate_bias":
            shared["gate_bias"] = f(a.reshape(n_layers, 3 * D))
        else:
            shared[k] = f(a)
    maps = []
    for c in cores:
        m = dict(shared)
        m["x"] = f(inputs["x"][c])
        m["mem"] = f(inputs["mem"][c])
        m["pos"] = f(np.asarray(inputs["positions"][c]).astype(np.int32).reshape(NT, 128).T)
        maps.append(m)
    return maps


_CACHE = {}


def kernel(**inputs):
    if "prog" not in _CACHE:
        _CACHE["prog"] = build_program()
    P = _CACHE["prog"]
    maps = make_in_maps(inputs)
    res = run_bass_kernel_spmd(P.nc, maps, core_ids=list(range(8)))
    return np.stack([np.asarray(r["y"]).reshape(S, D) for r in res.results], axis=0).astype(np.float32)
```

```python
import numpy as np
from contextlib import ExitStack
import concourse.bass as bass
import concourse.mybir as mybir
from concourse.bass_utils import run_bass_kernel_spmd

F32 = mybir.dt.float32
BF16 = mybir.dt.bfloat16
I32 = mybir.dt.int32
AF = mybir.ActivationFunctionType
ALU = mybir.AluOpType
AX = mybir.AxisListType

D = 1024
S = 4096
NT = S // 128
DEPTH = 4
N_IN = 8984
MEM = 256
NE = 32
CAP = 640
NCAPT = CAP // 128
XB_ROWS = NE * CAP
EPS = 1e-6


class Buf:
    __slots__ = ("name", "writer", "readers")

    def __init__(self, name=""):
        self.name = name
        self.writer = None
        self.readers = {}


class FW:
    def __init__(self, nc, es, n_dma_sems=10):
        self.nc = nc
        self.es = es
        self.eng = {"pe": nc.tensor, "act": nc.scalar, "dve": nc.vector, "pool": nc.gpsimd, "sp": nc.sync}
        self.sem = {}
        self.cnt = {}
        for k in ("pe", "act", "dve", "pool"):
            self.sem[k] = es.enter_context(nc.semaphore("S_" + k))
            self.cnt[k] = 0
        self.dq = {}
        for q in ("sp", "act", "pool"):
            ring = []
            for i in range(n_dma_sems):
                key = f"d_{q}{i}"
                self.sem[key] = es.enter_context(nc.semaphore("S_" + key))
                self.cnt[key] = 0
                ring.append(key)
            self.dq[q] = [ring, 0]
        self.known = {k: {} for k in ("pe", "act", "dve", "pool", "sp")}
        self.n_inst = 0
        self.n_wait = 0

    def _wait(self, stream, dep):
        key, val = dep
        if stream == "pe" and key == "pe":
            return
        if self.known[stream].get(key, 0) >= val:
            return
        self.eng[stream].wait_ge(self.sem[key], val)
        self.known[stream][key] = val
        self.n_wait += 1

    def _deps(self, stream, reads, writes):
        for b in reads:
            if b.writer is not None:
                self._wait(stream, b.writer)
        for b in writes:
            if b.writer is not None:
                self._wait(stream, b.writer)
            for k, v in b.readers.items():
                self._wait(stream, (k, v))

    def _commit(self, done, reads, writes):
        k, v = done
        for b in writes:
            b.writer = done
            b.readers = {}
        for b in reads:
            if b.readers.get(k, 0) < v:
                b.readers[k] = v

    def op(self, stream, fn, reads=(), writes=(), inc=True):
        self._deps(stream, reads, writes)
        ins = fn()
        self.n_inst += 1
        if inc:
            self.cnt[stream] += 1
            ins.then_inc(self.sem[stream], 1)
            done = (stream, self.cnt[stream])
        else:
            done = (stream, self.cnt[stream] + 1)
        self._commit(done, reads, writes)
        return ins

    def _dma_common(self, q, issue, reads, writes):
        ring, idx = self.dq[q]
        key = ring[idx % len(ring)]
        self.dq[q][1] = idx + 1
        if self.cnt[key] > 0:
            self._wait(q, (key, self.cnt[key]))
        self._deps(q, reads, writes)
        ins = issue()
        self.cnt[key] += 16
        ins.then_inc(self.sem[key], 16)
        self.n_inst += 1
        done = (key, self.cnt[key])
        self._commit(done, reads, writes)
        return done

    def dma(self, q, out, in_, reads=(), writes=(), **kw):
        return self._dma_common(q, lambda: self.eng[q].dma_start(out=out, in_=in_, **kw), reads, writes)

    def idma(self, out, out_off, in_, in_off, reads=(), writes=(), **kw):
        return self._dma_common(
            "pool", lambda: self.nc.gpsimd.indirect_dma_start(out, out_off, in_, in_off, **kw), reads, writes)

    def barrier(self):
        for stream in ("pe", "act", "dve", "pool", "sp"):
            for key, c in self.cnt.items():
                if c > 0:
                    self._wait(stream, (key, c))


class T:
    __slots__ = ("t", "b", "ps", "tb")

    def __init__(self, t, name="", b=None, ps=False, tb=None):
        self.t = t
        self.b = b if b is not None else Buf(name)
        self.ps = ps
        self.tb = tb


class Ring:
    def __init__(self, items):
        self.items = items
        self.i = 0

    def next(self):
        it = self.items[self.i % len(self.items)]
        self.i += 1
        return it


def _consts():
    i = np.arange(128)
    same = (i[:, None] // 64) == (i[None, :] // 64)
    c = {}
    c["ident"] = np.eye(128, dtype=np.float32)
    c["ones"] = np.ones((128, 128), np.float32)
    c["mbt"] = (same & (i[:, None] <= i[None, :])).astype(np.float32)
    c["mrev"] = (same & (i[:, None] > i[None, :])).astype(np.float32)
    c["sel0"] = np.zeros((128, 128), np.float32); c["sel0"][:64, :] = 1.0
    c["sel1"] = np.zeros((128, 128), np.float32); c["sel1"][64:, :] = 1.0
    c["selc"] = same.astype(np.float32)
    c["bigls"] = np.where(same & (i[None, :] < i[:, None]), 0.0, 1e4).astype(np.float32)
    c["negu"] = np.where(same & (i[:, None] <= i[None, :]), 0.0, -1e4).astype(np.float32)
    c["lt"] = (i[:, None] < i[None, :]).astype(np.float32)
    names = list(c.keys())
    arr = np.stack([c[n] for n in names], axis=1)
    m = np.zeros((128, 2, 256), np.float32)
    m[:, 0, :128] = (i[:, None] >= i[None, :]); m[:, 0, 128:] = (i[:, None] <= i[None, :])
    m[:, 1, 128:] = (i[:, None] <= i[None, :])
    misc = np.zeros((128, 128), np.float32)
    misc[:, 0:32] = (10000.0 ** (-np.arange(0, 64, 2, dtype=np.float32) / 64))[None, :]
    misc[:, 32:64] = (np.arange(32, dtype=np.float32) * CAP)[None, :]
    misc[:, 64] = (i // 64 == 0); misc[:, 65] = (i // 64 == 1)
    return names, np.ascontiguousarray(arr), m, misc


CNAMES, CARR, SWAMASK, MISC = _consts()
NCONST = len(CNAMES)

OFF = {}
_o = 0
for _n, _w in (("a_q", 512), ("a_k", 512), ("a_v", 512), ("a_z", 512), ("a_ab", 8), ("b_q", 768), ("b_k", 768),
               ("b_v", 768), ("c_q", 256), ("c_k", 256), ("c_v", 512), ("c_r", 512), ("c_low", 16), ("gates", 3072)):
    OFF[_n] = _o
    _o += _w
assert _o == N_IN


class Prog:
    def __init__(self, n_layers=DEPTH, debug=(), stop_after=None):
        self.L = n_layers
        self.debug = set(debug)
        self.stop_after = stop_after
        self.nc = nc = bass.Bass("TRN2", target_bir_lowering=False)
        self.es = ExitStack()
        self.fw = FW(nc, self.es)
        self.dram = {}
        self.dbuf = {}
        self.uid = 0
        self._cc = {}

    def din(self, name, shape, dt=F32):
        self.dram[name] = self.nc.dram_tensor(name, list(shape), dt, kind="ExternalInput").ap()
        self.dbuf[name] = Buf(name)
        return self.dram[name]

    def dscratch(self, name, shape, dt=F32):
        kind = "ExternalOutput" if name in self.debug else "Internal"
        self.dram[name] = self.nc.dram_tensor(name, list(shape), dt, kind=kind).ap()
        self.dbuf[name] = Buf(name)
        return self.dram[name]

    def sb(self, es, name, shape, dt=F32):
        self.uid += 1
        return T(es.enter_context(self.nc.sbuf_tensor(f"{name}_{self.uid}", list(shape), dt)), name)

    def sbring(self, es, name, shape, dt, n):
        return Ring([self.sb(es, f"{name}{i}", shape, dt) for i in range(n)])

    @staticmethod
    def _b(xs):
        return [x.b if isinstance(x, T) else x for x in xs]

    def mm(self, out, lhsT, rhs, r, w, start=True, stop=True):
        nc = self.nc
        return self.fw.op("pe", lambda: nc.tensor.matmul(out, lhsT=lhsT, rhs=rhs, start=start, stop=stop),
                          self._b(r), self._b(w), inc=stop)

    def tr(self, out, in_, ident, r, w, inc=True):
        nc = self.nc
        return self.fw.op("pe", lambda: nc.tensor.transpose(out=out, in_=in_, identity=ident),
                          self._b(r), self._b(w), inc=inc)

    @staticmethod
    def _psw(r, w):
        return list(w) + [x for x in r if isinstance(x, T) and x.ps]

    def act(self, out, in_, func, r, w, **kw):
        nc = self.nc
        return self.fw.op("act", lambda: nc.scalar.activation(out=out, in_=in_, func=func, **kw),
                          self._b(r), self._b(self._psw(r, w)))

    def v(self, eng, method, r, w, *a, **kw):
        e = self.nc.vector if eng == "dve" else self.nc.gpsimd
        return self.fw.op(eng, lambda: getattr(e, method)(*a, **kw), self._b(r), self._b(self._psw(r, w)))

    def dma(self, q, out, in_, r, w, **kw):
        return self.fw.dma(q, out, in_, self._b(r), self._b(w), **kw)

    def rsqrt(self, out, in_, addc, r, w):
        self.act(out, in_, AF.Sqrt, r, w, bias=self.constcol(addc))
        self.v("dve", "reciprocal", w, w, out=out, in_=out)

    def constcol(self, val):
        key = float(val)
        if key not in self._cc:
            t = self.sb(self.es, "cc", [128, 1])
            self.v("pool", "memset", [], [t], t.t[:], key)
            self._cc[key] = t
        return self._cc[key].t[:, 0:1]


def bcast_rows(ap, n, parts=128):
    return bass.AP(ap.tensor, ap.offset, [[0, parts], [1, n]])


def build_program(n_layers=DEPTH, debug=(), stop_after=None, moe_decl=NE):
    P = Prog(n_layers, debug, stop_after)
    nc, fw = P.nc, P.fw
    L = n_layers
    x_in = P.din("x", [S, D])
    mem_in = P.din("mem", [MEM, D])
    pos_in = P.din("pos", [128, NT], I32)
    carr_in = P.din("carr", [128, NCONST, 128])
    swam_in = P.din("swamask", [128, 2, 256])
    misc_in = P.din("misc", [128, 128])
    W = {}
    for name, shape in (
        ("norm_mix", [L, D]), ("w_in", [L, D, N_IN]), ("gate_bias", [L, 3 * D]), ("gdn_convT", [L, 1536, 4]),
        ("gdn_a_log", [L, 4]), ("gdn_dt_bias", [L, 4]), ("gdn_norm", [L, 128]), ("swa_q_norm", [L, 64]),
        ("swa_k_norm", [L, 64]), ("gla_gate_up", [L, 16, 256]), ("gla_gate_bias", [L, 256]),
        ("gla_norm", [L, 128]), ("w_branch_a", [L, 512, D]), ("w_branch_b", [L, 256, D]),
        ("w_branch_c", [L, 512, D]), ("w_mix_out", [L, D, D]), ("norm_cross", [L, D]), ("norm_mem", [L, D]),
        ("xa_wq", [L, D, 512]), ("xa_wkv", [L, D, 1024]), ("xa_q_norm", [L, 128]), ("xa_k_norm", [L, 128]),
        ("xa_wo", [L, 512, D]), ("norm_ffn", [L, D]), ("router_w", [L, D, NE]), ("router_b", [L, NE]),
        ("moe_w_in", [L, moe_decl, D, 2 * D]), ("moe_b_in", [L, 128, NE, 16]), ("moe_w_out", [L, moe_decl, D, D]),
        ("moe_b_out", [L, NE, D]),
    ):
        W[name] = P.din(name, shape)
    y = P.nc.dram_tensor("y", [S, D], F32, kind="ExternalOutput").ap()
    P.dram["y"] = y
    P.dbuf["y"] = Buf("y")
    GQKV = P.dscratch("GQKV", [12, 128, S], BF16)
    ZS = P.dscratch("ZS", [S, 512], BF16)
    SQ = P.dscratch("SQ", [S, 768], BF16)
    SK = P.dscratch("SK", [S, 768], BF16)
    SV = P.dscratch("SV", [S, 768], BF16)
    CQ = P.dscratch("CQ", [S, 256])
    CK = P.dscratch("CK", [S, 256])
    CV = P.dscratch("CV", [S, 512], BF16)
    RS = P.dscratch("RS", [S, 512], BF16)
    LA = P.dscratch("LA", [S, 256])
    GT = P.dscratch("GT", [S, 3 * D], BF16)
    GA = P.dscratch("GA", [S, 512], BF16)
    GC = P.dscratch("GC", [S, 512], BF16)
    NUM = P.dscratch("NUM", [3, S, 260])
    H3 = P.dscratch("H3", [S, D], BF16)
    XB = P.dscratch("XB", [XB_ROWS, D], BF16)
    YB = P.dscratch("YB", [XB_ROWS, D])
    if "HT" in P.debug:
        P.dscratch("HT", [128, 8, S], BF16)
    db = P.dbuf

    es = P.es
    cst = P.sb(es, "cst", [128, NCONST, 128])
    identb = P.sb(es, "identb", [128, 128], BF16)
    onesb = P.sb(es, "onesb", [128, 128], BF16)
    swam = P.sb(es, "swam", [128, 2, 256], BF16)
    misc = P.sb(es, "misc", [128, 128])
    cosT = P.sb(es, "cosT", [128, NT, 32])
    sinT = P.sb(es, "sinT", [128, NT, 32])
    gbs = P.sb(es, "gbs", [128, NT, 8])
    C = {n: cst.t[:, i, :] for i, n in enumerate(CNAMES)}
    ident = C["ident"]
    PS = [T(es.enter_context(nc.psum_tensor(f"ps{i}", [128, 512], F32)), f"ps{i}", ps=True) for i in range(8)]
    psr = Ring(PS)

    for cv in (1.0, D * EPS, 64 * EPS, 128 * EPS, 1e-6):
        P.constcol(cv)
    P.dma("sp", cst.t[:], carr_in, [db["carr"]], [cst])
    P.dma("sp", misc.t[:], misc_in, [db["misc"]], [misc])
    P.dma("pool", swam.t[:], swam_in, [db["swamask"]], [swam])
    P.dma("pool", identb.t[:], carr_in[:, CNAMES.index("ident"), :], [db["carr"]], [identb])
    P.dma("pool", onesb.t[:], carr_in[:, CNAMES.index("ones"), :], [db["carr"]], [onesb])

    with ExitStack() as s0:
        posi = P.sb(s0, "posi", [128, NT], I32)
        posf = P.sb(s0, "posf", [128, NT])
        ang = P.sb(s0, "ang", [128, NT, 32])
        red = P.sb(s0, "red", [128, NT, 32])
        P.dma("sp", posi.t[:], pos_in, [db["pos"]], [posi])
        P.v("dve", "tensor_copy", [posi], [posf], out=posf.t[:], in_=posi.t[:])
        TWO_PI = 2.0 * np.pi
        for t in range(NT):
            P.v("dve", "tensor_scalar", [posf, misc], [ang], out=ang.t[:, t, :], in0=misc.t[:, 0:32],
                scalar1=posf.t[:, t:t + 1], scalar2=None, op0=ALU.mult)
        ki = P.sb(s0, "ki", [128, NT, 32], I32)
        kf = P.sb(s0, "kf", [128, NT, 32])
        for dst, shift in ((sinT, 0.0), (cosT, 0.5 * np.pi)):
            P.v("dve", "tensor_scalar", [ang], [red], out=red.t[:], in0=ang.t[:], scalar1=float(shift),
                scalar2=float(1.0 / TWO_PI), op0=ALU.add, op1=ALU.mult)
            P.v("dve", "tensor_copy", [red], [ki], out=ki.t[:], in_=red.t[:])
            P.v("dve", "tensor_copy", [ki], [kf], out=kf.t[:], in_=ki.t[:])
            P.v("dve", "tensor_scalar", [ang], [red], out=red.t[:], in0=ang.t[:], scalar1=float(shift),
                scalar2=None, op0=ALU.add)
            P.v("dve", "scalar_tensor_tensor", [kf, red], [red], out=red.t[:], in0=kf.t[:], scalar=float(-TWO_PI),
                in1=red.t[:], op0=ALU.mult, op1=ALU.add)
            P.v("dve", "tensor_scalar", [red], [kf], out=kf.t[:], in0=red.t[:], scalar1=float(np.pi),
                scalar2=float(-TWO_PI), op0=ALU.is_gt, op1=ALU.mult)
            P.v("dve", "tensor_tensor", [red, kf], [red], out=red.t[:], in0=red.t[:], in1=kf.t[:], op=ALU.add)
            P.v("dve", "tensor_scalar", [red], [red], out=red.t[:], in0=red.t[:], scalar1=float(-np.pi),
                scalar2=float(np.pi), op0=ALU.max, op1=ALU.min)
            P.act(dst.t[:], red.t[:], AF.Sin, [red], [dst])
        fw.barrier()

    ctx = dict(P=P, W=W, C=C, cst=cst, identb=identb, onesb=onesb, swam=swam, misc=misc, cosT=cosT, sinT=sinT,
               gbs=gbs, psr=psr, PS=PS, y=y, x_in=x_in, mem_in=mem_in)
    for l in range(L):
        xsrc, xb = (x_in, db["x"]) if l == 0 else (y, db["y"])
        phase_A(ctx, l, xsrc, xb)
        if stop_after == ("A", l):
            break
        phase_G(ctx, l)
        if stop_after == ("G", l):
            break
        phase_O(ctx, l, xsrc, xb)
        if stop_after == ("O", l):
            break
        phase_X(ctx, l)
        if stop_after == ("X", l):
            break
        phase_M(ctx, l)
        if stop_after == ("M", l):
            break
    fw.barrier()
    es.close()
    return P


def rms_rows(P, s, xt, rows, ncols, scratch, tag):
    ssq = P.sb(s, "ssq" + tag, [128, 1])
    P.act(scratch.t[:rows, :ncols], xt.t[:rows, :ncols], AF.Square, [xt], [scratch, ssq], accum_out=ssq.t[:rows, :])
    P.rsqrt(ssq.t[:rows, :], ssq.t[:rows, :], ncols * EPS, [ssq], [ssq])
    return ssq


def norm_transpose(ctx, s, src_ap, src_buf, gain_ap, gain_buf, hT, out_rows=None, f32T=None, h3_dst=None):
    P = ctx["P"]; nc = P.nc
    psr = ctx["psr"]; identb = ctx["identb"]
    ntile = src_ap.shape[0] // 128
    with ExitStack() as s1:
        g32 = P.sb(s1, "g32", [128, D])
        P.dma("sp", g32.t[:], bcast_rows(gain_ap, D), [gain_buf], [g32])
        P.v("dve", "tensor_scalar", [g32], [g32], out=g32.t[:], in0=g32.t[:], scalar1=float(np.sqrt(D)), scalar2=None,
            op0=ALU.mult)
        xr = P.sbring(s1, "xr", [128, D], F32, 2)
        jr = P.sbring(s1, "junk", [128, D], F32, 2)
        hr = P.sbring(s1, "hr", [128, D], BF16, 2)
        hfr = P.sbring(s1, "hfr", [128, D], F32, 2) if f32T is not None else None
        sr = Ring([P.sb(s1, f"ssqA{i}", [128, 1]) for i in range(4)])
        for t in range(ntile):
            xt = xr.next(); jk = jr.next(); hb = hr.next(); ssq = sr.next()
            P.dma("sp", xt.t[:], src_ap[t * 128:(t + 1) * 128, :], [src_buf], [xt])
            P.act(jk.t[:], xt.t[:], AF.Square, [xt], [jk, ssq], accum_out=ssq.t[:])
            P.rsqrt(ssq.t[:], ssq.t[:], D * EPS, [ssq], [ssq])
            P.act(jk.t[:], xt.t[:], AF.Copy, [xt, ssq], [jk], scale=ssq.t[:, 0:1])
            if hfr is not None:
                hf = hfr.next()
                P.v("dve", "tensor_tensor", [jk, g32], [hf], out=hf.t[:], in0=jk.t[:], in1=g32.t[:], op=ALU.mult)
                P.v("pool", "tensor_copy", [hf], [hb], out=hb.t[:], in_=hf.t[:])
            else:
                P.v("dve", "tensor_tensor", [jk, g32], [hb], out=hb.t[:], in0=jk.t[:], in1=g32.t[:], op=ALU.mult)
            if h3_dst is not None:
                P.dma("act", h3_dst[0][t * 128:(t + 1) * 128, :], hb.t[:], [hb], [h3_dst[1]])
            ps = psr.next()
            pv = ps.t[:].bitcast(BF16)
            for k in range(8):
                P.tr(pv[:, k * 128:(k + 1) * 128], hb.t[:, k * 128:(k + 1) * 128], identb.t[:], [hb, identb], [ps],
                     inc=(k == 7))
            P.act(hT.t[:, :, t * 128:(t + 1) * 128], pv.rearrange("p (k n) -> p k n", k=8), AF.Copy, [ps], [hT])
            if f32T is not None:
                for half in range(2):
                    ps2 = psr.next()
                    for k in range(4):
                        kk = half * 4 + k
                        P.tr(ps2.t[:, k * 128:(k + 1) * 128], hf.t[:, kk * 128:(kk + 1) * 128], ctx["C"]["ident"],
                             [hf, ctx["cst"]], [ps2], inc=(k == 3))
                    P.v("dve", "tensor_copy", [ps2], [f32T], out=f32T.t[:, half * 4:(half + 1) * 4, t * 128:(t + 1) * 128],
                        in_=ps2.t[:].rearrange("p (k n) -> p k n", k=4))
        P.fw.barrier()


def phase_A(ctx, l, xsrc, xbuf):
    P = ctx["P"]; nc = P.nc; fw = P.fw; W = ctx["W"]; db = P.dbuf; dr = P.dram
    psr = ctx["psr"]; C = ctx["C"]; cst = ctx["cst"]; onesb = ctx["onesb"]; gbs = ctx["gbs"]
    cosT, sinT = ctx["cosT"], ctx["sinT"]
    w_in = W["w_in"][l]
    with ExitStack() as s:
        hT = P.sb(s, "hT", [128, 8, S], BF16)
        norm_transpose(ctx, s, xsrc, xbuf, W["norm_mix"][l], db["norm_mix"], hT)
        if "HT" in P.debug:
            P.dma("sp", dr["HT"], hT.t[:], [hT], [db["HT"]])
        if P.stop_after == ("A0", l):
            fw.barrier()
            return
        wr = P.sbring(s, "wt", [128, 8, 512], BF16, 2)

        def load_w(c0, n):
            wt = wr.next()
            P.dma("pool", wt.t[:, :, :n], w_in[:, c0:c0 + n].rearrange("(k p) n -> p k n", p=128), [db["w_in"]], [wt])
            return wt

        def tjob(c0, n, epi, extra=None):
            wt = load_w(c0, n)
            for t in range(NT):
                ps = psr.next()
                for k in range(8):
                    P.mm(ps.t[:, :n], hT.t[:, k, t * 128:(t + 1) * 128], wt.t[:, k, :n], [hT, wt], [ps],
                         start=(k == 0), stop=(k == 7 and extra is None))
                if extra is not None:
                    extra(ps, n)
                epi(t, ps, n)

        def fjob(c0, n, epi):
            wt = load_w(c0, n)
            for tb in range(S // 512):
                ps = psr.next()
                for k in range(8):
                    P.mm(ps.t[:n, :], wt.t[:, k, :n], hT.t[:, k, tb * 512:(tb + 1) * 512], [hT, wt], [ps],
                         start=(k == 0), stop=(k == 7))
                epi(tb, ps, n)

        with ExitStack() as s2:
          if 'SKIPGDN' not in P.debug:
              cw = P.sb(s2, "cw", [128, 12, 4])
              P.dma("sp", cw.t[:], W["gdn_convT"][l].rearrange("(b p) j -> p b j", p=128), [db["gdn_convT"]], [cw])
              prer = P.sbring(s2, "pre", [128, S + 3], F32, 2)
              accr = P.sbring(s2, "acc", [128, S], F32, 2)
              accbr = P.sbring(s2, "accb", [128, S], BF16, 2)
              sqr = P.sbring(s2, "sq", [128, 512], F32, 2)
              rnr = P.sbring(s2, "rn", [128, 512], F32, 2)
              for fb in range(12):
                  pre = prer.next(); acc = accr.next()
                  P.v("pool", "memset", [], [pre], pre.t[:, 0:3], 0.0)

                  def epi(tb, ps, n, pre=pre):
                      P.act(pre.t[:, 3 + tb * 512:3 + (tb + 1) * 512], ps.t[:, :], AF.Copy, [ps], [pre])
                  fjob(OFF["a_q"] + fb * 128, 128, epi)
                  eng = "dve"
                  P.v(eng, "tensor_scalar", [pre, cw], [acc], out=acc.t[:], in0=pre.t[:, 3:3 + S],
                      scalar1=cw.t[:, fb, 3:4], scalar2=None, op0=ALU.mult)
                  for j in range(3):
                      P.v(eng, "scalar_tensor_tensor", [pre, cw, acc], [acc], out=acc.t[:], in0=pre.t[:, j:j + S],
                          scalar=cw.t[:, fb, j:j + 1], in1=acc.t[:], op0=ALU.mult, op1=ALU.add)
                  accb = accbr.next()
                  if fb < 8:
                      P.act(acc.t[:], acc.t[:], AF.Silu, [acc], [acc])
                      qs = (128.0 ** -0.5) if fb < 4 else 1.0
                      for tb in range(S // 512):
                          sq = sqr.next(); rn = rnr.next(); ps = psr.next()
                          sl = slice(tb * 512, (tb + 1) * 512)
                          P.act(sq.t[:], acc.t[:, sl], AF.Square, [acc], [sq])
                          P.mm(ps.t[:], C["ones"], sq.t[:], [cst, sq], [ps])
                          P.rsqrt(rn.t[:], ps.t[:], 1e-6, [ps], [rn])
                          P.v("dve", "scalar_tensor_tensor", [acc, rn], [accb], out=accb.t[:, sl], in0=acc.t[:, sl],
                              scalar=float(qs), in1=rn.t[:], op0=ALU.mult, op1=ALU.mult)
                  else:
                      P.act(accb.t[:], acc.t[:], AF.Silu, [acc], [accb])
                  P.dma("sp", dr["GQKV"][fb], accb.t[:], [accb], [db["GQKV"]])
          fw.barrier()

        with ExitStack() as s2:
            o16r = P.sbring(s2, "o16", [128, 512], BF16, 3)
            o32r = P.sbring(s2, "o32", [128, 512], F32, 3)

            def store(dst, c0, dt16, func=AF.Copy):
                def epi(t, ps, n):
                    o = (o16r if dt16 else o32r).next()
                    P.act(o.t[:, :n], ps.t[:, :n], func, [ps], [o])
                    P.dma("sp", dr[dst][t * 128:(t + 1) * 128, c0:c0 + n], o.t[:, :n], [o], [db[dst]])
                return epi

            if "ONLYCQ" in P.debug:
                tjob(OFF["c_q"], 256, store("CQ", 0, False))
                fw.barrier()
                return
            tjob(OFF["a_z"], 512, store("ZS", 0, True, AF.Silu))
            par = P.sb(s2, "par", [128, 8])
            P.dma("sp", par.t[:, 0:4], bcast_rows(W["gdn_a_log"][l], 4), [db["gdn_a_log"]], [par])
            P.dma("sp", par.t[:, 4:8], bcast_rows(W["gdn_dt_bias"][l], 4), [db["gdn_dt_bias"]], [par])
            P.act(par.t[:, 0:4], par.t[:, 0:4], AF.Exp, [par], [par])
            t8r = P.sbring(s2, "t8", [128, 8], F32, 2)

            def epi_ab(t, ps, n):
                t8 = t8r.next()
                P.v("dve", "tensor_tensor", [ps, par], [t8], out=t8.t[:, 0:4], in0=ps.t[:, 0:4], in1=par.t[:, 4:8],
                    op=ALU.add)
                P.act(t8.t[:, 0:4], t8.t[:, 0:4], AF.Exp, [t8], [t8])
                P.act(t8.t[:, 0:4], t8.t[:, 0:4], AF.Ln, [t8], [t8], bias=P.constcol(1.0))
                P.v("dve", "scalar_tensor_tensor", [t8, par], [gbs], out=gbs.t[:, t, 0:4], in0=t8.t[:, 0:4],
                    scalar=-1.0, in1=par.t[:, 0:4], op0=ALU.mult, op1=ALU.mult)
                P.act(gbs.t[:, t, 4:8], ps.t[:, 4:8], AF.Sigmoid, [ps], [gbs])
            tjob(OFF["a_ab"], 8, epi_ab)

            gq = P.sb(s2, "gq", [128, 2, 64])
            P.dma("sp", gq.t[:, 0, :], bcast_rows(W["swa_q_norm"][l], 64), [db["swa_q_norm"]], [gq])
            P.dma("sp", gq.t[:, 1, :], bcast_rows(W["swa_k_norm"][l], 64), [db["swa_k_norm"]], [gq])
            P.v("dve", "tensor_scalar", [gq], [gq], out=gq.t[:], in0=gq.t[:], scalar1=8.0, scalar2=None, op0=ALU.mult)
            sqr = P.sbring(s2, "sq2", [128, 512], F32, 2)
            xnr = P.sbring(s2, "xn", [128, 512], F32, 2)
            tmr = P.sbring(s2, "tm", [128, 8, 32], F32, 2)
            ssr = P.sbring(s2, "ss", [128, 8], F32, 2)

            def qk_epi(dst, c0, which):
                def epi(t, ps, n):
                    nh = n // 64
                    sq = sqr.next(); xn = xnr.next(); ss = ssr.next(); o = o16r.next(); tm = tmr.next()
                    P.act(sq.t[:, :n], ps.t[:, :n], AF.Square, [ps], [sq])
                    P.v("dve", "tensor_reduce", [sq], [ss], out=ss.t[:, :nh],
                        in_=sq.t[:, :n].rearrange("p (h d) -> p h d", d=64), axis=AX.X, op=ALU.add)
                    P.rsqrt(ss.t[:, :nh], ss.t[:, :nh], 64 * EPS, [ss], [ss])
                    x3 = xn.t[:, :n].rearrange("p (h d) -> p h d", d=64)
                    P.v("dve", "tensor_tensor", [ps, ss], [xn], out=x3, in0=ps.t[:, :n].rearrange("p (h d) -> p h d", d=64),
                        in1=ss.t[:, :nh].unsqueeze(2).to_broadcast([128, nh, 64]), op=ALU.mult)
                    P.v("pool", "tensor_tensor", [xn, gq], [xn], out=x3, in0=x3,
                        in1=gq.t[:, which:which + 1, :].to_broadcast([128, nh, 64]), op=ALU.mult)
                    o3 = o.t[:, :n].rearrange("p (h d) -> p h d", d=64)
                    cb = cosT.t[:, t:t + 1, :].to_broadcast([128, nh, 32])
                    sb_ = sinT.t[:, t:t + 1, :].to_broadcast([128, nh, 32])
                    x1 = x3[:, :, 0:32]; x2 = x3[:, :, 32:64]
                    P.v("dve", "tensor_tensor", [xn, sinT], [tm], out=tm.t[:, :nh, :], in0=x2, in1=sb_, op=ALU.mult)
                    P.v("pool", "tensor_tensor", [xn, cosT], [sq], out=sq.t[:, :nh * 32].rearrange("p (h d) -> p h d", d=32),
                        in0=x1, in1=cb, op=ALU.mult)
                    P.v("dve", "tensor_tensor", [sq, tm], [o], out=o3[:, :, 0:32],
                        in0=sq.t[:, :nh * 32].rearrange("p (h d) -> p h d", d=32), in1=tm.t[:, :nh, :], op=ALU.subtract)
                    P.v("dve", "tensor_tensor", [xn, sinT], [tm], out=tm.t[:, :nh, :], in0=x1, in1=sb_, op=ALU.mult)
                    P.v("pool", "tensor_tensor", [xn, cosT], [sq], out=sq.t[:, :nh * 32].rearrange("p (h d) -> p h d", d=32),
                        in0=x2, in1=cb, op=ALU.mult)
                    P.v("dve", "tensor_tensor", [sq, tm], [o], out=o3[:, :, 32:64],
                        in0=sq.t[:, :nh * 32].rearrange("p (h d) -> p h d", d=32), in1=tm.t[:, :nh, :], op=ALU.add)
                    P.dma("sp", dr[dst][t * 128:(t + 1) * 128, c0:c0 + n], o.t[:, :n], [o], [db[dst]])
                return epi

            tjob(OFF["b_q"], 512, qk_epi("SQ", 0, 0))
            tjob(OFF["b_q"] + 512, 256, qk_epi("SQ", 512, 0))
            tjob(OFF["b_k"], 512, qk_epi("SK", 0, 1))
            tjob(OFF["b_k"] + 512, 256, qk_epi("SK", 512, 1))
            tjob(OFF["b_v"], 512, store("SV", 0, True))
            tjob(OFF["b_v"] + 512, 256, store("SV", 512, True))
            tjob(OFF["c_q"], 256, store("CQ", 0, False))
            tjob(OFF["c_k"], 256, store("CK", 0, False))
            tjob(OFF["c_v"], 512, store("CV", 0, True))
            tjob(OFF["c_r"], 512, store("RS", 0, True, AF.Silu))
            clT = P.sb(s2, "clT", [32, S])
            gu = P.sb(s2, "gu", [32, 256])
            P.v("pool", "memset", [], [clT], clT.t[:], 1.0)
            P.dma("sp", gu.t[0:16, :], W["gla_gate_up"][l], [db["gla_gate_up"]], [gu])
            P.dma("sp", gu.t[16:17, :], W["gla_gate_bias"][l:l + 1, :], [db["gla_gate_bias"]], [gu])

            def epi_cl(tb, ps, n):
                P.act(clT.t[0:16, tb * 512:(tb + 1) * 512], ps.t[0:16, :], AF.Copy, [ps], [clT])
            fjob(OFF["c_low"], 16, epi_cl)
            for t in range(NT):
                ps = psr.next(); o = o32r.next()
                P.mm(ps.t[:, :256], clT.t[0:17, t * 128:(t + 1) * 128], gu.t[0:17, :], [clT, gu], [ps])
                P.act(o.t[:, :256], ps.t[:, :256], AF.Exp, [ps], [o], scale=-1.0)
                P.act(o.t[:, :256], o.t[:, :256], AF.Ln, [o], [o], bias=P.constcol(1.0))
                P.v("dve", "tensor_scalar", [o], [o], out=o.t[:, :256], in0=o.t[:, :256], scalar1=-1.0 / 16.0,
                    scalar2=None, op0=ALU.mult)
                P.dma("sp", dr["LA"][t * 128:(t + 1) * 128, :], o.t[:, :256], [o], [db["LA"]])
            gbias = P.sb(s2, "gbias", [1, 3 * D], BF16)
            P.dma("pool", gbias.t[:], W["gate_bias"][l:l + 1, :], [db["gate_bias"]], [gbias])
            for j in range(6):
                def extra(ps, n, j=j):
                    P.mm(ps.t[:, :n], onesb.t[0:1, :], gbias.t[0:1, j * 512:(j + 1) * 512], [onesb, gbias], [ps],
                         start=False, stop=True)
                tjob(OFF["gates"] + j * 512, 512, store("GT", j * 512, True, AF.Sigmoid), extra=extra)
        fw.barrier()


def head_norm_gate(P, s, rings, o_t, gain, gate, dst, dst_buf, t):
    jk = rings["jk"].next(); ss = rings["ss"].next(); ob = rings["ob"].next()
    P.act(jk.t[:], o_t.t[:], AF.Square, [o_t], [jk])
    P.v("dve", "tensor_reduce", [jk], [ss], out=ss.t[:], in_=jk.t[:].rearrange("p (h d) -> p h d", d=128), axis=AX.X,
        op=ALU.add)
    P.rsqrt(ss.t[:], ss.t[:], 128 * EPS, [ss], [ss])
    o3 = o_t.t[:].rearrange("p (h d) -> p h d", d=128)
    j3 = jk.t[:].rearrange("p (h d) -> p h d", d=128)
    P.v("dve", "tensor_tensor", [o_t, ss], [jk], out=j3, in0=o3, in1=ss.t[:].unsqueeze(2).to_broadcast([128, 4, 128]),
        op=ALU.mult)
    P.v("pool", "tensor_tensor", [jk, gain], [jk], out=j3, in0=j3, in1=gain.t[:].unsqueeze(1).to_broadcast([128, 4, 128]),
        op=ALU.mult)
    P.v("dve", "tensor_tensor", [jk, gate], [ob], out=ob.t[:], in0=jk.t[:], in1=gate.t[:], op=ALU.mult)
    P.dma("sp", dst[t * 128:(t + 1) * 128, :], ob.t[:], [ob], [dst_buf])


def phase_G(ctx, l):
    P = ctx["P"]; nc = P.nc; fw = P.fw; W = ctx["W"]; db = P.dbuf; dr = P.dram
    C = ctx["C"]; cst = ctx["cst"]; gbs = ctx["gbs"]; misc = ctx["misc"]; PS = ctx["PS"]; identb = ctx["identb"]
    ident = C["ident"]
    with ExitStack() as s:
        def quarters(b):
            bf = PS[b].t[:].bitcast(BF16)
            return Ring([T(PS[b].t[:, q * 128:(q + 1) * 128], b=PS[b].b, ps=True, tb=bf[:, q * 256:q * 256 + 128])
                         for q in range(4)])
        PQh = [quarters(h) for h in range(4)]
        PQc = Ring([T(PS[b].t[:, 0:128], b=PS[b].b, ps=True, tb=PS[b].t[:].bitcast(BF16)[:, 0:128]) for b in (4, 5, 6)])
        PW = Ring([PS[7]])
        Sg = [P.sb(s, f"Sg{h}", [128, 128]) for h in range(4)]
        Sc = [P.sb(s, f"Sc{h}", [128, 128]) for h in range(4)]
        Sgb = [P.sb(s, f"Sgb{h}", [128, 128], BF16) for h in range(4)]
        Scb = [P.sb(s, f"Scb{h}", [128, 128], BF16) for h in range(4)]
        for h in range(4):
            for st in (Sg[h], Sc[h], Sgb[h], Scb[h]):
                P.v("pool", "memset", [], [st], st.t[:], 0.0)
        gn_a = P.sb(s, "gn_a", [128, 128]); gn_c = P.sb(s, "gn_c", [128, 128])
        for g_, nm in ((gn_a, "gdn_norm"), (gn_c, "gla_norm")):
            P.dma("sp", g_.t[:], bcast_rows(W[nm][l], 128), [db[nm]], [g_])
            P.v("dve", "tensor_scalar", [g_], [g_], out=g_.t[:], in0=g_.t[:], scalar1=float(np.sqrt(128.0)), scalar2=None,
                op0=ALU.mult)
        qkvr = P.sbring(s, "qkv", [128, 12, 128], BF16, 2)
        zr = P.sbring(s, "zt", [128, 512], BF16, 2)
        rsr = P.sbring(s, "rst", [128, 512], BF16, 2)
        lar = P.sbring(s, "la", [128, 256], F32, 2)
        cqr = P.sbring(s, "cq", [128, 256], F32, 2)
        ckr = P.sbring(s, "ck", [128, 256], F32, 2)
        cvr = P.sbring(s, "cv", [128, 512], BF16, 2)
        gsr = P.sbring(s, "gs", [128, 16], F32, 2)
        exr = P.sbring(s, "ex", [128, 12], F32, 2)
        smr = P.sbring(s, "sm", [128, 16], F32, 2)
        HB = []
        for h in range(4):
            d = {}
            for nm in ("dg", "tl", "tu", "dl", "du", "tq"):
                d[nm] = P.sb(s, f"{nm}{h}", [128, 128], F32)
            for nm in ("aq", "rt", "bv", "kd", "r2", "vn", "scT"):
                d[nm] = P.sb(s, f"{nm}{h}", [128, 128], BF16)
            d["xm"] = P.sbring(s, f"xm{h}", [128, 128], BF16, 3)
            d["xt"] = P.sbring(s, f"xt{h}", [128, 128], BF16, 3)
            HB.append(d)
        oar = P.sbring(s, "oa", [128, 512], F32, 2)
        ocr = P.sbring(s, "oc", [128, 512], F32, 2)
        rings = dict(jk=P.sbring(s, "jkh", [128, 512], F32, 2), ss=P.sbring(s, "ssh", [128, 4], F32, 2),
                     ob=P.sbring(s, "obh", [128, 512], BF16, 2))
        ebr = P.sbring(s, "eb", [128, 256], F32, 2); enr = P.sbring(s, "enb", [128, 256], F32, 2)
        err = P.sbring(s, "erev", [128, 256], F32, 2)
        qtr = P.sbring(s, "qt", [128, 256], BF16, 2); ktr = P.sbring(s, "kt", [128, 256], BF16, 2)
        kcr = P.sbring(s, "kdc", [128, 256], BF16, 2)
        qTr = P.sbring(s, "qtT", [128, 2, 128], BF16, 2); kTr = P.sbring(s, "ktT", [128, 2, 128], BF16, 2)
        eblr = P.sbring(s, "ebl", [128, 4], F32, 2)
        mbtb = P.sb(s, "mbtb", [128, 128], BF16)
        P.dma("pool", mbtb.t[:], P.dram["carr"][:, CNAMES.index("mbt"), :], [db["carr"]], [mbtb])
        bgen = phase_B_gen(ctx, l)
        next(bgen)
        b_alive = True

        def gdn_head(h, t, qkv, gs, ex, sm, oa):
            PQ = PQh[h]; B_ = HB[h]
            qT = qkv.t[:, h, :]; kT = qkv.t[:, 4 + h, :]; vT = qkv.t[:, 8 + h, :]
            gc_h = gs.t[:, h:h + 1]
            dg, tl, tu, dl, du, tq = (B_[k] for k in ("dg", "tl", "tu", "dl", "du", "tq"))
            aq, rt, bv, kd, r2, vn = (B_[k] for k in ("aq", "rt", "bv", "kd", "r2", "vn"))
            xmr, xtr = B_["xm"], B_["xt"]
            P.v("dve", "tensor_scalar", [cst, gs], [dg], out=dg.t[:], in0=ident, scalar1=gc_h, scalar2=None, op0=ALU.mult)
            yield
            pB = PQ.next(); pV = PQ.next(); pK = PQ.next()
            P.mm(pB.t[:], C["ones"], dg.t[:], [cst, dg], [pB])
            P.tr(pV.tb, vT, identb.t[:], [qkv, identb], [pV])
            P.tr(pK.tb, kT, identb.t[:], [qkv, identb], [pK])
            yield
            P.v("dve", "scalar_tensor_tensor", [pB, gs, cst], [tl], out=tl.t[:], in0=pB.t[:], scalar=gc_h,
                in1=C["bigls"], op0=ALU.subtract, op1=ALU.max)
            P.v("dve", "scalar_tensor_tensor", [pB, gs, cst], [tu], out=tu.t[:], in0=pB.t[:], scalar=gc_h,
                in1=C["negu"], op0=ALU.subtract, op1=ALU.min)
            P.act(bv.t[:], pV.tb, AF.Copy, [pV, gbs], [bv], scale=gbs.t[:, t, 4 + h:5 + h])
            P.act(kd.t[:], pK.tb, AF.Copy, [pK, sm], [kd], scale=sm.t[:, h:h + 1])
            P.act(dl.t[:], tl.t[:], AF.Exp, [tl], [dl], scale=-1.0)
            P.act(du.t[:], tu.t[:], AF.Exp, [tu], [du])
            yield
            pKK = PQ.next(); pKQ = PQ.next()
            P.mm(pKK.t[:], kT, kT, [qkv], [pKK])
            P.mm(pKQ.t[:], kT, qT, [qkv], [pKQ])
            yield
            xm = xmr.next(); xt = xtr.next()
            P.v("dve", "scalar_tensor_tensor", [pKK, sm, dl], [xm], out=xm.t[:], in0=pKK.t[:], scalar=sm.t[:, 4 + h:5 + h],
                in1=dl.t[:], op0=ALU.mult, op1=ALU.mult)
            P.v("dve", "tensor_tensor", [pKQ, du], [aq], out=aq.t[:], in0=pKQ.t[:], in1=du.t[:], op=ALU.mult)
            yield
            pXT = PQ.next()
            P.tr(pXT.tb, xm.t[:], identb.t[:], [xm, identb], [pXT])
            yield
            P.act(xt.t[:], pXT.tb, AF.Copy, [pXT], [xt])
            P.v("dve", "tensor_tensor", [pXT, identb], [rt], out=rt.t[:], in0=pXT.tb, in1=identb.t[:], op=ALU.add)
            yield
            Pm, PTm = xm, xt
            for lev in range(5):
                p1 = PQ.next()
                P.mm(p1.t[:], PTm.t[:], Pm.t[:], [PTm, Pm], [p1])
                if lev < 4:
                    p2 = PQ.next()
                    P.mm(p2.t[:], Pm.t[:], PTm.t[:], [PTm, Pm], [p2])
                yield
                n1 = xmr.next()
                P.act(n1.t[:], p1.t[:], AF.Copy, [p1], [n1])
                if lev < 4:
                    n2 = xtr.next()
                    P.v("dve", "tensor_copy", [p2], [n2], out=n2.t[:], in_=p2.t[:])
                yield
                p3 = PQ.next()
                P.mm(p3.t[:], n1.t[:], rt.t[:], [n1, rt], [p3])
                yield
                P.v("dve", "tensor_tensor", [p3, rt], [rt], out=rt.t[:], in0=rt.t[:], in1=p3.t[:], op=ALU.add)
                yield
                Pm = n1
                if lev < 4:
                    PTm = n2
            for c in range(2):
                r = slice(64 * c, 64 * c + 64)
                pKS = PQ.next(); pQS = PQ.next()
                P.mm(pKS.t[:], kT, Sgb[h].t[:], [qkv, Sgb[h]], [pKS])
                P.mm(pQS.t[:], qT, Sgb[h].t[:], [qkv, Sgb[h]], [pQS])
                yield
                P.v("dve", "scalar_tensor_tensor", [pKS, sm, bv], [r2], out=r2.t[r, :], in0=pKS.t[r, :],
                    scalar=sm.t[r, 8 + h:9 + h], in1=bv.t[r, :], op0=ALU.mult, op1=ALU.add)
                P.act(tq.t[r, :], pQS.t[r, :], AF.Copy, [pQS, ex], [tq], scale=ex.t[r, h:h + 1])
                yield
                pVN = PQ.next()
                P.mm(pVN.t[:], rt.t[r, :], r2.t[r, :], [rt, r2], [pVN])
                yield
                P.act(vn.t[r, :], pVN.t[r, :], AF.Copy, [pVN], [vn])
                yield
                pAV = PQ.next(); pSU = PQ.next()
                P.mm(pAV.t[:], aq.t[r, :], vn.t[r, :], [aq, vn], [pAV])
                P.mm(pSU.t[:], kd.t[r, :], vn.t[r, :], [kd, vn], [pSU])
                yield
                egl = ex.t[:, 4 + 4 * c + h:5 + 4 * c + h]
                P.v("dve", "scalar_tensor_tensor", [Sg[h], ex, pSU], [Sg[h]], out=Sg[h].t[:], in0=Sg[h].t[:], scalar=egl,
                    in1=pSU.t[:], op0=ALU.mult, op1=ALU.add)
                P.v("dve", "tensor_tensor", [pAV, tq], [oa], out=oa.t[r, h * 128:(h + 1) * 128], in0=pAV.t[r, :],
                    in1=tq.t[r, :], op=ALU.add)
                P.v("pool", "tensor_copy", [Sg[h]], [Sgb[h]], out=Sgb[h].t[:], in_=Sg[h].t[:])
                yield

        def gla_tile(t, la, cq, ck, cv, oc):
            PQ = PQc
            eb = ebr.next(); enb = enr.next(); erev = err.next(); qt = qtr.next(); kt = ktr.next(); kdc = kcr.next()
            pb = PW.next()
            P.mm(pb.t[:, 0:256], C["mbt"], la.t[:], [cst, la], [pb])
            P.mm(pb.t[:, 256:512], C["mrev"], la.t[:], [cst, la], [pb])
            pe_ = PQ.next()
            for p in range(2):
                P.mm(pe_.t[:, 2 * p:2 * p + 2], la.t[:, p * 128:(p + 1) * 128], misc.t[:, 64:66], [la, misc], [pe_])
            yield
            ebl = eblr.next()
            P.act(eb.t[:], pb.t[:, 0:256], AF.Exp, [pb], [eb])
            P.act(enb.t[:], pb.t[:, 0:256], AF.Exp, [pb], [enb], scale=-1.0)
            P.act(erev.t[:], pb.t[:, 256:512], AF.Exp, [pb], [erev])
            P.act(ebl.t[:], pe_.t[:, 0:4], AF.Exp, [pe_], [ebl])
            yield
            P.v("dve", "scalar_tensor_tensor", [cq, eb], [qt], out=qt.t[:], in0=cq.t[:], scalar=0.125, in1=eb.t[:],
                op0=ALU.mult, op1=ALU.mult)
            P.v("pool", "tensor_tensor", [ck, enb], [kt], out=kt.t[:], in0=ck.t[:], in1=enb.t[:], op=ALU.mult)
            P.v("pool", "tensor_tensor", [ck, erev], [kdc], out=kdc.t[:], in0=ck.t[:], in1=erev.t[:], op=ALU.mult)
            yield
            qtT = qTr.next(); ktT = kTr.next()
            for p in range(2):
                pq_ = PQ.next(); pk_ = PQ.next()
                P.tr(pq_.tb, qt.t[:, p * 128:(p + 1) * 128], identb.t[:], [qt, identb], [pq_])
                P.tr(pk_.tb, kt.t[:, p * 128:(p + 1) * 128], identb.t[:], [kt, identb], [pk_])
                yield
                P.act(qtT.t[:, p, :], pq_.tb, AF.Copy, [pq_], [qtT])
                P.v("dve", "tensor_copy", [pk_], [ktT], out=ktT.t[:, p, :], in_=pk_.tb)
                yield
            for h in range(4):
                p = h // 2; o = 64 * (h % 2); fo = slice(o, o + 64)
                scT = HB[h]["scT"]
                pS = PQ.next()
                P.mm(pS.t[:], ktT.t[fo, p, :], qtT.t[fo, p, :], [ktT, qtT], [pS])
                yield
                P.v("dve", "tensor_tensor", [pS, mbtb], [scT], out=scT.t[:], in0=pS.t[:], in1=mbtb.t[:], op=ALU.mult)
                yield
                for c in range(2):
                    r = slice(64 * c, 64 * c + 64)
                    pO = PQ.next(); pSU = PQ.next()
                    P.mm(pO.t[:], qtT.t[:, p, :], Scb[h].t[:, :], [qtT, Scb[h]], [pO], start=True, stop=False)
                    P.mm(pO.t[:], scT.t[:, :], cv.t[:, h * 128:(h + 1) * 128], [scT, cv], [pO], start=False, stop=True)
                    P.mm(pSU.t[:], kdc.t[r, p * 128:(p + 1) * 128], cv.t[r, h * 128:(h + 1) * 128], [kdc, cv], [pSU])
                    yield
                    P.act(oc.t[r, h * 128:(h + 1) * 128], pO.t[r, :], AF.Copy, [pO], [oc])
                    P.v("dve", "scalar_tensor_tensor", [Sc[h], ebl, pSU], [Sc[h]], out=Sc[h].t[fo, :], in0=Sc[h].t[fo, :],
                        scalar=ebl.t[fo, 2 * p + c:2 * p + c + 1], in1=pSU.t[fo, :], op0=ALU.mult, op1=ALU.add)
                    P.v("pool", "tensor_copy", [Sc[h]], [Scb[h]], out=Scb[h].t[fo, :], in_=Sc[h].t[fo, :])
                    yield

        for t in range(P.gnt if hasattr(P, 'gnt') else NT):
            sl = slice(t * 128, (t + 1) * 128)
            qkv = qkvr.next(); zt = zr.next(); rst = rsr.next(); la = lar.next(); cq = cqr.next(); ck = ckr.next()
            cv = cvr.next()
            for g3 in range(3):
                P.dma("sp", qkv.t[:, 4 * g3:4 * g3 + 4, :], dr["GQKV"][4 * g3:4 * g3 + 4, :, sl].rearrange("f p n -> p f n"),
                      [db["GQKV"]], [qkv])
            P.dma("act", zt.t[:], dr["ZS"][sl, :], [db["ZS"]], [zt])
            P.dma("act", rst.t[:], dr["RS"][sl, :], [db["RS"]], [rst])
            P.dma("sp", la.t[:], dr["LA"][sl, :], [db["LA"]], [la])
            P.dma("sp", cq.t[:], dr["CQ"][sl, :], [db["CQ"]], [cq])
            P.dma("act", ck.t[:], dr["CK"][sl, :], [db["CK"]], [ck])
            P.dma("act", cv.t[:], dr["CV"][sl, :], [db["CV"]], [cv])
            gs = gsr.next(); ex = exr.next(); sm = smr.next()
            pg = PQc.next()
            for i, nm in enumerate(("mbt", "sel0", "sel1", "selc")):
                P.mm(pg.t[:, i * 4:(i + 1) * 4], C[nm], gbs.t[:, t, 0:4], [cst, gbs], [pg])
            P.v("dve", "tensor_copy", [pg], [gs], out=gs.t[:], in_=pg.t[:, 0:16])
            P.act(ex.t[:], gs.t[:, 0:12], AF.Exp, [gs], [ex])
            P.v("dve", "tensor_tensor", [gs], [sm], out=sm.t[:, 12:16], in0=gs.t[:, 12:16], in1=gs.t[:, 0:4],
                op=ALU.subtract)
            P.act(sm.t[:, 0:4], sm.t[:, 12:16], AF.Exp, [sm], [sm])
            P.v("dve", "tensor_scalar", [gbs], [sm], out=sm.t[:, 4:8], in0=gbs.t[:, t, 4:8], scalar1=-1.0, scalar2=None,
                op0=ALU.mult)
            P.v("dve", "tensor_tensor", [sm, ex], [sm], out=sm.t[:, 8:12], in0=sm.t[:, 4:8], in1=ex.t[:, 0:4],
                op=ALU.mult)
            oa = oar.next(); oc = ocr.next()
            gens = []
            if 'NOGDN' not in P.debug:
                gens += [gdn_head(h, t, qkv, gs, ex, sm, oa) for h in range(4)]
            if 'NOGLA' not in P.debug:
                gens.append(gla_tile(t, la, cq, ck, cv, oc))
            while gens:
                for g_ in list(gens):
                    try:
                        next(g_)
                    except StopIteration:
                        gens.remove(g_)
            if 'NOGDN' not in P.debug:
                head_norm_gate(P, s, rings, oa, gn_a, zt, dr["GA"], db["GA"], t)
            if 'NOGLA' not in P.debug:
                head_norm_gate(P, s, rings, oc, gn_c, rst, dr["GC"], db["GC"], t)
            for _ in range(3):
                if b_alive:
                    try:
                        next(bgen)
                    except StopIteration:
                        b_alive = False
        while b_alive:
            try:
                next(bgen)
            except StopIteration:
                b_alive = False
        fw.barrier()


def phase_B_gen(ctx, l):
    P = ctx["P"]; nc = P.nc; fw = P.fw; db = P.dbuf; dr = P.dram
    identb = ctx["identb"]; swam = ctx["swam"]; PS = ctx["PS"]
    psr = ctx["psr"]
    with ExitStack() as s:
        qr = P.sbring(s, "bq", [128, 256], BF16, 2)
        kr = P.sbring(s, "bk", [128, 256], BF16, 2)
        vr = P.sbring(s, "bv", [128, 256], BF16, 2)
        ver = P.sbring(s, "vext", [128, 4, 65], BF16, 3)
        qkr = P.sbring(s, "qkT", [128, 4, 128], BF16, 3)
        per = P.sbring(s, "pexp", [128, 256], BF16, 4)
        pmr = P.sbring(s, "pm", [128, 256], BF16, 4)
        nor = P.sbring(s, "numsb", [128, 260], F32, 2)
        for it in ver.items:
            P.v("pool", "memset", [], [it], it.t[:], 1.0)
        for it in qkr.items:
            P.v("pool", "memset", [], [it], it.t[:], 0.0)
        prev_qk = qkr.items[-1]; prev_ve = ver.items[-1]
        yield
        for g, dil in enumerate((1, 4, 16)):
            Lg = S // dil
            for res in range(dil):
                for n in range(Lg // 128):
                    row0 = res + dil * 128 * n
                    def rows(name, width, c0):
                        a = dr[name]
                        return bass.AP(a.tensor, a.offset + row0 * width + c0, [[dil * width, 128], [1, 256 if width == 768 else 260]])
                    qt = qr.next(); kt = kr.next(); vt = vr.next(); ve = ver.next(); qk = qkr.next()
                    P.dma("sp", qt.t[:], rows("SQ", 768, g * 256), [db["SQ"]], [qt])
                    P.dma("act", kt.t[:], rows("SK", 768, g * 256), [db["SK"]], [kt])
                    P.dma("sp", vt.t[:], rows("SV", 768, g * 256), [db["SV"]], [vt])
                    P.v("pool", "tensor_copy", [vt], [ve], out=ve.t[:, :, 0:64], in_=vt.t[:].rearrange("p (h d) -> p h d", d=64))
                    pt = psr.next()
                    pv = pt.t[:].bitcast(BF16)
                    for i, src in enumerate((qt, qt, kt, kt)):
                        P.tr(pv[:, i * 128:(i + 1) * 128], src.t[:, (i % 2) * 128:(i % 2 + 1) * 128], identb.t[:], [src, identb], [pt],
                             inc=(i == 3))
                    P.act(qk.t[:], pv[:, 0:512].rearrange("p (a n) -> p a n", a=4), AF.Copy, [pt], [qk])
                    pms = []
                    for h in range(4):
                        p = h // 2; fo = slice(64 * (h % 2), 64 * (h % 2) + 64)
                        sc = psr.next()
                        P.mm(sc.t[:, 0:128], prev_qk.t[fo, 2 + p, :], qk.t[fo, p, :], [prev_qk, qk], [sc])
                        P.mm(sc.t[:, 128:256], qk.t[fo, 2 + p, :], qk.t[fo, p, :], [qk], [sc])
                        pe = per.next(); pm = pmr.next()
                        P.act(pe.t[:], sc.t[:, 0:256], AF.Exp, [sc], [pe], scale=0.125)
                        P.v("dve" if h % 2 == 0 else "pool", "tensor_tensor", [pe, swam], [pm], out=pm.t[:], in0=pe.t[:],
                            in1=swam.t[:, 1 if n == 0 else 0, :], op=ALU.mult)
                        pms.append(pm)
                    nu = psr.next()
                    for h in range(4):
                        P.mm(nu.t[:, h * 65:(h + 1) * 65], pms[h].t[:, 0:128], prev_ve.t[:, h, :], [pms[h], prev_ve], [nu],
                             start=True, stop=False)
                        P.mm(nu.t[:, h * 65:(h + 1) * 65], pms[h].t[:, 128:256], ve.t[:, h, :], [pms[h], ve], [nu],
                             start=False, stop=True)
                    no = nor.next()
                    P.act(no.t[:], nu.t[:, 0:260], AF.Copy, [nu], [no])
                    a = dr["NUM"]
                    dst = bass.AP(a.tensor, a.offset + (g * S + row0) * 260, [[dil * 260, 128], [1, 260]])
                    P.dma("sp", dst, no.t[:], [no], [db["NUM"]])
                    prev_qk = qk; prev_ve = ve
                    yield


def phase_O(ctx, l, xsrc, xbuf):
    P = ctx["P"]; nc = P.nc; fw = P.fw; W = ctx["W"]; db = P.dbuf; dr = P.dram
    identb = ctx["identb"]; psr = ctx["psr"]; y = ctx["y"]
    with ExitStack() as s:
        WA = P.sb(s, "WA", [128, 4, D], BF16); WB = P.sb(s, "WB", [128, 2, D], BF16)
        WC = P.sb(s, "WC", [128, 4, D], BF16); WM = P.sb(s, "WM", [128, 8, D], BF16)
        for wt, nm in ((WA, "w_branch_a"), (WB, "w_branch_b"), (WC, "w_branch_c"), (WM, "w_mix_out")):
            P.dma("pool", wt.t[:], W[nm][l].rearrange("(k p) n -> p k n", p=128), [db[nm]], [wt])
        gar = P.sbring(s, "ga", [128, 512], BF16, 2); gcr = P.sbring(s, "gc", [128, 512], BF16, 2)
        nmr = P.sbring(s, "nm", [128, 3, 260], F32, 2)
        gtr = P.sbring(s, "gt", [128, 3 * D], BF16, 2)
        xr = P.sbring(s, "xo", [128, D], F32, 2)
        rdr = P.sbring(s, "rden", [128, 4], F32, 2)
        obr = P.sbring(s, "obb", [128, 256], BF16, 2)
        aTr = P.sbring(s, "aT", [128, 8, 128], BF16, 2); bTr = P.sbring(s, "bT", [128, 2, 128], BF16, 2)
        t1r = P.sbring(s, "t1", [128, 512], F32, 2); t2r = P.sbring(s, "t2", [128, 512], F32, 2)
        ybr = P.sbring(s, "yb", [128, D], BF16, 2); yTr = P.sbring(s, "yT", [128, 8, 128], BF16, 2)
        for t in range(NT):
            sl = slice(t * 128, (t + 1) * 128)
            ga = gar.next(); gc = gcr.next(); nm = nmr.next(); gt = gtr.next(); xt = xr.next()
            P.dma("sp", ga.t[:], dr["GA"][sl, :], [db["GA"]], [ga])
            P.dma("act", gc.t[:], dr["GC"][sl, :], [db["GC"]], [gc])
            P.dma("sp", nm.t[:], dr["NUM"][:, sl, :].rearrange("g p n -> p g n"), [db["NUM"]], [nm])
            P.dma("act", gt.t[:], dr["GT"][sl, :], [db["GT"]], [gt])
            P.dma("sp", xt.t[:], xsrc[sl, :], [xbuf], [xt])
            P.v("dve", "tensor_tensor", [nm], [nm], out=nm.t[:, 0, :], in0=nm.t[:, 0, :], in1=nm.t[:, 1, :], op=ALU.add)
            P.v("dve", "tensor_tensor", [nm], [nm], out=nm.t[:, 0, :], in0=nm.t[:, 0, :], in1=nm.t[:, 2, :], op=ALU.add)
            rd = rdr.next(); ob = obr.next()
            n3 = nm.t[:, 0, :].rearrange("p (h d) -> p h d", d=65)
            P.v("dve", "reciprocal", [nm], [rd], out=rd.t[:].unsqueeze(2), in_=n3[:, :, 64:65])
            P.v("dve", "tensor_tensor", [nm, rd], [ob], out=ob.t[:].rearrange("p (h d) -> p h d", d=64), in0=n3[:, :, 0:64],
                in1=rd.t[:].unsqueeze(2).to_broadcast([128, 4, 64]), op=ALU.mult)
            aT = aTr.next(); bT = bTr.next()
            pa_ = psr.next(); pva = pa_.t[:].bitcast(BF16)
            for k in range(8):
                src = ga if k < 4 else gc
                P.tr(pva[:, k * 128:(k + 1) * 128], src.t[:, (k % 4) * 128:(k % 4 + 1) * 128], identb.t[:], [src, identb], [pa_],
                     inc=(k == 7))
            P.act(aT.t[:], pva.rearrange("p (k n) -> p k n", k=8), AF.Copy, [pa_], [aT])
            pb_ = psr.next(); pvb = pb_.t[:].bitcast(BF16)
            for k in range(2):
                P.tr(pvb[:, k * 128:(k + 1) * 128], ob.t[:, k * 128:(k + 1) * 128], identb.t[:], [ob, identb], [pb_], inc=(k == 1))
            P.v("dve", "tensor_copy", [pb_], [bT], out=bT.t[:], in_=pvb[:, 0:256].rearrange("p (k n) -> p k n", k=2))
            yb = ybr.next()
            for half in range(2):
                cs = slice(half * 512, (half + 1) * 512)
                pA = psr.next(); pB = psr.next(); pC = psr.next()
                for k in range(4):
                    P.mm(pA.t[:], aT.t[:, k, :], WA.t[:, k, cs], [aT, WA], [pA], start=(k == 0), stop=(k == 3))
                for k in range(2):
                    P.mm(pB.t[:], bT.t[:, k, :], WB.t[:, k, cs], [bT, WB], [pB], start=(k == 0), stop=(k == 1))
                for k in range(4):
                    P.mm(pC.t[:], aT.t[:, 4 + k, :], WC.t[:, k, cs], [aT, WC], [pC], start=(k == 0), stop=(k == 3))
                t1 = t1r.next(); t2 = t2r.next()
                P.v("dve", "tensor_tensor", [pA, gt], [t1], out=t1.t[:], in0=pA.t[:], in1=gt.t[:, half * 512:(half + 1) * 512],
                    op=ALU.mult)
                P.v("dve", "tensor_tensor", [pB, gt], [t2], out=t2.t[:], in0=pB.t[:], in1=gt.t[:, D + half * 512:D + (half + 1) * 512],
                    op=ALU.mult)
                P.v("pool", "tensor_tensor", [t1, t2], [t1], out=t1.t[:], in0=t1.t[:], in1=t2.t[:], op=ALU.add)
                P.v("dve", "tensor_tensor", [pC, gt], [t2], out=t2.t[:], in0=pC.t[:],
                    in1=gt.t[:, 2 * D + half * 512:2 * D + (half + 1) * 512], op=ALU.mult)
                P.v("pool", "tensor_tensor", [t1, t2], [yb], out=yb.t[:, cs], in0=t1.t[:], in1=t2.t[:], op=ALU.add)
            yT = yTr.next()
            py = psr.next(); pvy = py.t[:].bitcast(BF16)
            for k in range(8):
                P.tr(pvy[:, k * 128:(k + 1) * 128], yb.t[:, k * 128:(k + 1) * 128], identb.t[:], [yb, identb], [py], inc=(k == 7))
            P.act(yT.t[:], pvy.rearrange("p (k n) -> p k n", k=8), AF.Copy, [py], [yT])
            for half in range(2):
                cs = slice(half * 512, (half + 1) * 512)
                po = psr.next()
                for k in range(8):
                    P.mm(po.t[:], yT.t[:, k, :], WM.t[:, k, cs], [yT, WM], [po], start=(k == 0), stop=(k == 7))
                P.v("dve", "tensor_tensor", [po, xt], [xt], out=xt.t[:, cs], in0=po.t[:], in1=xt.t[:, cs], op=ALU.add)
            P.dma("sp", y[sl, :], xt.t[:], [xt], [db["y"]])
        fw.barrier()


def head_rms(P, rings, src_ps, gain, out_bf):
    jk = rings["jk"].next(); ss = rings["ss"].next()
    P.act(jk.t[:], src_ps.t[:], AF.Square, [src_ps], [jk])
    P.v("dve", "tensor_reduce", [jk], [ss], out=ss.t[:], in_=jk.t[:].rearrange("p (h d) -> p h d", d=128), axis=AX.X,
        op=ALU.add)
    P.rsqrt(ss.t[:], ss.t[:], 128 * EPS, [ss], [ss])
    j3 = jk.t[:].rearrange("p (h d) -> p h d", d=128)
    P.v("dve", "tensor_tensor", [src_ps, ss], [jk], out=j3, in0=src_ps.t[:].rearrange("p (h d) -> p h d", d=128),
        in1=ss.t[:].unsqueeze(2).to_broadcast([128, 4, 128]), op=ALU.mult)
    P.v("pool", "tensor_tensor", [jk, gain], [out_bf], out=out_bf.t[:].rearrange("p (h d) -> p h d", d=128), in0=j3,
        in1=gain.t[:].unsqueeze(1).to_broadcast([128, 4, 128]), op=ALU.mult)


def phase_X(ctx, l):
    P = ctx["P"]; nc = P.nc; fw = P.fw; W = ctx["W"]; db = P.dbuf; dr = P.dram
    identb = ctx["identb"]; psr = ctx["psr"]; y = ctx["y"]
    with ExitStack() as s:
        hT = P.sb(s, "hTx", [128, 8, S], BF16)
        mT = P.sb(s, "mT", [128, 8, MEM], BF16)
        norm_transpose(ctx, s, y, db["y"], W["norm_cross"][l], db["norm_cross"], hT)
        norm_transpose(ctx, s, ctx["mem_in"], db["mem"], W["norm_mem"][l], db["norm_mem"], mT)
        WQ = P.sb(s, "WQ", [128, 8, 512], BF16); WKV = P.sb(s, "WKV", [128, 8, D], BF16); WO = P.sb(s, "WO", [128, 4, D], BF16)
        for wt, nm in ((WQ, "xa_wq"), (WKV, "xa_wkv"), (WO, "xa_wo")):
            P.dma("pool", wt.t[:], W[nm][l].rearrange("(k p) n -> p k n", p=128), [db[nm]], [wt])
        gq = P.sb(s, "xgq", [128, 128]); gk = P.sb(s, "xgk", [128, 128])
        for g_, nm in ((gq, "xa_q_norm"), (gk, "xa_k_norm")):
            P.dma("sp", g_.t[:], bcast_rows(W[nm][l], 128), [db[nm]], [g_])
            P.v("dve", "tensor_scalar", [g_], [g_], out=g_.t[:], in0=g_.t[:], scalar1=float(np.sqrt(128.0)), scalar2=None,
                op0=ALU.mult)
        rings = dict(jk=P.sbring(s, "jkx", [128, 512], F32, 2), ss=P.sbring(s, "ssx", [128, 4], F32, 2))
        kT = P.sb(s, "kTx", [128, 4, MEM], BF16)
        vext = P.sb(s, "vxx", [128, 2, 4, 129], BF16)
        P.v("pool", "memset", [], [vext], vext.t[:], 1.0)
        khr = P.sbring(s, "khat", [128, 512], BF16, 2)
        for mt in range(2):
            pk = psr.next(); pv_ = psr.next()
            for k in range(8):
                P.mm(pk.t[:], mT.t[:, k, mt * 128:(mt + 1) * 128], WKV.t[:, k, 0:512], [mT, WKV], [pk], start=(k == 0), stop=(k == 7))
            for k in range(8):
                P.mm(pv_.t[:], mT.t[:, k, mt * 128:(mt + 1) * 128], WKV.t[:, k, 512:1024], [mT, WKV], [pv_], start=(k == 0),
                     stop=(k == 7))
            kh = khr.next()
            head_rms(P, rings, pk, gk, kh)
            P.act(vext.t[:, mt, :, 0:128], pv_.t[:].rearrange("p (h d) -> p h d", d=128), AF.Copy, [pv_], [vext])
            pt = psr.next(); pvt = pt.t[:].bitcast(BF16)
            for h in range(4):
                P.tr(pvt[:, h * 128:(h + 1) * 128], kh.t[:, h * 128:(h + 1) * 128], identb.t[:], [kh, identb], [pt], inc=(h == 3))
            P.act(kT.t[:, :, mt * 128:(mt + 1) * 128], pvt[:, 0:512].rearrange("p (h n) -> p h n", h=4), AF.Copy, [pt], [kT])
        qhr = P.sbring(s, "qhat", [128, 512], BF16, 2)
        qTr = P.sbring(s, "qTx", [128, 4, 128], BF16, 2)
        per = P.sbring(s, "pex", [128, 256], BF16, 4)
        nsr = P.sbring(s, "nsx", [128, 4, 129], F32, 2)
        rdr = P.sbring(s, "rdx", [128, 4], F32, 2)
        obr = P.sbring(s, "obx", [128, 512], BF16, 2)
        oTr = P.sbring(s, "oTx", [128, 4, 128], BF16, 2)
        xr = P.sbring(s, "xx", [128, D], F32, 2)
        for t in range(NT):
            sl = slice(t * 128, (t + 1) * 128)
            xt = xr.next()
            P.dma("sp", xt.t[:], y[sl, :], [db["y"]], [xt])
            pq = psr.next()
            for k in range(8):
                P.mm(pq.t[:], hT.t[:, k, sl], WQ.t[:, k, :], [hT, WQ], [pq], start=(k == 0), stop=(k == 7))
            qh = qhr.next(); qT = qTr.next()
            head_rms(P, rings, pq, gq, qh)
            pt = psr.next(); pvt = pt.t[:].bitcast(BF16)
            for h in range(4):
                P.tr(pvt[:, h * 128:(h + 1) * 128], qh.t[:, h * 128:(h + 1) * 128], identb.t[:], [qh, identb], [pt], inc=(h == 3))
            P.act(qT.t[:], pvt[:, 0:512].rearrange("p (h n) -> p h n", h=4), AF.Copy, [pt], [qT])
            pes = []
            for h in range(4):
                sc = psr.next()
                for mt in range(2):
                    P.mm(sc.t[:, mt * 128:(mt + 1) * 128], kT.t[:, h, mt * 128:(mt + 1) * 128], qT.t[:, h, :], [kT, qT], [sc])
                pe = per.next()
                P.act(pe.t[:], sc.t[:, 0:256], AF.Exp, [sc], [pe], scale=float(128.0 ** -0.5))
                pes.append(pe)
            ns = nsr.next()
            for hp in range(2):
                nu = psr.next()
                for hh in range(2):
                    h = hp * 2 + hh
                    for mt in range(2):
                        P.mm(nu.t[:, hh * 129:(hh + 1) * 129], pes[h].t[:, mt * 128:(mt + 1) * 128], vext.t[:, mt, h, :],
                             [pes[h], vext], [nu], start=(mt == 0), stop=(mt == 1))
                P.act(ns.t[:, hp * 2:hp * 2 + 2, :], nu.t[:, 0:258].rearrange("p (h d) -> p h d", d=129), AF.Copy, [nu], [ns])
            rd = rdr.next(); ob = obr.next(); oT = oTr.next()
            P.v("dve", "reciprocal", [ns], [rd], out=rd.t[:].unsqueeze(2), in_=ns.t[:, :, 128:129])
            P.v("dve", "tensor_tensor", [ns, rd], [ob], out=ob.t[:].rearrange("p (h d) -> p h d", d=128), in0=ns.t[:, :, 0:128],
                in1=rd.t[:].unsqueeze(2).to_broadcast([128, 4, 128]), op=ALU.mult)
            pt2 = psr.next(); pvt2 = pt2.t[:].bitcast(BF16)
            for h in range(4):
                P.tr(pvt2[:, h * 128:(h + 1) * 128], ob.t[:, h * 128:(h + 1) * 128], identb.t[:], [ob, identb], [pt2], inc=(h == 3))
            P.act(oT.t[:], pvt2[:, 0:512].rearrange("p (h n) -> p h n", h=4), AF.Copy, [pt2], [oT])
            for half in range(2):
                cs = slice(half * 512, (half + 1) * 512)
                po = psr.next()
                for k in range(4):
                    P.mm(po.t[:], oT.t[:, k, :], WO.t[:, k, cs], [oT, WO], [po], start=(k == 0), stop=(k == 3))
                P.v("dve", "tensor_tensor", [po, xt], [xt], out=xt.t[:, cs], in0=po.t[:], in1=xt.t[:, cs], op=ALU.add)
            P.dma("sp", y[sl, :], xt.t[:], [xt], [db["y"]])
        fw.barrier()


def phase_M(ctx, l):
    P = ctx["P"]; nc = P.nc; fw = P.fw; W = ctx["W"]; db = P.dbuf; dr = P.dram
    identb = ctx["identb"]; onesb = ctx["onesb"]; psr = ctx["psr"]; y = ctx["y"]; C = ctx["C"]; cst = ctx["cst"]
    misc = ctx["misc"]
    XBd = dr["XB"]; YBd = dr["YB"]
    if "breg" not in ctx:
        ctx["breg"] = nc.gpsimd.to_reg(XB_ROWS - 1)
    breg = ctx["breg"]
    with ExitStack() as s:
        DST = P.sb(s, "DST", [128, NT, 4], I32)
        GTE = P.sb(s, "GTE", [128, NT, 4])
        with ExitStack() as s1:
            g32 = P.sb(s1, "g32m", [128, D])
            P.dma("sp", g32.t[:], bcast_rows(W["norm_ffn"][l], D), [db["norm_ffn"]], [g32])
            P.v("dve", "tensor_scalar", [g32], [g32], out=g32.t[:], in0=g32.t[:], scalar1=float(np.sqrt(D)), scalar2=None,
                op0=ALU.mult)
            RW = P.sb(s1, "RW", [128, 8, NE])
            P.dma("sp", RW.t[:], W["router_w"][l].rearrange("(k p) e -> p k e", p=128), [db["router_w"]], [RW])
            rb = P.sb(s1, "rb", [1, NE])
            P.dma("sp", rb.t[:], W["router_b"][l:l + 1, :], [db["router_b"]], [rb])
            ltb = P.sb(s1, "ltb", [128, 128], BF16)
            P.dma("pool", ltb.t[:], P.dram["carr"][:, CNAMES.index("lt"), :], [db["carr"]], [ltb])
            carry = P.sb(s1, "carry", [128, NE])
            P.v("dve", "memset", [], [carry], carry.t[:], 0.0)
            xr = P.sbring(s1, "xm", [128, D], F32, 2); jr = P.sbring(s1, "jm", [128, D], F32, 2)
            hfr = P.sbring(s1, "hfm", [128, D], F32, 2); hbr = P.sbring(s1, "hbm", [128, D], BF16, 3)
            sqr = Ring([P.sb(s1, f"ssqm{i}", [128, 1]) for i in range(3)])
            hTr = P.sbring(s1, "hTf", [128, 8, 128], F32, 2)
            lgr = P.sbring(s1, "lg", [128, NE], F32, 2); t8r = P.sbring(s1, "top8", [128, 8], F32, 2)
            smr = P.sbring(s1, "smm", [128, 16], F32, 2)
            mkr = P.sbring(s1, "mk", [128, NE], F32, 2); mbr = P.sbring(s1, "mkb", [128, NE], BF16, 2)
            pfr = P.sbring(s1, "posf", [128, NE], F32, 2); ovr = P.sbring(s1, "ovf", [128, NE], F32, 2)
            ohr = P.sbring(s1, "oh", [128, NE], F32, 2)
            for t in range(NT):
                sl = slice(t * 128, (t + 1) * 128)
                xt = xr.next(); jk = jr.next(); hf = hfr.next(); hb = hbr.next(); ssq = sqr.next()
                P.dma("sp", xt.t[:], y[sl, :], [db["y"]], [xt])
                P.act(jk.t[:], xt.t[:], AF.Square, [xt], [jk, ssq], accum_out=ssq.t[:])
                P.rsqrt(ssq.t[:], ssq.t[:], D * EPS, [ssq], [ssq])
                P.act(jk.t[:], xt.t[:], AF.Copy, [xt, ssq], [jk], scale=ssq.t[:, 0:1])
                P.v("dve", "tensor_tensor", [jk, g32], [hf], out=hf.t[:], in0=jk.t[:], in1=g32.t[:], op=ALU.mult)
                P.v("pool", "tensor_copy", [hf], [hb], out=hb.t[:], in_=hf.t[:])
                hTf = hTr.next()
                for half in range(2):
                    ps2 = psr.next()
                    for k in range(4):
                        kk = half * 4 + k
                        P.tr(ps2.t[:, k * 128:(k + 1) * 128], hf.t[:, kk * 128:(kk + 1) * 128], C["ident"], [hf, cst], [ps2],
                             inc=(k == 3))
                    P.act(hTf.t[:, half * 4:(half + 1) * 4, :], ps2.t[:].rearrange("p (k n) -> p k n", k=4), AF.Copy, [ps2], [hTf])
                pl = psr.next()
                for k in range(8):
                    P.mm(pl.t[:, 0:NE], hTf.t[:, k, :], RW.t[:, k, :], [hTf, RW], [pl], start=(k == 0), stop=False)
                P.mm(pl.t[:, 0:NE], C["ones"][0:1, :], rb.t[0:1, :], [cst, rb], [pl], start=False, stop=True)
                lg = lgr.next(); t8 = t8r.next(); sm = smr.next(); mk = mkr.next(); mkb = mbr.next()
                P.act(lg.t[:], pl.t[:, 0:NE], AF.Copy, [pl], [lg])
                P.v("dve", "max", [lg], [t8], out=t8.t[:], in_=lg.t[:])
                P.v("dve", "tensor_scalar", [t8], [sm], out=sm.t[:, 0:1], in0=t8.t[:, 0:1], scalar1=-1.0, scalar2=None, op0=ALU.mult)
                P.act(sm.t[:, 4:8], t8.t[:, 0:4], AF.Exp, [t8, sm], [sm], bias=sm.t[:, 0:1], accum_out=sm.t[:, 1:2])
                P.v("dve", "reciprocal", [sm], [sm], out=sm.t[:, 2:3], in_=sm.t[:, 1:2])
                P.v("dve", "tensor_scalar", [lg, t8], [mk], out=mk.t[:], in0=lg.t[:], scalar1=t8.t[:, 3:4], scalar2=None, op0=ALU.is_ge)
                P.v("pool", "tensor_copy", [mk], [mkb], out=mkb.t[:], in_=mk.t[:])
                pp = psr.next()
                P.mm(pp.t[:, 0:NE], ltb.t[:], mkb.t[:], [ltb, mkb], [pp])
                P.mm(pp.t[:, NE:2 * NE], onesb.t[:], mkb.t[:], [onesb, mkb], [pp])
                posf = pfr.next(); ovf = ovr.next()
                P.v("dve", "tensor_tensor", [pp, carry], [posf], out=posf.t[:], in0=pp.t[:, 0:NE], in1=carry.t[:], op=ALU.add)
                P.v("dve", "tensor_tensor", [pp, carry], [carry], out=carry.t[:], in0=pp.t[:, NE:2 * NE], in1=carry.t[:], op=ALU.add)
                P.v("dve", "tensor_scalar", [posf], [ovf], out=ovf.t[:], in0=posf.t[:], scalar1=float(CAP), scalar2=1e7,
                    op0=ALU.is_ge, op1=ALU.mult)
                P.v("dve", "tensor_tensor", [posf, misc], [posf], out=posf.t[:], in0=posf.t[:], in1=misc.t[:, 32:64], op=ALU.add)
                P.v("dve", "tensor_tensor", [posf, ovf], [posf], out=posf.t[:], in0=posf.t[:], in1=ovf.t[:], op=ALU.add)
                for j in range(4):
                    oh = ohr.next()
                    P.v("dve", "tensor_scalar", [lg, t8], [oh], out=oh.t[:], in0=lg.t[:], scalar1=t8.t[:, j:j + 1], scalar2=None,
                        op0=ALU.is_equal)
                    P.v("dve", "tensor_tensor", [oh, posf], [oh], out=oh.t[:], in0=oh.t[:], in1=posf.t[:], op=ALU.mult)
                    P.v("dve", "tensor_reduce", [oh], [sm], out=sm.t[:, 8 + j:9 + j], in_=oh.t[:], axis=AX.X, op=ALU.add)
                P.v("dve", "tensor_scalar", [sm], [sm], out=sm.t[:, 12:16], in0=sm.t[:, 8:12], scalar1=float(XB_ROWS), scalar2=None,
                    op0=ALU.is_lt)
                P.v("dve", "scalar_tensor_tensor", [sm], [GTE], out=GTE.t[:, t, :], in0=sm.t[:, 4:8], scalar=sm.t[:, 2:3],
                    in1=sm.t[:, 12:16], op0=ALU.mult, op1=ALU.mult)
                P.v("dve", "tensor_copy", [sm], [DST], out=DST.t[:, t, :], in_=sm.t[:, 8:12])
                for j in range(4):
                    fw.idma(XBd, bass.IndirectOffsetOnAxis(ap=DST.t[:, t, j:j + 1], axis=0), hb.t[:, :], None,
                            reads=[hb.b, DST.b], writes=[db["XB"]], bounds_check=breg, oob_is_err=False)
            fw.barrier()
        with ExitStack() as s2:
            bins = P.sb(s2, "bins", [128, NE, 16])
            P.dma("sp", bins.t[:], W["moe_b_in"][l], [db["moe_b_in"]], [bins])
            WIr = P.sbring(s2, "WI", [128, 8, 2 * D], BF16, 2)
            WOr = P.sbring(s2, "WO2", [128, 8, D], BF16, 2)
            bor = P.sbring(s2, "bo", [1, D], BF16, 2)
            xbr = P.sbring(s2, "xbt", [128, D], BF16, 3)
            xTr = P.sbring(s2, "xTe", [128, 8, CAP], BF16, 2)
            aTr = P.sbring(s2, "actT", [128, 8, CAP], BF16, 2)
            gr = P.sbring(s2, "eg", [128, CAP], F32, 2); sgr = P.sbring(s2, "esg", [128, CAP], F32, 2)
            lr = P.sbring(s2, "el", [128, CAP], F32, 2)
            yor = P.sbring(s2, "yo", [128, D], F32, 2)
            for e in range(NE):
                WI = WIr.next(); WO2 = WOr.next(); bo = bor.next()
                for kq in range(4):
                    P.dma("pool", WI.t[:, 2 * kq:2 * kq + 2, :],
                          W["moe_w_in"][l, e, kq * 256:(kq + 1) * 256, :].rearrange("(k p) n -> p k n", p=128), [db["moe_w_in"]], [WI])
                for kq in range(2):
                    P.dma("pool", WO2.t[:, 4 * kq:4 * kq + 4, :],
                          W["moe_w_out"][l, e, kq * 512:(kq + 1) * 512, :].rearrange("(k p) n -> p k n", p=128), [db["moe_w_out"]], [WO2])
                P.dma("pool", bo.t[:], W["moe_b_out"][l, e:e + 1, :], [db["moe_b_out"]], [bo])
                xT = xTr.next(); aT = aTr.next()
                for ct in range(NCAPT):
                    xb_ = xbr.next()
                    r0 = e * CAP + ct * 128
                    P.dma("sp", xb_.t[:], XBd[r0:r0 + 128, :], [db["XB"]], [xb_])
                    pt = psr.next(); pvt = pt.t[:].bitcast(BF16)
                    for k in range(8):
                        P.tr(pvt[:, k * 128:(k + 1) * 128], xb_.t[:, k * 128:(k + 1) * 128], identb.t[:], [xb_, identb], [pt], inc=(k == 7))
                    if ct % 2 == 0:
                        P.act(xT.t[:, :, ct * 128:(ct + 1) * 128], pvt.rearrange("p (k n) -> p k n", k=8), AF.Copy, [pt], [xT])
                    else:
                        P.v("dve", "tensor_copy", [pt], [xT], out=xT.t[:, :, ct * 128:(ct + 1) * 128],
                            in_=pvt.rearrange("p (k n) -> p k n", k=8))
                for fb in range(8):
                    pg0 = psr.next(); pl0 = psr.next(); prm = psr.next()
                    for (pt_, col, f0, n0, n1) in ((pg0, 0, fb, 0, 512), (pl0, 0, fb + 8, 0, 512), (prm, 0, fb, 512, CAP),
                                                    (prm, 128, fb + 8, 512, CAP)):
                        for k in range(8):
                            P.mm(pt_.t[:, col:col + (n1 - n0)], WI.t[:, k, f0 * 128:(f0 + 1) * 128], xT.t[:, k, n0:n1], [WI, xT], [pt_],
                                 start=(k == 0), stop=(k == 7))
                    g_ = gr.next(); sg = sgr.next(); l_ = lr.next()
                    bg = bins.t[:, e, fb:fb + 1]; bl = bins.t[:, e, fb + 8:fb + 9]
                    for (src, c0, c1, d0) in ((pg0, 0, 512, 0), (prm, 0, CAP - 512, 512)):
                        P.v("dve", "tensor_scalar", [src, bins], [g_], out=g_.t[:, d0:d0 + (c1 - c0)], in0=src.t[:, c0:c1], scalar1=bg,
                            scalar2=7.0, op0=ALU.add, op1=ALU.min)
                    for (src, c0, c1, d0) in ((pl0, 0, 512, 0), (prm, 128, 128 + CAP - 512, 512)):
                        P.v("dve", "tensor_scalar", [src, bins], [l_], out=l_.t[:, d0:d0 + (c1 - c0)], in0=src.t[:, c0:c1], scalar1=bl,
                            scalar2=7.0, op0=ALU.add, op1=ALU.min)
                    P.act(sg.t[:], g_.t[:], AF.Sigmoid, [g_], [sg], scale=1.702)
                    P.v("dve", "tensor_scalar", [l_], [l_], out=l_.t[:], in0=l_.t[:], scalar1=-7.0, scalar2=1.0, op0=ALU.max, op1=ALU.add)
                    P.v("dve", "tensor_tensor", [g_, sg], [g_], out=g_.t[:], in0=g_.t[:], in1=sg.t[:], op=ALU.mult)
                    P.v("dve", "tensor_tensor", [g_, l_], [aT], out=aT.t[:, fb, :], in0=g_.t[:], in1=l_.t[:], op=ALU.mult)
                for ct in range(NCAPT):
                    yo = yor.next()
                    for half in range(2):
                        cs = slice(half * 512, (half + 1) * 512)
                        po = psr.next()
                        for k in range(8):
                            P.mm(po.t[:], aT.t[:, k, ct * 128:(ct + 1) * 128], WO2.t[:, k, cs], [aT, WO2], [po], start=(k == 0), stop=False)
                        P.mm(po.t[:], onesb.t[0:1, :], bo.t[0:1, cs], [onesb, bo], [po], start=False, stop=True)
                        P.act(yo.t[:, cs], po.t[:], AF.Copy, [po], [yo])
                    r0 = e * CAP + ct * 128
                    P.dma("sp", YBd[r0:r0 + 128, :], yo.t[:], [yo], [db["YB"]])
            fw.barrier()
        with ExitStack() as s3:
            xr = P.sbring(s3, "xc", [128, D], F32, 2)
            gr_ = P.sbring(s3, "gth", [128, D], F32, 4)
            for it in gr_.items:
                P.v("dve", "memset", [], [it], it.t[:], 0.0)
            for t in range(NT):
                sl = slice(t * 128, (t + 1) * 128)
                xt = xr.next()
                P.dma("sp", xt.t[:], y[sl, :], [db["y"]], [xt])
                for j in range(4):
                    gt_ = gr_.next()
                    fw.idma(gt_.t[:, :], None, YBd, bass.IndirectOffsetOnAxis(ap=DST.t[:, t, j:j + 1], axis=0),
                            reads=[db["YB"], DST.b], writes=[gt_.b], bounds_check=breg, oob_is_err=False)
                    P.v("dve", "scalar_tensor_tensor", [gt_, GTE, xt], [xt], out=xt.t[:], in0=gt_.t[:], scalar=GTE.t[:, t, j:j + 1],
                        in1=xt.t[:], op0=ALU.mult, op1=ALU.add)
                P.dma("sp", y[sl, :], xt.t[:], [xt], [db["y"]])
            fw.barrier()


def make_in_maps(inputs, n_layers=DEPTH, cores=range(8)):
    f = lambda a: np.ascontiguousarray(np.asarray(a))
    shared = {"carr": CARR, "swamask": SWAMASK, "misc": MISC}
    for k, v in inputs.items():
        if k in ("x", "mem", "positions"):
            continue
        a = np.asarray(v)[:n_layers]
        if k == "gdn_conv":
            shared["gdn_convT"] = f(a.transpose(0, 2, 1))
        elif k == "moe_b_in":
            shared[k] = f(a.reshape(n_layers, NE, 16, 128).transpose(0, 3, 1, 2))
        elif k == "gate_bias":
            shared["gate_bias"] = f(a.reshape(n_layers, 3 * D))
        else:
            shared[k] = f(a)
    maps = []
    for c in cores:
        m = dict(shared)
        m["x"] = f(inputs["x"][c])
        m["mem"] = f(inputs["mem"][c])
        m["pos"] = f(np.asarray(inputs["positions"][c]).astype(np.int32).reshape(NT, 128).T)
        maps.append(m)
    return maps


_CACHE = {}


def kernel(**inputs):
    if "prog" not in _CACHE:
        _CACHE["prog"] = build_program()
    P = _CACHE["prog"]
    maps = make_in_maps(inputs)
    res = run_bass_kernel_spmd(P.nc, maps, core_ids=list(range(8)))
    return np.stack([np.asarray(r["y"]).reshape(S, D) for r in res.results], axis=0).astype(np.float32)
```

```python
import numpy as np
from contextlib import ExitStack
import concourse.bass as bass
import concourse.mybir as mybir
from concourse.bass_utils import run_bass_kernel_spmd

F32 = mybir.dt.float32
BF16 = mybir.dt.bfloat16
I32 = mybir.dt.int32
AF = mybir.ActivationFunctionType
ALU = mybir.AluOpType
AX = mybir.AxisListType

D = 1024
S = 4096
NT = S // 128
DEPTH = 4
N_IN = 8984
MEM = 256
NE = 32
CAP = 640
NCAPT = CAP // 128
XB_ROWS = NE * CAP
EPS = 1e-6


class Buf:
    __slots__ = ("name", "writer", "readers")

    def __init__(self, name=""):
        self.name = name
        self.writer = None
        self.readers = {}


class FW:
    def __init__(self, nc, es, n_dma_sems=10):
        self.nc = nc
        self.es = es
        self.eng = {"pe": nc.tensor, "act": nc.scalar, "dve": nc.vector, "pool": nc.gpsimd, "sp": nc.sync}
        self.sem = {}
        self.cnt = {}
        for k in ("pe", "act", "dve", "pool"):
            self.sem[k] = es.enter_context(nc.semaphore("S_" + k))
            self.cnt[k] = 0
        self.dq = {}
        for q in ("sp", "act", "pool"):
            ring = []
            for i in range(n_dma_sems):
                key = f"d_{q}{i}"
                self.sem[key] = es.enter_context(nc.semaphore("S_" + key))
                self.cnt[key] = 0
                ring.append(key)
            self.dq[q] = [ring, 0]
        self.known = {k: {} for k in ("pe", "act", "dve", "pool", "sp")}
        self.n_inst = 0
        self.n_wait = 0

    def _wait(self, stream, dep):
        key, val = dep
        if stream == "pe" and key == "pe":
            return
        if self.known[stream].get(key, 0) >= val:
            return
        self.eng[stream].wait_ge(self.sem[key], val)
        self.known[stream][key] = val
        self.n_wait += 1

    def _deps(self, stream, reads, writes):
        for b in reads:
            if b.writer is not None:
                self._wait(stream, b.writer)
        for b in writes:
            if b.writer is not None:
                self._wait(stream, b.writer)
            for k, v in b.readers.items():
                self._wait(stream, (k, v))

    def _commit(self, done, reads, writes):
        k, v = done
        for b in writes:
            b.writer = done
            b.readers = {}
        for b in reads:
            if b.readers.get(k, 0) < v:
                b.readers[k] = v

    def op(self, stream, fn, reads=(), writes=(), inc=True):
        self._deps(stream, reads, writes)
        ins = fn()
        self.n_inst += 1
        if inc:
            self.cnt[stream] += 1
            ins.then_inc(self.sem[stream], 1)
            done = (stream, self.cnt[stream])
        else:
            done = (stream, self.cnt[stream] + 1)
        self._commit(done, reads, writes)
        return ins

    def _dma_common(self, q, issue, reads, writes):
        ring, idx = self.dq[q]
        key = ring[idx % len(ring)]
        self.dq[q][1] = idx + 1
        if self.cnt[key] > 0:
            self._wait(q, (key, self.cnt[key]))
        self._deps(q, reads, writes)
        ins = issue()
        self.cnt[key] += 16
        ins.then_inc(self.sem[key], 16)
        self.n_inst += 1
        done = (key, self.cnt[key])
        self._commit(done, reads, writes)
        return done

    def dma(self, q, out, in_, reads=(), writes=(), **kw):
        return self._dma_common(q, lambda: self.eng[q].dma_start(out=out, in_=in_, **kw), reads, writes)

    def idma(self, out, out_off, in_, in_off, reads=(), writes=(), **kw):
        return self._dma_common(
            "pool", lambda: self.nc.gpsimd.indirect_dma_start(out, out_off, in_, in_off, **kw), reads, writes)

    def barrier(self):
        for stream in ("pe", "act", "dve", "pool", "sp"):
            for key, c in self.cnt.items():
                if c > 0:
                    self._wait(stream, (key, c))


class T:
    __slots__ = ("t", "b", "ps", "tb")

    def __init__(self, t, name="", b=None, ps=False, tb=None):
        self.t = t
        self.b = b if b is not None else Buf(name)
        self.ps = ps
        self.tb = tb


class Ring:
    def __init__(self, items):
        self.items = items
        self.i = 0

    def next(self):
        it = self.items[self.i % len(self.items)]
        self.i += 1
        return it


def _consts():
    i = np.arange(128)
    same = (i[:, None] // 64) == (i[None, :] // 64)
    c = {}
    c["ident"] = np.eye(128, dtype=np.float32)
    c["ones"] = np.ones((128, 128), np.float32)
    c["mbt"] = (same & (i[:, None] <= i[None, :])).astype(np.float32)
    c["mrev"] = (same & (i[:, None] > i[None, :])).astype(np.float32)
    c["sel0"] = np.zeros((128, 128), np.float32); c["sel0"][:64, :] = 1.0
    c["sel1"] = np.zeros((128, 128), np.float32); c["sel1"][64:, :] = 1.0
    c["selc"] = same.astype(np.float32)
    c["bigls"] = np.where(same & (i[None, :] < i[:, None]), 0.0, 1e4).astype(np.float32)
    c["negu"] = np.where(same & (i[:, None] <= i[None, :]), 0.0, -1e4).astype(np.float32)
    c["lt"] = (i[:, None] < i[None, :]).astype(np.float32)
    names = list(c.keys())
    arr = np.stack([c[n] for n in names], axis=1)
    m = np.zeros((128, 2, 256), np.float32)
    m[:, 0, :128] = (i[:, None] >= i[None, :]); m[:, 0, 128:] = (i[:, None] <= i[None, :])
    m[:, 1, 128:] = (i[:, None] <= i[None, :])
    misc = np.zeros((128, 128), np.float32)
    misc[:, 0:32] = (10000.0 ** (-np.arange(0, 64, 2, dtype=np.float32) / 64))[None, :]
    misc[:, 32:64] = (np.arange(32, dtype=np.float32) * CAP)[None, :]
    misc[:, 64] = (i // 64 == 0); misc[:, 65] = (i // 64 == 1)
    return names, np.ascontiguousarray(arr), m, misc


CNAMES, CARR, SWAMASK, MISC = _consts()
NCONST = len(CNAMES)

OFF = {}
_o = 0
for _n, _w in (("a_q", 512), ("a_k", 512), ("a_v", 512), ("a_z", 512), ("a_ab", 8), ("b_q", 768), ("b_k", 768),
               ("b_v", 768), ("c_q", 256), ("c_k", 256), ("c_v", 512), ("c_r", 512), ("c_low", 16), ("gates", 3072)):
    OFF[_n] = _o
    _o += _w
assert _o == N_IN


class Prog:
    def __init__(self, n_layers=DEPTH, debug=(), stop_after=None):
        self.L = n_layers
        self.debug = set(debug)
        self.stop_after = stop_after
        self.nc = nc = bass.Bass("TRN2", target_bir_lowering=False)
        self.es = ExitStack()
        self.fw = FW(nc, self.es)
        self.dram = {}
        self.dbuf = {}
        self.uid = 0
        self._cc = {}

    def din(self, name, shape, dt=F32):
        self.dram[name] = self.nc.dram_tensor(name, list(shape), dt, kind="ExternalInput").ap()
        self.dbuf[name] = Buf(name)
        return self.dram[name]

    def dscratch(self, name, shape, dt=F32):
        kind = "ExternalOutput" if name in self.debug else "Internal"
        self.dram[name] = self.nc.dram_tensor(name, list(shape), dt, kind=kind).ap()
        self.dbuf[name] = Buf(name)
        return self.dram[name]

    def sb(self, es, name, shape, dt=F32):
        self.uid += 1
        return T(es.enter_context(self.nc.sbuf_tensor(f"{name}_{self.uid}", list(shape), dt)), name)

    def sbring(self, es, name, shape, dt, n):
        return Ring([self.sb(es, f"{name}{i}", shape, dt) for i in range(n)])

    @staticmethod
    def _b(xs):
        return [x.b if isinstance(x, T) else x for x in xs]

    def mm(self, out, lhsT, rhs, r, w, start=True, stop=True):
        nc = self.nc
        return self.fw.op("pe", lambda: nc.tensor.matmul(out, lhsT=lhsT, rhs=rhs, start=start, stop=stop),
                          self._b(r), self._b(w), inc=stop)

    def tr(self, out, in_, ident, r, w, inc=True):
        nc = self.nc
        return self.fw.op("pe", lambda: nc.tensor.transpose(out=out, in_=in_, identity=ident),
                          self._b(r), self._b(w), inc=inc)

    @staticmethod
    def _psw(r, w):
        return list(w) + [x for x in r if isinstance(x, T) and x.ps]

    def act(self, out, in_, func, r, w, **kw):
        nc = self.nc
        return self.fw.op("act", lambda: nc.scalar.activation(out=out, in_=in_, func=func, **kw),
                          self._b(r), self._b(self._psw(r, w)))

    def v(self, eng, method, r, w, *a, **kw):
        e = self.nc.vector if eng == "dve" else self.nc.gpsimd
        return self.fw.op(eng, lambda: getattr(e, method)(*a, **kw), self._b(r), self._b(self._psw(r, w)))

    def dma(self, q, out, in_, r, w, **kw):
        return self.fw.dma(q, out, in_, self._b(r), self._b(w), **kw)

    def rsqrt(self, out, in_, addc, r, w):
        self.act(out, in_, AF.Sqrt, r, w, bias=self.constcol(addc))
        self.v("dve", "reciprocal", w, w, out=out, in_=out)

    def constcol(self, val):
        key = float(val)
        if key not in self._cc:
            t = self.sb(self.es, "cc", [128, 1])
            self.v("pool", "memset", [], [t], t.t[:], key)
            self._cc[key] = t
        return self._cc[key].t[:, 0:1]


def run_window(gen_iter, window):
    active = []

    def step_all():
        for a in list(active):
            try:
                next(a)
            except StopIteration:
                active.remove(a)
    for g in gen_iter:
        if g is None:
            continue
        active.append(g)
        while len(active) >= window:
            step_all()
    while active:
        step_all()


def bcast_rows(ap, n, parts=128):
    return bass.AP(ap.tensor, ap.offset, [[0, parts], [1, n]])


def build_program(n_layers=DEPTH, debug=(), stop_after=None, moe_decl=NE):
    P = Prog(n_layers, debug, stop_after)
    nc, fw = P.nc, P.fw
    L = n_layers
    x_in = P.din("x", [S, D])
    mem_in = P.din("mem", [MEM, D])
    pos_in = P.din("pos", [128, NT], I32)
    carr_in = P.din("carr", [128, NCONST, 128])
    swam_in = P.din("swamask", [128, 2, 256])
    misc_in = P.din("misc", [128, 128])
    W = {}
    for name, shape in (
        ("norm_mix", [L, D]), ("w_in", [L, D, N_IN]), ("gate_bias", [L, 3 * D]), ("gdn_convT", [L, 1536, 4]),
        ("gdn_a_log", [L, 4]), ("gdn_dt_bias", [L, 4]), ("gdn_norm", [L, 128]), ("swa_q_norm", [L, 64]),
        ("swa_k_norm", [L, 64]), ("gla_gate_up", [L, 16, 256]), ("gla_gate_bias", [L, 256]),
        ("gla_norm", [L, 128]), ("w_branch_a", [L, 512, D]), ("w_branch_b", [L, 256, D]),
        ("w_branch_c", [L, 512, D]), ("w_mix_out", [L, D, D]), ("norm_cross", [L, D]), ("norm_mem", [L, D]),
        ("xa_wq", [L, D, 512]), ("xa_wkv", [L, D, 1024]), ("xa_q_norm", [L, 128]), ("xa_k_norm", [L, 128]),
        ("xa_wo", [L, 512, D]), ("norm_ffn", [L, D]), ("router_w", [L, D, NE]), ("router_b", [L, NE]),
        ("moe_w_in", [L, moe_decl, D, 2 * D]), ("moe_b_in", [L, 128, NE, 16]), ("moe_w_out", [L, moe_decl, D, D]),
        ("moe_b_out", [L, NE, D]),
    ):
        W[name] = P.din(name, shape)
    y = P.nc.dram_tensor("y", [S, D], F32, kind="ExternalOutput").ap()
    P.dram["y"] = y
    P.dbuf["y"] = Buf("y")
    GQKV = P.dscratch("GQKV", [12, 128, S], BF16)
    ZS = P.dscratch("ZS", [S, 512], BF16)
    SQ = P.dscratch("SQ", [S, 768], BF16)
    SK = P.dscratch("SK", [S, 768], BF16)
    SV = P.dscratch("SV", [S, 768], BF16)
    CQ = P.dscratch("CQ", [S, 256])
    CK = P.dscratch("CK", [S, 256])
    CV = P.dscratch("CV", [S, 512], BF16)
    RS = P.dscratch("RS", [S, 512], BF16)
    LA = P.dscratch("LA", [S, 256])
    GT = P.dscratch("GT", [S, 3 * D], BF16)
    GA = P.dscratch("GA", [S, 512], BF16)
    GC = P.dscratch("GC", [S, 512], BF16)
    NUM = P.dscratch("NUM", [3, S, 260])
    H3 = P.dscratch("H3", [S, D], BF16)
    XB = P.dscratch("XB", [XB_ROWS, D], BF16)
    YB = P.dscratch("YB", [XB_ROWS, D])
    if "HT" in P.debug:
        P.dscratch("HT", [128, 8, S], BF16)
    db = P.dbuf

    es = P.es
    cst = P.sb(es, "cst", [128, NCONST, 128])
    identb = P.sb(es, "identb", [128, 128], BF16)
    onesb = P.sb(es, "onesb", [128, 128], BF16)
    swam = P.sb(es, "swam", [128, 2, 256], BF16)
    misc = P.sb(es, "misc", [128, 128])
    cosT = P.sb(es, "cosT", [128, NT, 32])
    sinT = P.sb(es, "sinT", [128, NT, 32])
    gbs = P.sb(es, "gbs", [128, NT, 8])
    C = {n: cst.t[:, i, :] for i, n in enumerate(CNAMES)}
    ident = C["ident"]
    PS = [T(es.enter_context(nc.psum_tensor(f"ps{i}", [128, 512], F32)), f"ps{i}", ps=True) for i in range(8)]
    psr = Ring(PS)

    for cv in (1.0, D * EPS, 64 * EPS, 128 * EPS, 1e-6):
        P.constcol(cv)
    P.dma("sp", cst.t[:], carr_in, [db["carr"]], [cst])
    P.dma("sp", misc.t[:], misc_in, [db["misc"]], [misc])
    P.dma("pool", swam.t[:], swam_in, [db["swamask"]], [swam])
    P.dma("pool", identb.t[:], carr_in[:, CNAMES.index("ident"), :], [db["carr"]], [identb])
    P.dma("pool", onesb.t[:], carr_in[:, CNAMES.index("ones"), :], [db["carr"]], [onesb])

    with ExitStack() as s0:
        posi = P.sb(s0, "posi", [128, NT], I32)
        posf = P.sb(s0, "posf", [128, NT])
        ang = P.sb(s0, "ang", [128, NT, 32])
        red = P.sb(s0, "red", [128, NT, 32])
        P.dma("sp", posi.t[:], pos_in, [db["pos"]], [posi])
        P.v("dve", "tensor_copy", [posi], [posf], out=posf.t[:], in_=posi.t[:])
        TWO_PI = 2.0 * np.pi
        for t in range(NT):
            P.v("dve", "tensor_scalar", [posf, misc], [ang], out=ang.t[:, t, :], in0=misc.t[:, 0:32],
                scalar1=posf.t[:, t:t + 1], scalar2=None, op0=ALU.mult)
        ki = P.sb(s0, "ki", [128, NT, 32], I32)
        kf = P.sb(s0, "kf", [128, NT, 32])
        for dst, shift in ((sinT, 0.0), (cosT, 0.5 * np.pi)):
            P.v("dve", "tensor_scalar", [ang], [red], out=red.t[:], in0=ang.t[:], scalar1=float(shift),
                scalar2=float(1.0 / TWO_PI), op0=ALU.add, op1=ALU.mult)
            P.v("dve", "tensor_copy", [red], [ki], out=ki.t[:], in_=red.t[:])
            P.v("dve", "tensor_copy", [ki], [kf], out=kf.t[:], in_=ki.t[:])
            P.v("dve", "tensor_scalar", [ang], [red], out=red.t[:], in0=ang.t[:], scalar1=float(shift),
                scalar2=None, op0=ALU.add)
            P.v("dve", "scalar_tensor_tensor", [kf, red], [red], out=red.t[:], in0=kf.t[:], scalar=float(-TWO_PI),
                in1=red.t[:], op0=ALU.mult, op1=ALU.add)
            P.v("dve", "tensor_scalar", [red], [kf], out=kf.t[:], in0=red.t[:], scalar1=float(np.pi),
                scalar2=float(-TWO_PI), op0=ALU.is_gt, op1=ALU.mult)
            P.v("dve", "tensor_tensor", [red, kf], [red], out=red.t[:], in0=red.t[:], in1=kf.t[:], op=ALU.add)
            P.v("dve", "tensor_scalar", [red], [red], out=red.t[:], in0=red.t[:], scalar1=float(-np.pi),
                scalar2=float(np.pi), op0=ALU.max, op1=ALU.min)
            P.act(dst.t[:], red.t[:], AF.Sin, [red], [dst])
        fw.barrier()

    ctx = dict(P=P, W=W, C=C, cst=cst, identb=identb, onesb=onesb, swam=swam, misc=misc, cosT=cosT, sinT=sinT,
               gbs=gbs, psr=psr, PS=PS, y=y, x_in=x_in, mem_in=mem_in)
    for l in range(L):
        xsrc, xb = (x_in, db["x"]) if l == 0 else (y, db["y"])
        phase_A(ctx, l, xsrc, xb)
        if stop_after == ("A", l):
            break
        phase_G(ctx, l)
        if stop_after == ("G", l):
            break
        phase_O(ctx, l, xsrc, xb)
        if stop_after == ("O", l):
            break
        phase_X(ctx, l)
        if stop_after == ("X", l):
            break
        phase_M(ctx, l)
        if stop_after == ("M", l):
            break
    fw.barrier()
    es.close()
    return P


def rms_rows(P, s, xt, rows, ncols, scratch, tag):
    ssq = P.sb(s, "ssq" + tag, [128, 1])
    P.act(scratch.t[:rows, :ncols], xt.t[:rows, :ncols], AF.Square, [xt], [scratch, ssq], accum_out=ssq.t[:rows, :])
    P.rsqrt(ssq.t[:rows, :], ssq.t[:rows, :], ncols * EPS, [ssq], [ssq])
    return ssq


def norm_transpose(ctx, s, src_ap, src_buf, gain_ap, gain_buf, hT):
    P = ctx["P"]; nc = P.nc
    psr = ctx["psr"]; identb = ctx["identb"]
    ntile = src_ap.shape[0] // 128
    with ExitStack() as s1:
        g32 = P.sb(s1, "g32", [128, D])
        P.dma("sp", g32.t[:], bcast_rows(gain_ap, D), [gain_buf], [g32])
        P.v("dve", "tensor_scalar", [g32], [g32], out=g32.t[:], in0=g32.t[:], scalar1=float(np.sqrt(D)), scalar2=None,
            op0=ALU.mult)
        xr = P.sbring(s1, "xr", [128, D], F32, 4)
        jr = P.sbring(s1, "junk", [128, D], F32, 4)
        hr = P.sbring(s1, "hr", [128, D], BF16, 4)
        sr = Ring([P.sb(s1, f"ssqA{i}", [128, 1]) for i in range(6)])

        def tile(t):
            xt = xr.next(); jk = jr.next(); hb = hr.next(); ssq = sr.next()
            P.dma("sp" if t % 2 == 0 else "act", xt.t[:], src_ap[t * 128:(t + 1) * 128, :], [src_buf], [xt])
            P.act(jk.t[:], xt.t[:], AF.Square, [xt], [jk, ssq], accum_out=ssq.t[:])
            P.act(ssq.t[:], ssq.t[:], AF.Sqrt, [ssq], [ssq], bias=P.constcol(D * EPS))
            yield
            P.v("dve", "reciprocal", [ssq], [ssq], out=ssq.t[:], in_=ssq.t[:])
            yield
            P.act(jk.t[:], xt.t[:], AF.Copy, [xt, ssq], [jk], scale=ssq.t[:, 0:1])
            yield
            P.v("dve", "tensor_tensor", [jk, g32], [hb], out=hb.t[:], in0=jk.t[:], in1=g32.t[:], op=ALU.mult)
            yield
            ps = psr.next()
            pv = ps.t[:].bitcast(BF16)
            for k in range(8):
                P.tr(pv[:, k * 128:(k + 1) * 128], hb.t[:, k * 128:(k + 1) * 128], identb.t[:], [hb, identb], [ps],
                     inc=(k == 7))
            yield
            P.act(hT.t[:, :, t * 128:(t + 1) * 128], pv.rearrange("p (k n) -> p k n", k=8), AF.Copy, [ps], [hT])
        run_window((tile(t) for t in range(ntile)), 3)
        P.fw.barrier()


def phase_A(ctx, l, xsrc, xbuf):
    P = ctx["P"]; nc = P.nc; fw = P.fw; W = ctx["W"]; db = P.dbuf; dr = P.dram
    psr = ctx["psr"]; C = ctx["C"]; cst = ctx["cst"]; onesb = ctx["onesb"]; gbs = ctx["gbs"]
    cosT, sinT = ctx["cosT"], ctx["sinT"]
    w_in = W["w_in"][l]
    with ExitStack() as s:
        hT = P.sb(s, "hT", [128, 8, S], BF16)
        norm_transpose(ctx, s, xsrc, xbuf, W["norm_mix"][l], db["norm_mix"], hT)
        if "HT" in P.debug:
            P.dma("sp", dr["HT"], hT.t[:], [hT], [db["HT"]])
        if P.stop_after == ("A0", l):
            fw.barrier()
            return
        wr = P.sbring(s, "wt", [128, 8, 512], BF16, 2)

        def load_w(c0, n):
            wt = wr.next()
            P.dma("pool", wt.t[:, :, :n], w_in[:, c0:c0 + n].rearrange("(k p) n -> p k n", p=128), [db["w_in"]], [wt])
            return wt

        def tjob(c0, n, epi, extra=None):
            wt = load_w(c0, n)

            def tiles():
                for t in range(NT):
                    ps = psr.next()
                    for k in range(8):
                        P.mm(ps.t[:, :n], hT.t[:, k, t * 128:(t + 1) * 128], wt.t[:, k, :n], [hT, wt], [ps],
                             start=(k == 0), stop=(k == 7 and extra is None))
                    if extra is not None:
                        extra(ps, n)
                    r = epi(t, ps, n)
                    yield r if hasattr(r, "__next__") else None
            run_window(tiles(), 3)

        def fjob(c0, n, epi):
            wt = load_w(c0, n)
            for tb in range(S // 512):
                ps = psr.next()
                for k in range(8):
                    P.mm(ps.t[:n, :], wt.t[:, k, :n], hT.t[:, k, tb * 512:(tb + 1) * 512], [hT, wt], [ps],
                         start=(k == 0), stop=(k == 7))
                epi(tb, ps, n)

        with ExitStack() as s2:
          if 'SKIPGDN' not in P.debug:
              cw = P.sb(s2, "cw", [128, 12, 4])
              P.dma("sp", cw.t[:], W["gdn_convT"][l].rearrange("(b p) j -> p b j", p=128), [db["gdn_convT"]], [cw])
              prer = P.sbring(s2, "pre", [128, S + 3], F32, 2)
              accr = P.sbring(s2, "acc", [128, S], F32, 2)
              accbr = P.sbring(s2, "accb", [128, S], BF16, 2)
              sqr = P.sbring(s2, "sq", [128, 512], BF16, 8)
              rnr = P.sbring(s2, "rn", [128, 512], F32, 4)
              for fb in range(12):
                  pre = prer.next(); acc = accr.next()
                  P.v("pool", "memset", [], [pre], pre.t[:, 0:3], 0.0)

                  def epi(tb, ps, n, pre=pre):
                      P.act(pre.t[:, 3 + tb * 512:3 + (tb + 1) * 512], ps.t[:, :], AF.Copy, [ps], [pre])
                  fjob(OFF["a_q"] + fb * 128, 128, epi)
                  eng = "dve"
                  P.v(eng, "tensor_scalar", [pre, cw], [acc], out=acc.t[:], in0=pre.t[:, 3:3 + S],
                      scalar1=cw.t[:, fb, 3:4], scalar2=None, op0=ALU.mult)
                  for j in range(3):
                      P.v(eng, "scalar_tensor_tensor", [pre, cw, acc], [acc], out=acc.t[:], in0=pre.t[:, j:j + S],
                          scalar=cw.t[:, fb, j:j + 1], in1=acc.t[:], op0=ALU.mult, op1=ALU.add)
                  accb = accbr.next()
                  if fb < 8:
                      P.act(acc.t[:], acc.t[:], AF.Silu, [acc], [acc])
                      qs = (128.0 ** -0.5) if fb < 4 else 1.0
                      nb_ = S // 512
                      sqs = [sqr.next() for _ in range(nb_)]; rns = {}
                      for tb in range(nb_):
                          P.act(sqs[tb].t[:], acc.t[:, tb * 512:(tb + 1) * 512], AF.Square, [acc], [sqs[tb]])
                      for hb_ in range(2):
                          pss = []
                          for tb in range(hb_ * 4, hb_ * 4 + 4):
                              rns[tb] = rnr.next()
                              ps = psr.next(); pss.append(ps)
                              P.mm(ps.t[:], onesb.t[:], sqs[tb].t[:], [onesb, sqs[tb]], [ps])
                          for i, tb in enumerate(range(hb_ * 4, hb_ * 4 + 4)):
                              P.act(rns[tb].t[:], pss[i].t[:], AF.Sqrt, [pss[i]], [rns[tb]], bias=P.constcol(1e-6))
                          for tb in range(hb_ * 4, hb_ * 4 + 4):
                              sl = slice(tb * 512, (tb + 1) * 512)
                              P.v("dve", "reciprocal", [rns[tb]], [rns[tb]], out=rns[tb].t[:], in_=rns[tb].t[:])
                              P.v("dve", "scalar_tensor_tensor", [acc, rns[tb]], [accb], out=accb.t[:, sl], in0=acc.t[:, sl],
                                  scalar=float(qs), in1=rns[tb].t[:], op0=ALU.mult, op1=ALU.mult)
                  else:
                      P.act(accb.t[:], acc.t[:], AF.Silu, [acc], [accb])
                  P.dma("sp", dr["GQKV"][fb], accb.t[:], [accb], [db["GQKV"]])
          fw.barrier()

        with ExitStack() as s2:
            o16r = P.sbring(s2, "o16", [128, 512], BF16, 5)
            o32r = P.sbring(s2, "o32", [128, 512], F32, 4)

            def store(dst, c0, dt16, func=AF.Copy):
                def epi(t, ps, n):
                    o = (o16r if dt16 else o32r).next()
                    P.act(o.t[:, :n], ps.t[:, :n], func, [ps], [o])
                    P.dma("sp", dr[dst][t * 128:(t + 1) * 128, c0:c0 + n], o.t[:, :n], [o], [db[dst]])
                return epi

            if "ONLYCQ" in P.debug:
                tjob(OFF["c_q"], 256, store("CQ", 0, False))
                fw.barrier()
                return
            tjob(OFF["a_z"], 512, store("ZS", 0, True, AF.Silu))
            par = P.sb(s2, "par", [128, 8])
            P.dma("sp", par.t[:, 0:4], bcast_rows(W["gdn_a_log"][l], 4), [db["gdn_a_log"]], [par])
            P.dma("sp", par.t[:, 4:8], bcast_rows(W["gdn_dt_bias"][l], 4), [db["gdn_dt_bias"]], [par])
            P.act(par.t[:, 0:4], par.t[:, 0:4], AF.Exp, [par], [par])
            t8r = P.sbring(s2, "t8", [128, 8], F32, 2)

            def epi_ab(t, ps, n):
                t8 = t8r.next()
                P.v("dve", "tensor_tensor", [ps, par], [t8], out=t8.t[:, 0:4], in0=ps.t[:, 0:4], in1=par.t[:, 4:8],
                    op=ALU.add)
                P.act(t8.t[:, 0:4], t8.t[:, 0:4], AF.Exp, [t8], [t8])
                P.act(t8.t[:, 0:4], t8.t[:, 0:4], AF.Ln, [t8], [t8], bias=P.constcol(1.0))
                P.v("dve", "scalar_tensor_tensor", [t8, par], [gbs], out=gbs.t[:, t, 0:4], in0=t8.t[:, 0:4],
                    scalar=-1.0, in1=par.t[:, 0:4], op0=ALU.mult, op1=ALU.mult)
                P.act(gbs.t[:, t, 4:8], ps.t[:, 4:8], AF.Sigmoid, [ps], [gbs])
            tjob(OFF["a_ab"], 8, epi_ab)

            gq = P.sb(s2, "gq", [128, 2, 64])
            P.dma("sp", gq.t[:, 0, :], bcast_rows(W["swa_q_norm"][l], 64), [db["swa_q_norm"]], [gq])
            P.dma("sp", gq.t[:, 1, :], bcast_rows(W["swa_k_norm"][l], 64), [db["swa_k_norm"]], [gq])
            P.v("dve", "tensor_scalar", [gq], [gq], out=gq.t[:], in0=gq.t[:], scalar1=8.0, scalar2=None, op0=ALU.mult)
            sqr = P.sbring(s2, "sq2", [128, 512], F32, 4)
            xnr = P.sbring(s2, "xn", [128, 512], F32, 4)
            tmr = P.sbring(s2, "tm", [128, 256], F32, 16)
            ssr = P.sbring(s2, "ss", [128, 8], F32, 4)

            def qk_epi(dst, c0, which):
                def epi(t, ps, n):
                    nh = n // 64
                    sq = sqr.next(); xn = xnr.next(); ss = ssr.next(); o = o16r.next(); tm = tmr.next(); tm2 = tmr.next()
                    cc = tmr.next(); cc2 = tmr.next()
                    v3 = lambda tt, w: tt.t[:, :nh * w].rearrange("p (h d) -> p h d", d=w)
                    P.act(sq.t[:, :n], ps.t[:, :n], AF.Square, [ps], [sq])
                    yield
                    P.v("dve", "tensor_reduce", [sq], [ss], out=ss.t[:, :nh], in_=v3(sq, 64), axis=AX.X, op=ALU.add)
                    yield
                    P.act(ss.t[:, :nh], ss.t[:, :nh], AF.Sqrt, [ss], [ss], bias=P.constcol(64 * EPS))
                    yield
                    P.v("dve", "reciprocal", [ss], [ss], out=ss.t[:, :nh], in_=ss.t[:, :nh])
                    x3 = v3(xn, 64)
                    P.v("dve", "tensor_tensor", [ps, ss], [xn], out=x3, in0=ps.t[:, :n].rearrange("p (h d) -> p h d", d=64),
                        in1=ss.t[:, :nh].unsqueeze(2).to_broadcast([128, nh, 64]), op=ALU.mult)
                    yield
                    P.v("pool", "tensor_tensor", [xn, gq], [xn], out=x3, in0=x3,
                        in1=gq.t[:, which:which + 1, :].to_broadcast([128, nh, 64]), op=ALU.mult)
                    yield
                    o3 = o.t[:, :n].rearrange("p (h d) -> p h d", d=64)
                    cb = cosT.t[:, t:t + 1, :].to_broadcast([128, nh, 32])
                    sb_ = sinT.t[:, t:t + 1, :].to_broadcast([128, nh, 32])
                    x1 = x3[:, :, 0:32]; x2 = x3[:, :, 32:64]
                    P.v("dve", "tensor_tensor", [xn, sinT], [tm], out=v3(tm, 32), in0=x2, in1=sb_, op=ALU.mult)
                    P.v("pool", "tensor_tensor", [xn, cosT], [cc], out=v3(cc, 32), in0=x1, in1=cb, op=ALU.mult)
                    P.v("dve", "tensor_tensor", [xn, sinT], [tm2], out=v3(tm2, 32), in0=x1, in1=sb_, op=ALU.mult)
                    P.v("pool", "tensor_tensor", [xn, cosT], [cc2], out=v3(cc2, 32), in0=x2, in1=cb, op=ALU.mult)
                    yield
                    P.v("dve", "tensor_tensor", [cc, tm], [o], out=o3[:, :, 0:32], in0=v3(cc, 32), in1=v3(tm, 32), op=ALU.subtract)
                    P.v("dve", "tensor_tensor", [cc2, tm2], [o], out=o3[:, :, 32:64], in0=v3(cc2, 32), in1=v3(tm2, 32), op=ALU.add)
                    P.dma("sp", dr[dst][t * 128:(t + 1) * 128, c0:c0 + n], o.t[:, :n], [o], [db[dst]])
                return epi

            tjob(OFF["b_q"], 512, qk_epi("SQ", 0, 0))
            tjob(OFF["b_q"] + 512, 256, qk_epi("SQ", 512, 0))
            tjob(OFF["b_k"], 512, qk_epi("SK", 0, 1))
            tjob(OFF["b_k"] + 512, 256, qk_epi("SK", 512, 1))
            tjob(OFF["b_v"], 512, store("SV", 0, True))
            tjob(OFF["b_v"] + 512, 256, store("SV", 512, True))
            tjob(OFF["c_q"], 256, store("CQ", 0, False))
            tjob(OFF["c_k"], 256, store("CK", 0, False))
            tjob(OFF["c_v"], 512, store("CV", 0, True))
            tjob(OFF["c_r"], 512, store("RS", 0, True, AF.Silu))
            clT = P.sb(s2, "clT", [32, S])
            gu = P.sb(s2, "gu", [32, 256])
            P.v("pool", "memset", [], [clT], clT.t[:], 1.0)
            P.dma("sp", gu.t[0:16, :], W["gla_gate_up"][l], [db["gla_gate_up"]], [gu])
            P.dma("sp", gu.t[16:17, :], W["gla_gate_bias"][l:l + 1, :], [db["gla_gate_bias"]], [gu])

            def epi_cl(tb, ps, n):
                P.act(clT.t[0:16, tb * 512:(tb + 1) * 512], ps.t[0:16, :], AF.Copy, [ps], [clT])
            fjob(OFF["c_low"], 16, epi_cl)
            for t in range(NT):
                ps = psr.next(); o = o32r.next()
                P.mm(ps.t[:, :256], clT.t[0:17, t * 128:(t + 1) * 128], gu.t[0:17, :], [clT, gu], [ps])
                P.act(o.t[:, :256], ps.t[:, :256], AF.Exp, [ps], [o], scale=-1.0)
                P.act(o.t[:, :256], o.t[:, :256], AF.Ln, [o], [o], bias=P.constcol(1.0))
                P.v("dve", "tensor_scalar", [o], [o], out=o.t[:, :256], in0=o.t[:, :256], scalar1=-1.0 / 16.0,
                    scalar2=None, op0=ALU.mult)
                P.dma("sp", dr["LA"][t * 128:(t + 1) * 128, :], o.t[:, :256], [o], [db["LA"]])
            gbias = P.sb(s2, "gbias", [1, 3 * D], BF16)
            P.dma("pool", gbias.t[:], W["gate_bias"][l:l + 1, :], [db["gate_bias"]], [gbias])
            for j in range(6):
                def extra(ps, n, j=j):
                    P.mm(ps.t[:, :n], onesb.t[0:1, :], gbias.t[0:1, j * 512:(j + 1) * 512], [onesb, gbias], [ps],
                         start=False, stop=True)
                tjob(OFF["gates"] + j * 512, 512, store("GT", j * 512, True, AF.Sigmoid), extra=extra)
        fw.barrier()


def head_norm_gate(P, s, rings, o_t, gain, gate, dst, dst_buf, t):
    jk = rings["jk"].next(); ss = rings["ss"].next(); ob = rings["ob"].next()
    P.act(jk.t[:], o_t.t[:], AF.Square, [o_t], [jk])
    P.v("dve", "tensor_reduce", [jk], [ss], out=ss.t[:], in_=jk.t[:].rearrange("p (h d) -> p h d", d=128), axis=AX.X,
        op=ALU.add)
    P.rsqrt(ss.t[:], ss.t[:], 128 * EPS, [ss], [ss])
    o3 = o_t.t[:].rearrange("p (h d) -> p h d", d=128)
    j3 = jk.t[:].rearrange("p (h d) -> p h d", d=128)
    P.v("dve", "tensor_tensor", [o_t, ss], [jk], out=j3, in0=o3, in1=ss.t[:].unsqueeze(2).to_broadcast([128, 4, 128]),
        op=ALU.mult)
    P.v("pool", "tensor_tensor", [jk, gain], [jk], out=j3, in0=j3, in1=gain.t[:].unsqueeze(1).to_broadcast([128, 4, 128]),
        op=ALU.mult)
    P.v("dve", "tensor_tensor", [jk, gate], [ob], out=ob.t[:], in0=jk.t[:], in1=gate.t[:], op=ALU.mult)
    P.dma("sp", dst[t * 128:(t + 1) * 128, :], ob.t[:], [ob], [dst_buf])


def phase_G(ctx, l):
    P = ctx["P"]; nc = P.nc; fw = P.fw; W = ctx["W"]; db = P.dbuf; dr = P.dram
    C = ctx["C"]; cst = ctx["cst"]; gbs = ctx["gbs"]; misc = ctx["misc"]; PS = ctx["PS"]; identb = ctx["identb"]
    ident = C["ident"]
    with ExitStack() as s:
        def quarters(b):
            bf = PS[b].t[:].bitcast(BF16)
            return Ring([T(PS[b].t[:, q * 128:(q + 1) * 128], b=PS[b].b, ps=True, tb=bf[:, q * 256:q * 256 + 128])
                         for q in range(4)])
        PQh = [quarters(h) for h in range(4)]
        PQc = Ring([T(PS[b].t[:, 0:128], b=PS[b].b, ps=True, tb=PS[b].t[:].bitcast(BF16)[:, 0:128]) for b in (4, 5, 6)])
        PW = Ring([PS[7]])
        Sg = [P.sb(s, f"Sg{h}", [128, 128]) for h in range(4)]
        Sc = [P.sb(s, f"Sc{h}", [128, 128]) for h in range(4)]
        Sgb = [P.sb(s, f"Sgb{h}", [128, 128], BF16) for h in range(4)]
        Scb = [P.sb(s, f"Scb{h}", [128, 128], BF16) for h in range(4)]
        for h in range(4):
            for st in (Sg[h], Sc[h], Sgb[h], Scb[h]):
                P.v("pool", "memset", [], [st], st.t[:], 0.0)
        gn_a = P.sb(s, "gn_a", [128, 128]); gn_c = P.sb(s, "gn_c", [128, 128])
        for g_, nm in ((gn_a, "gdn_norm"), (gn_c, "gla_norm")):
            P.dma("sp", g_.t[:], bcast_rows(W[nm][l], 128), [db[nm]], [g_])
            P.v("dve", "tensor_scalar", [g_], [g_], out=g_.t[:], in0=g_.t[:], scalar1=float(np.sqrt(128.0)), scalar2=None,
                op0=ALU.mult)
        qkvr = P.sbring(s, "qkv", [128, 12, 128], BF16, 2)
        zr = P.sbring(s, "zt", [128, 512], BF16, 2)
        rsr = P.sbring(s, "rst", [128, 512], BF16, 2)
        lar = P.sbring(s, "la", [128, 256], F32, 2)
        cqr = P.sbring(s, "cq", [128, 256], F32, 2)
        ckr = P.sbring(s, "ck", [128, 256], F32, 2)
        cvr = P.sbring(s, "cv", [128, 512], BF16, 2)
        gsr = P.sbring(s, "gs", [128, 16], F32, 2)
        exr = P.sbring(s, "ex", [128, 12], F32, 2)
        smr = P.sbring(s, "sm", [128, 16], F32, 2)
        HB = []
        for h in range(4):
            d = {}
            for nm in ("dg", "tl", "tu", "dl", "du", "tq"):
                d[nm] = P.sb(s, f"{nm}{h}", [128, 128], F32)
            for nm in ("aq", "rt", "bv", "kd", "r2", "vn", "scT"):
                d[nm] = P.sb(s, f"{nm}{h}", [128, 128], BF16)
            d["xm"] = P.sbring(s, f"xm{h}", [128, 128], BF16, 3)
            d["xt"] = P.sbring(s, f"xt{h}", [128, 128], BF16, 3)
            HB.append(d)
        oar = P.sbring(s, "oa", [128, 512], F32, 2)
        ocr = P.sbring(s, "oc", [128, 512], F32, 2)
        rings = dict(jk=P.sbring(s, "jkh", [128, 512], F32, 2), ss=P.sbring(s, "ssh", [128, 4], F32, 2),
                     ob=P.sbring(s, "obh", [128, 512], BF16, 2))
        ebr = P.sbring(s, "eb", [128, 256], F32, 2); enr = P.sbring(s, "enb", [128, 256], F32, 2)
        err = P.sbring(s, "erev", [128, 256], F32, 2)
        qtr = P.sbring(s, "qt", [128, 256], BF16, 2); ktr = P.sbring(s, "kt", [128, 256], BF16, 2)
        kcr = P.sbring(s, "kdc", [128, 256], BF16, 2)
        qTr = P.sbring(s, "qtT", [128, 2, 128], BF16, 2); kTr = P.sbring(s, "ktT", [128, 2, 128], BF16, 2)
        eblr = P.sbring(s, "ebl", [128, 4], F32, 2)
        mbtb = P.sb(s, "mbtb", [128, 128], BF16)
        P.dma("pool", mbtb.t[:], P.dram["carr"][:, CNAMES.index("mbt"), :], [db["carr"]], [mbtb])
        bgen = phase_B_gen(ctx, l)
        next(bgen)
        b_alive = True

        def gdn_head(h, t, qkv, gs, ex, sm, oa):
            PQ = PQh[h]; B_ = HB[h]
            qT = qkv.t[:, h, :]; kT = qkv.t[:, 4 + h, :]; vT = qkv.t[:, 8 + h, :]
            gc_h = gs.t[:, h:h + 1]
            dg, tl, tu, dl, du, tq = (B_[k] for k in ("dg", "tl", "tu", "dl", "du", "tq"))
            aq, rt, bv, kd, r2, vn = (B_[k] for k in ("aq", "rt", "bv", "kd", "r2", "vn"))
            xmr, xtr = B_["xm"], B_["xt"]
            P.v("dve", "tensor_scalar", [cst, gs], [dg], out=dg.t[:], in0=ident, scalar1=gc_h, scalar2=None, op0=ALU.mult)
            yield
            pB = PQ.next(); pV = PQ.next(); pK = PQ.next()
            P.mm(pB.t[:], C["ones"], dg.t[:], [cst, dg], [pB])
            P.tr(pV.tb, vT, identb.t[:], [qkv, identb], [pV])
            P.tr(pK.tb, kT, identb.t[:], [qkv, identb], [pK])
            yield
            P.v("dve", "scalar_tensor_tensor", [pB, gs, cst], [tl], out=tl.t[:], in0=pB.t[:], scalar=gc_h,
                in1=C["bigls"], op0=ALU.subtract, op1=ALU.max)
            P.v("dve", "scalar_tensor_tensor", [pB, gs, cst], [tu], out=tu.t[:], in0=pB.t[:], scalar=gc_h,
                in1=C["negu"], op0=ALU.subtract, op1=ALU.min)
            P.act(bv.t[:], pV.tb, AF.Copy, [pV, gbs], [bv], scale=gbs.t[:, t, 4 + h:5 + h])
            P.act(kd.t[:], pK.tb, AF.Copy, [pK, sm], [kd], scale=sm.t[:, h:h + 1])
            P.act(dl.t[:], tl.t[:], AF.Exp, [tl], [dl], scale=-1.0)
            P.act(du.t[:], tu.t[:], AF.Exp, [tu], [du])
            yield
            pKK = PQ.next(); pKQ = PQ.next()
            P.mm(pKK.t[:], kT, kT, [qkv], [pKK])
            P.mm(pKQ.t[:], kT, qT, [qkv], [pKQ])
            yield
            xm = xmr.next(); xt = xtr.next()
            P.v("dve", "scalar_tensor_tensor", [pKK, sm, dl], [xm], out=xm.t[:], in0=pKK.t[:], scalar=sm.t[:, 4 + h:5 + h],
                in1=dl.t[:], op0=ALU.mult, op1=ALU.mult)
            P.v("dve", "tensor_tensor", [pKQ, du], [aq], out=aq.t[:], in0=pKQ.t[:], in1=du.t[:], op=ALU.mult)
            yield
            pXT = PQ.next()
            P.tr(pXT.tb, xm.t[:], identb.t[:], [xm, identb], [pXT])
            yield
            P.act(xt.t[:], pXT.tb, AF.Copy, [pXT], [xt])
            P.v("dve", "tensor_tensor", [pXT, identb], [rt], out=rt.t[:], in0=pXT.tb, in1=identb.t[:], op=ALU.add)
            yield
            Pm, PTm = xm, xt
            for lev in range(5):
                p1 = PQ.next()
                P.mm(p1.t[:], PTm.t[:], Pm.t[:], [PTm, Pm], [p1])
                if lev < 4:
                    p2 = PQ.next()
                    P.mm(p2.t[:], Pm.t[:], PTm.t[:], [PTm, Pm], [p2])
                yield
                n1 = xmr.next()
                P.act(n1.t[:], p1.t[:], AF.Copy, [p1], [n1])
                if lev < 4:
                    n2 = xtr.next()
                    P.v("dve", "tensor_copy", [p2], [n2], out=n2.t[:], in_=p2.t[:])
                yield
                p3 = PQ.next()
                P.mm(p3.t[:], n1.t[:], rt.t[:], [n1, rt], [p3])
                yield
                P.v("dve", "tensor_tensor", [p3, rt], [rt], out=rt.t[:], in0=rt.t[:], in1=p3.t[:], op=ALU.add)
                yield
                Pm = n1
                if lev < 4:
                    PTm = n2
            for c in range(2):
                r = slice(64 * c, 64 * c + 64)
                pKS = PQ.next(); pQS = PQ.next()
                P.mm(pKS.t[:], kT, Sgb[h].t[:], [qkv, Sgb[h]], [pKS])
                P.mm(pQS.t[:], qT, Sgb[h].t[:], [qkv, Sgb[h]], [pQS])
                yield
                P.v("dve", "scalar_tensor_tensor", [pKS, sm, bv], [r2], out=r2.t[r, :], in0=pKS.t[r, :],
                    scalar=sm.t[r, 8 + h:9 + h], in1=bv.t[r, :], op0=ALU.mult, op1=ALU.add)
                P.act(tq.t[r, :], pQS.t[r, :], AF.Copy, [pQS, ex], [tq], scale=ex.t[r, h:h + 1])
                yield
                pVN = PQ.next()
                P.mm(pVN.t[:], rt.t[r, :], r2.t[r, :], [rt, r2], [pVN])
                yield
                P.act(vn.t[r, :], pVN.t[r, :], AF.Copy, [pVN], [vn])
                yield
                pAV = PQ.next(); pSU = PQ.next()
                P.mm(pAV.t[:], aq.t[r, :], vn.t[r, :], [aq, vn], [pAV])
                P.mm(pSU.t[:], kd.t[r, :], vn.t[r, :], [kd, vn], [pSU])
                yield
                egl = ex.t[:, 4 + 4 * c + h:5 + 4 * c + h]
                P.v("dve", "scalar_tensor_tensor", [Sg[h], ex, pSU], [Sg[h]], out=Sg[h].t[:], in0=Sg[h].t[:], scalar=egl,
                    in1=pSU.t[:], op0=ALU.mult, op1=ALU.add)
                P.v("dve", "tensor_tensor", [pAV, tq], [oa], out=oa.t[r, h * 128:(h + 1) * 128], in0=pAV.t[r, :],
                    in1=tq.t[r, :], op=ALU.add)
                P.v("pool", "tensor_copy", [Sg[h]], [Sgb[h]], out=Sgb[h].t[:], in_=Sg[h].t[:])
                yield

        def gla_tile(t, la, cq, ck, cv, oc):
            PQ = PQc
            eb = ebr.next(); enb = enr.next(); erev = err.next(); qt = qtr.next(); kt = ktr.next(); kdc = kcr.next()
            pb = PW.next()
            P.mm(pb.t[:, 0:256], C["mbt"], la.t[:], [cst, la], [pb])
            P.mm(pb.t[:, 256:512], C["mrev"], la.t[:], [cst, la], [pb])
            pe_ = PQ.next()
            for p in range(2):
                P.mm(pe_.t[:, 2 * p:2 * p + 2], la.t[:, p * 128:(p + 1) * 128], misc.t[:, 64:66], [la, misc], [pe_])
            yield
            ebl = eblr.next()
            P.act(eb.t[:], pb.t[:, 0:256], AF.Exp, [pb], [eb])
            P.act(enb.t[:], pb.t[:, 0:256], AF.Exp, [pb], [enb], scale=-1.0)
            P.act(erev.t[:], pb.t[:, 256:512], AF.Exp, [pb], [erev])
            P.act(ebl.t[:], pe_.t[:, 0:4], AF.Exp, [pe_], [ebl])
            yield
            P.v("dve", "scalar_tensor_tensor", [cq, eb], [qt], out=qt.t[:], in0=cq.t[:], scalar=0.125, in1=eb.t[:],
                op0=ALU.mult, op1=ALU.mult)
            P.v("pool", "tensor_tensor", [ck, enb], [kt], out=kt.t[:], in0=ck.t[:], in1=enb.t[:], op=ALU.mult)
            P.v("pool", "tensor_tensor", [ck, erev], [kdc], out=kdc.t[:], in0=ck.t[:], in1=erev.t[:], op=ALU.mult)
            yield
            qtT = qTr.next(); ktT = kTr.next()
            for p in range(2):
                pq_ = PQ.next(); pk_ = PQ.next()
                P.tr(pq_.tb, qt.t[:, p * 128:(p + 1) * 128], identb.t[:], [qt, identb], [pq_])
                P.tr(pk_.tb, kt.t[:, p * 128:(p + 1) * 128], identb.t[:], [kt, identb], [pk_])
                yield
                P.act(qtT.t[:, p, :], pq_.tb, AF.Copy, [pq_], [qtT])
                P.v("dve", "tensor_copy", [pk_], [ktT], out=ktT.t[:, p, :], in_=pk_.tb)
                yield
            for h in range(4):
                p = h // 2; o = 64 * (h % 2); fo = slice(o, o + 64)
                scT = HB[h]["scT"]
                pS = PQ.next()
                P.mm(pS.t[:], ktT.t[fo, p, :], qtT.t[fo, p, :], [ktT, qtT], [pS])
                yield
                P.v("dve", "tensor_tensor", [pS, mbtb], [scT], out=scT.t[:], in0=pS.t[:], in1=mbtb.t[:], op=ALU.mult)
                yield
                for c in range(2):
                    r = slice(64 * c, 64 * c + 64)
                    pO = PQ.next(); pSU = PQ.next()
                    P.mm(pO.t[:], qtT.t[:, p, :], Scb[h].t[:, :], [qtT, Scb[h]], [pO], start=True, stop=False)
                    P.mm(pO.t[:], scT.t[:, :], cv.t[:, h * 128:(h + 1) * 128], [scT, cv], [pO], start=False, stop=True)
                    P.mm(pSU.t[:], kdc.t[r, p * 128:(p + 1) * 128], cv.t[r, h * 128:(h + 1) * 128], [kdc, cv], [pSU])
                    yield
                    P.act(oc.t[r, h * 128:(h + 1) * 128], pO.t[r, :], AF.Copy, [pO], [oc])
                    P.v("dve", "scalar_tensor_tensor", [Sc[h], ebl, pSU], [Sc[h]], out=Sc[h].t[fo, :], in0=Sc[h].t[fo, :],
                        scalar=ebl.t[fo, 2 * p + c:2 * p + c + 1], in1=pSU.t[fo, :], op0=ALU.mult, op1=ALU.add)
                    P.v("pool", "tensor_copy", [Sc[h]], [Scb[h]], out=Scb[h].t[fo, :], in_=Sc[h].t[fo, :])
                    yield

        for t in range(P.gnt if hasattr(P, 'gnt') else NT):
            sl = slice(t * 128, (t + 1) * 128)
            qkv = qkvr.next(); zt = zr.next(); rst = rsr.next(); la = lar.next(); cq = cqr.next(); ck = ckr.next()
            cv = cvr.next()
            for g3 in range(3):
                P.dma("sp", qkv.t[:, 4 * g3:4 * g3 + 4, :], dr["GQKV"][4 * g3:4 * g3 + 4, :, sl].rearrange("f p n -> p f n"),
                      [db["GQKV"]], [qkv])
            P.dma("act", zt.t[:], dr["ZS"][sl, :], [db["ZS"]], [zt])
            P.dma("act", rst.t[:], dr["RS"][sl, :], [db["RS"]], [rst])
            P.dma("sp", la.t[:], dr["LA"][sl, :], [db["LA"]], [la])
            P.dma("sp", cq.t[:], dr["CQ"][sl, :], [db["CQ"]], [cq])
            P.dma("act", ck.t[:], dr["CK"][sl, :], [db["CK"]], [ck])
            P.dma("act", cv.t[:], dr["CV"][sl, :], [db["CV"]], [cv])
            gs = gsr.next(); ex = exr.next(); sm = smr.next()
            pg = PQc.next()
            for i, nm in enumerate(("mbt", "sel0", "sel1", "selc")):
                P.mm(pg.t[:, i * 4:(i + 1) * 4], C[nm], gbs.t[:, t, 0:4], [cst, gbs], [pg])
            P.v("dve", "tensor_copy", [pg], [gs], out=gs.t[:], in_=pg.t[:, 0:16])
            P.act(ex.t[:], gs.t[:, 0:12], AF.Exp, [gs], [ex])
            P.v("dve", "tensor_tensor", [gs], [sm], out=sm.t[:, 12:16], in0=gs.t[:, 12:16], in1=gs.t[:, 0:4],
                op=ALU.subtract)
            P.act(sm.t[:, 0:4], sm.t[:, 12:16], AF.Exp, [sm], [sm])
            P.v("dve", "tensor_scalar", [gbs], [sm], out=sm.t[:, 4:8], in0=gbs.t[:, t, 4:8], scalar1=-1.0, scalar2=None,
                op0=ALU.mult)
            P.v("dve", "tensor_tensor", [sm, ex], [sm], out=sm.t[:, 8:12], in0=sm.t[:, 4:8], in1=ex.t[:, 0:4],
                op=ALU.mult)
            oa = oar.next(); oc = ocr.next()
            gens = []
            if 'NOGDN' not in P.debug:
                gens += [gdn_head(h, t, qkv, gs, ex, sm, oa) for h in range(4)]
            if 'NOGLA' not in P.debug:
                gens.append(gla_tile(t, la, cq, ck, cv, oc))
            while gens:
                for g_ in list(gens):
                    try:
                        next(g_)
                    except StopIteration:
                        gens.remove(g_)
            if 'NOGDN' not in P.debug:
                head_norm_gate(P, s, rings, oa, gn_a, zt, dr["GA"], db["GA"], t)
            if 'NOGLA' not in P.debug:
                head_norm_gate(P, s, rings, oc, gn_c, rst, dr["GC"], db["GC"], t)
            for _ in range(3):
                if b_alive:
                    try:
                        next(bgen)
                    except StopIteration:
                        b_alive = False
        while b_alive:
            try:
                next(bgen)
            except StopIteration:
                b_alive = False
        fw.barrier()


def phase_B_gen(ctx, l):
    P = ctx["P"]; nc = P.nc; fw = P.fw; db = P.dbuf; dr = P.dram
    identb = ctx["identb"]; swam = ctx["swam"]; PS = ctx["PS"]
    psr = ctx["psr"]
    with ExitStack() as s:
        qr = P.sbring(s, "bq", [128, 256], BF16, 2)
        kr = P.sbring(s, "bk", [128, 256], BF16, 2)
        vr = P.sbring(s, "bv", [128, 256], BF16, 2)
        ver = P.sbring(s, "vext", [128, 4, 65], BF16, 3)
        qkr = P.sbring(s, "qkT", [128, 4, 128], BF16, 3)
        per = P.sbring(s, "pexp", [128, 256], BF16, 4)
        pmr = P.sbring(s, "pm", [128, 256], BF16, 4)
        nor = P.sbring(s, "numsb", [128, 260], F32, 2)
        for it in ver.items:
            P.v("pool", "memset", [], [it], it.t[:], 1.0)
        for it in qkr.items:
            P.v("pool", "memset", [], [it], it.t[:], 0.0)
        prev_qk = qkr.items[-1]; prev_ve = ver.items[-1]
        yield
        for g, dil in enumerate((1, 4, 16)):
            Lg = S // dil
            for res in range(dil):
                for n in range(Lg // 128):
                    row0 = res + dil * 128 * n
                    def rows(name, width, c0):
                        a = dr[name]
                        return bass.AP(a.tensor, a.offset + row0 * width + c0, [[dil * width, 128], [1, 256 if width == 768 else 260]])
                    qt = qr.next(); kt = kr.next(); vt = vr.next(); ve = ver.next(); qk = qkr.next()
                    P.dma("sp", qt.t[:], rows("SQ", 768, g * 256), [db["SQ"]], [qt])
                    P.dma("act", kt.t[:], rows("SK", 768, g * 256), [db["SK"]], [kt])
                    P.dma("sp", vt.t[:], rows("SV", 768, g * 256), [db["SV"]], [vt])
                    P.v("pool", "tensor_copy", [vt], [ve], out=ve.t[:, :, 0:64], in_=vt.t[:].rearrange("p (h d) -> p h d", d=64))
                    pt = psr.next()
                    pv = pt.t[:].bitcast(BF16)
                    for i, src in enumerate((qt, qt, kt, kt)):
                        P.tr(pv[:, i * 128:(i + 1) * 128], src.t[:, (i % 2) * 128:(i % 2 + 1) * 128], identb.t[:], [src, identb], [pt],
                             inc=(i == 3))
                    P.act(qk.t[:], pv[:, 0:512].rearrange("p (a n) -> p a n", a=4), AF.Copy, [pt], [qk])
                    pms = []
                    for h in range(4):
                        p = h // 2; fo = slice(64 * (h % 2), 64 * (h % 2) + 64)
                        sc = psr.next()
                        P.mm(sc.t[:, 0:128], prev_qk.t[fo, 2 + p, :], qk.t[fo, p, :], [prev_qk, qk], [sc])
                        P.mm(sc.t[:, 128:256], qk.t[fo, 2 + p, :], qk.t[fo, p, :], [qk], [sc])
                        pe = per.next(); pm = pmr.next()
                        P.act(pe.t[:], sc.t[:, 0:256], AF.Exp, [sc], [pe], scale=0.125)
                        P.v("dve" if h % 2 == 0 else "pool", "tensor_tensor", [pe, swam], [pm], out=pm.t[:], in0=pe.t[:],
                            in1=swam.t[:, 1 if n == 0 else 0, :], op=ALU.mult)
                        pms.append(pm)
                    nu = psr.next()
                    for h in range(4):
                        P.mm(nu.t[:, h * 65:(h + 1) * 65], pms[h].t[:, 0:128], prev_ve.t[:, h, :], [pms[h], prev_ve], [nu],
                             start=True, stop=False)
                        P.mm(nu.t[:, h * 65:(h + 1) * 65], pms[h].t[:, 128:256], ve.t[:, h, :], [pms[h], ve], [nu],
                             start=False, stop=True)
                    no = nor.next()
                    P.act(no.t[:], nu.t[:, 0:260], AF.Copy, [nu], [no])
                    a = dr["NUM"]
                    dst = bass.AP(a.tensor, a.offset + (g * S + row0) * 260, [[dil * 260, 128], [1, 260]])
                    P.dma("sp", dst, no.t[:], [no], [db["NUM"]])
                    prev_qk = qk; prev_ve = ve
                    yield


def phase_O(ctx, l, xsrc, xbuf):
    P = ctx["P"]; nc = P.nc; fw = P.fw; W = ctx["W"]; db = P.dbuf; dr = P.dram
    identb = ctx["identb"]; psr = ctx["psr"]; y = ctx["y"]
    with ExitStack() as s:
        WA = P.sb(s, "WA", [128, 4, D], BF16); WB = P.sb(s, "WB", [128, 2, D], BF16)
        WC = P.sb(s, "WC", [128, 4, D], BF16); WM = P.sb(s, "WM", [128, 8, D], BF16)
        for wt, nm in ((WA, "w_branch_a"), (WB, "w_branch_b"), (WC, "w_branch_c"), (WM, "w_mix_out")):
            P.dma("pool", wt.t[:], W[nm][l].rearrange("(k p) n -> p k n", p=128), [db[nm]], [wt])
        gar = P.sbring(s, "ga", [128, 512], BF16, 3); gcr = P.sbring(s, "gc", [128, 512], BF16, 3)
        nmr = P.sbring(s, "nm", [128, 3, 260], F32, 3)
        gtr = P.sbring(s, "gt", [128, 3 * D], BF16, 3)
        xr = P.sbring(s, "xo", [128, D], F32, 3)
        rdr = P.sbring(s, "rden", [128, 4], F32, 3)
        obr = P.sbring(s, "obb", [128, 256], BF16, 3)
        aTr = P.sbring(s, "aT", [128, 8, 128], BF16, 3); bTr = P.sbring(s, "bT", [128, 2, 128], BF16, 3)
        t1r = P.sbring(s, "t1", [128, 512], F32, 4); t2r = P.sbring(s, "t2", [128, 512], F32, 8)
        ybr = P.sbring(s, "yb", [128, D], BF16, 3); yTr = P.sbring(s, "yT", [128, 8, 128], BF16, 3)
        PSsub = [Ring(ctx["PS"][0:4]), Ring(ctx["PS"][4:8])]

        def otile(t):
            psr = PSsub[t % 2]
            sl = slice(t * 128, (t + 1) * 128)
            ga = gar.next(); gc = gcr.next(); nm = nmr.next(); gt = gtr.next(); xt = xr.next()
            P.dma("sp", ga.t[:], dr["GA"][sl, :], [db["GA"]], [ga])
            P.dma("act", gc.t[:], dr["GC"][sl, :], [db["GC"]], [gc])
            P.dma("sp", nm.t[:], dr["NUM"][:, sl, :].rearrange("g p n -> p g n"), [db["NUM"]], [nm])
            P.dma("act", gt.t[:], dr["GT"][sl, :], [db["GT"]], [gt])
            P.dma("sp", xt.t[:], xsrc[sl, :], [xbuf], [xt])
            yield
            P.v("dve", "tensor_tensor", [nm], [nm], out=nm.t[:, 0, :], in0=nm.t[:, 0, :], in1=nm.t[:, 1, :], op=ALU.add)
            P.v("dve", "tensor_tensor", [nm], [nm], out=nm.t[:, 0, :], in0=nm.t[:, 0, :], in1=nm.t[:, 2, :], op=ALU.add)
            rd = rdr.next(); ob = obr.next()
            n3 = nm.t[:, 0, :].rearrange("p (h d) -> p h d", d=65)
            P.v("dve", "reciprocal", [nm], [rd], out=rd.t[:].unsqueeze(2), in_=n3[:, :, 64:65])
            P.v("dve", "tensor_tensor", [nm, rd], [ob], out=ob.t[:].rearrange("p (h d) -> p h d", d=64), in0=n3[:, :, 0:64],
                in1=rd.t[:].unsqueeze(2).to_broadcast([128, 4, 64]), op=ALU.mult)
            yield
            aT = aTr.next(); bT = bTr.next()
            pa_ = psr.next(); pva = pa_.t[:].bitcast(BF16)
            for k in range(8):
                src = ga if k < 4 else gc
                P.tr(pva[:, k * 128:(k + 1) * 128], src.t[:, (k % 4) * 128:(k % 4 + 1) * 128], identb.t[:], [src, identb], [pa_],
                     inc=(k == 7))
            pb_ = psr.next(); pvb = pb_.t[:].bitcast(BF16)
            for k in range(2):
                P.tr(pvb[:, k * 128:(k + 1) * 128], ob.t[:, k * 128:(k + 1) * 128], identb.t[:], [ob, identb], [pb_], inc=(k == 1))
            yield
            P.act(aT.t[:], pva.rearrange("p (k n) -> p k n", k=8), AF.Copy, [pa_], [aT])
            P.v("dve", "tensor_copy", [pb_], [bT], out=bT.t[:], in_=pvb[:, 0:256].rearrange("p (k n) -> p k n", k=2))
            yield
            yb = ybr.next()
            for half in range(2):
                cs = slice(half * 512, (half + 1) * 512)
                pA = psr.next(); pB = psr.next(); pC = psr.next()
                for k in range(4):
                    P.mm(pA.t[:], aT.t[:, k, :], WA.t[:, k, cs], [aT, WA], [pA], start=(k == 0), stop=(k == 3))
                for k in range(2):
                    P.mm(pB.t[:], bT.t[:, k, :], WB.t[:, k, cs], [bT, WB], [pB], start=(k == 0), stop=(k == 1))
                for k in range(4):
                    P.mm(pC.t[:], aT.t[:, 4 + k, :], WC.t[:, k, cs], [aT, WC], [pC], start=(k == 0), stop=(k == 3))
                yield
                t1 = t1r.next(); t2 = t2r.next(); t3 = t2r.next()
                P.v("dve", "tensor_tensor", [pA, gt], [t1], out=t1.t[:], in0=pA.t[:], in1=gt.t[:, half * 512:(half + 1) * 512],
                    op=ALU.mult)
                P.v("dve", "tensor_tensor", [pB, gt], [t2], out=t2.t[:], in0=pB.t[:], in1=gt.t[:, D + half * 512:D + (half + 1) * 512],
                    op=ALU.mult)
                P.v("dve", "tensor_tensor", [pC, gt], [t3], out=t3.t[:], in0=pC.t[:],
                    in1=gt.t[:, 2 * D + half * 512:2 * D + (half + 1) * 512], op=ALU.mult)
                yield
                P.v("pool", "tensor_tensor", [t1, t2], [t1], out=t1.t[:], in0=t1.t[:], in1=t2.t[:], op=ALU.add)
                P.v("pool", "tensor_tensor", [t1, t3], [yb], out=yb.t[:, cs], in0=t1.t[:], in1=t3.t[:], op=ALU.add)
                yield
            yT = yTr.next()
            py = psr.next(); pvy = py.t[:].bitcast(BF16)
            for k in range(8):
                P.tr(pvy[:, k * 128:(k + 1) * 128], yb.t[:, k * 128:(k + 1) * 128], identb.t[:], [yb, identb], [py], inc=(k == 7))
            yield
            P.act(yT.t[:], pvy.rearrange("p (k n) -> p k n", k=8), AF.Copy, [py], [yT])
            yield
            pos = []
            for half in range(2):
                cs = slice(half * 512, (half + 1) * 512)
                po = psr.next(); pos.append(po)
                for k in range(8):
                    P.mm(po.t[:], yT.t[:, k, :], WM.t[:, k, cs], [yT, WM], [po], start=(k == 0), stop=(k == 7))
            yield
            for half in range(2):
                cs = slice(half * 512, (half + 1) * 512)
                P.v("dve", "tensor_tensor", [pos[half], xt], [xt], out=xt.t[:, cs], in0=pos[half].t[:], in1=xt.t[:, cs], op=ALU.add)
            P.dma("sp", y[sl, :], xt.t[:], [xt], [db["y"]])
        run_window((otile(t) for t in range(NT)), 2)
        fw.barrier()


def head_rms(P, rings, src_ps, gain, out_bf):
    jk = rings["jk"].next(); ss = rings["ss"].next()
    P.act(jk.t[:], src_ps.t[:], AF.Square, [src_ps], [jk])
    P.v("dve", "tensor_reduce", [jk], [ss], out=ss.t[:], in_=jk.t[:].rearrange("p (h d) -> p h d", d=128), axis=AX.X,
        op=ALU.add)
    P.rsqrt(ss.t[:], ss.t[:], 128 * EPS, [ss], [ss])
    j3 = jk.t[:].rearrange("p (h d) -> p h d", d=128)
    P.v("dve", "tensor_tensor", [src_ps, ss], [jk], out=j3, in0=src_ps.t[:].rearrange("p (h d) -> p h d", d=128),
        in1=ss.t[:].unsqueeze(2).to_broadcast([128, 4, 128]), op=ALU.mult)
    P.v("pool", "tensor_tensor", [jk, gain], [out_bf], out=out_bf.t[:].rearrange("p (h d) -> p h d", d=128), in0=j3,
        in1=gain.t[:].unsqueeze(1).to_broadcast([128, 4, 128]), op=ALU.mult)


def phase_X(ctx, l):
    P = ctx["P"]; nc = P.nc; fw = P.fw; W = ctx["W"]; db = P.dbuf; dr = P.dram
    identb = ctx["identb"]; psr = ctx["psr"]; y = ctx["y"]
    with ExitStack() as s:
        hT = P.sb(s, "hTx", [128, 8, S], BF16)
        mT = P.sb(s, "mT", [128, 8, MEM], BF16)
        norm_transpose(ctx, s, y, db["y"], W["norm_cross"][l], db["norm_cross"], hT)
        norm_transpose(ctx, s, ctx["mem_in"], db["mem"], W["norm_mem"][l], db["norm_mem"], mT)
        WQ = P.sb(s, "WQ", [128, 8, 512], BF16); WKV = P.sb(s, "WKV", [128, 8, D], BF16); WO = P.sb(s, "WO", [128, 4, D], BF16)
        for wt, nm in ((WQ, "xa_wq"), (WKV, "xa_wkv"), (WO, "xa_wo")):
            P.dma("pool", wt.t[:], W[nm][l].rearrange("(k p) n -> p k n", p=128), [db[nm]], [wt])
        gq = P.sb(s, "xgq", [128, 128]); gk = P.sb(s, "xgk", [128, 128])
        for g_, nm in ((gq, "xa_q_norm"), (gk, "xa_k_norm")):
            P.dma("sp", g_.t[:], bcast_rows(W[nm][l], 128), [db[nm]], [g_])
            P.v("dve", "tensor_scalar", [g_], [g_], out=g_.t[:], in0=g_.t[:], scalar1=float(np.sqrt(128.0)), scalar2=None,
                op0=ALU.mult)
        rings = dict(jk=P.sbring(s, "jkx", [128, 512], F32, 3), ss=P.sbring(s, "ssx", [128, 4], F32, 3))
        kT = P.sb(s, "kTx", [128, 4, MEM], BF16)
        vext = P.sb(s, "vxx", [128, 2, 4, 129], BF16)
        P.v("pool", "memset", [], [vext], vext.t[:], 1.0)
        khr = P.sbring(s, "khat", [128, 512], BF16, 2)
        for mt in range(2):
            pk = psr.next(); pv_ = psr.next()
            for k in range(8):
                P.mm(pk.t[:], mT.t[:, k, mt * 128:(mt + 1) * 128], WKV.t[:, k, 0:512], [mT, WKV], [pk], start=(k == 0), stop=(k == 7))
            for k in range(8):
                P.mm(pv_.t[:], mT.t[:, k, mt * 128:(mt + 1) * 128], WKV.t[:, k, 512:1024], [mT, WKV], [pv_], start=(k == 0),
                     stop=(k == 7))
            kh = khr.next()
            head_rms(P, rings, pk, gk, kh)
            P.act(vext.t[:, mt, :, 0:128], pv_.t[:].rearrange("p (h d) -> p h d", d=128), AF.Copy, [pv_], [vext])
            pt = psr.next(); pvt = pt.t[:].bitcast(BF16)
            for h in range(4):
                P.tr(pvt[:, h * 128:(h + 1) * 128], kh.t[:, h * 128:(h + 1) * 128], identb.t[:], [kh, identb], [pt], inc=(h == 3))
            P.act(kT.t[:, :, mt * 128:(mt + 1) * 128], pvt[:, 0:512].rearrange("p (h n) -> p h n", h=4), AF.Copy, [pt], [kT])
        qhr = P.sbring(s, "qhat", [128, 512], BF16, 3)
        qTr = P.sbring(s, "qTx", [128, 4, 128], BF16, 3)
        per = P.sbring(s, "pex", [128, 256], BF16, 12)
        nsr = P.sbring(s, "nsx", [128, 4, 129], F32, 3)
        rdr = P.sbring(s, "rdx", [128, 4], F32, 3)
        obr = P.sbring(s, "obx", [128, 512], BF16, 3)
        oTr = P.sbring(s, "oTx", [128, 4, 128], BF16, 3)
        xr = P.sbring(s, "xx", [128, D], F32, 3)
        PSsub = [Ring(ctx["PS"][0:4]), Ring(ctx["PS"][4:8])]

        def xtile(t):
            psr = PSsub[t % 2]
            sl = slice(t * 128, (t + 1) * 128)
            xt = xr.next()
            P.dma("sp", xt.t[:], y[sl, :], [db["y"]], [xt])
            pq = psr.next()
            for k in range(8):
                P.mm(pq.t[:], hT.t[:, k, sl], WQ.t[:, k, :], [hT, WQ], [pq], start=(k == 0), stop=(k == 7))
            yield
            qh = qhr.next(); qT = qTr.next()
            jk = rings["jk"].next(); ss = rings["ss"].next()
            P.act(jk.t[:], pq.t[:], AF.Square, [pq], [jk])
            yield
            P.v("dve", "tensor_reduce", [jk], [ss], out=ss.t[:], in_=jk.t[:].rearrange("p (h d) -> p h d", d=128), axis=AX.X,
                op=ALU.add)
            yield
            P.act(ss.t[:], ss.t[:], AF.Sqrt, [ss], [ss], bias=P.constcol(128 * EPS))
            yield
            P.v("dve", "reciprocal", [ss], [ss], out=ss.t[:], in_=ss.t[:])
            j3 = jk.t[:].rearrange("p (h d) -> p h d", d=128)
            P.v("dve", "tensor_tensor", [pq, ss], [jk], out=j3, in0=pq.t[:].rearrange("p (h d) -> p h d", d=128),
                in1=ss.t[:].unsqueeze(2).to_broadcast([128, 4, 128]), op=ALU.mult)
            yield
            P.v("pool", "tensor_tensor", [jk, gq], [qh], out=qh.t[:].rearrange("p (h d) -> p h d", d=128), in0=j3,
                in1=gq.t[:].unsqueeze(1).to_broadcast([128, 4, 128]), op=ALU.mult)
            yield
            pt = psr.next(); pvt = pt.t[:].bitcast(BF16)
            for h in range(4):
                P.tr(pvt[:, h * 128:(h + 1) * 128], qh.t[:, h * 128:(h + 1) * 128], identb.t[:], [qh, identb], [pt], inc=(h == 3))
            yield
            P.act(qT.t[:], pvt[:, 0:512].rearrange("p (h n) -> p h n", h=4), AF.Copy, [pt], [qT])
            yield
            scs = []
            for h in range(4):
                sc = psr.next(); scs.append(sc)
                for mt in range(2):
                    P.mm(sc.t[:, mt * 128:(mt + 1) * 128], kT.t[:, h, mt * 128:(mt + 1) * 128], qT.t[:, h, :], [kT, qT], [sc])
            yield
            pes = []
            for h in range(4):
                pe = per.next()
                P.act(pe.t[:], scs[h].t[:, 0:256], AF.Exp, [scs[h]], [pe], scale=float(128.0 ** -0.5))
                pes.append(pe)
            yield
            ns = nsr.next()
            nus = []
            for hp in range(2):
                nu = psr.next(); nus.append(nu)
                for hh in range(2):
                    h = hp * 2 + hh
                    for mt in range(2):
                        P.mm(nu.t[:, hh * 129:(hh + 1) * 129], pes[h].t[:, mt * 128:(mt + 1) * 128], vext.t[:, mt, h, :],
                             [pes[h], vext], [nu], start=(mt == 0), stop=(mt == 1))
            yield
            P.act(ns.t[:, 0:2, :], nus[0].t[:, 0:258].rearrange("p (h d) -> p h d", d=129), AF.Copy, [nus[0]], [ns])
            P.v("dve", "tensor_copy", [nus[1]], [ns], out=ns.t[:, 2:4, :], in_=nus[1].t[:, 0:258].rearrange("p (h d) -> p h d", d=129))
            yield
            rd = rdr.next(); ob = obr.next(); oT = oTr.next()
            P.v("dve", "reciprocal", [ns], [rd], out=rd.t[:].unsqueeze(2), in_=ns.t[:, :, 128:129])
            P.v("dve", "tensor_tensor", [ns, rd], [ob], out=ob.t[:].rearrange("p (h d) -> p h d", d=128), in0=ns.t[:, :, 0:128],
                in1=rd.t[:].unsqueeze(2).to_broadcast([128, 4, 128]), op=ALU.mult)
            yield
            pt2 = psr.next(); pvt2 = pt2.t[:].bitcast(BF16)
            for h in range(4):
                P.tr(pvt2[:, h * 128:(h + 1) * 128], ob.t[:, h * 128:(h + 1) * 128], identb.t[:], [ob, identb], [pt2], inc=(h == 3))
            yield
            P.act(oT.t[:], pvt2[:, 0:512].rearrange("p (h n) -> p h n", h=4), AF.Copy, [pt2], [oT])
            yield
            pos = []
            for half in range(2):
                cs = slice(half * 512, (half + 1) * 512)
                po = psr.next(); pos.append(po)
                for k in range(4):
                    P.mm(po.t[:], oT.t[:, k, :], WO.t[:, k, cs], [oT, WO], [po], start=(k == 0), stop=(k == 3))
            yield
            for half in range(2):
                cs = slice(half * 512, (half + 1) * 512)
                P.v("dve", "tensor_tensor", [pos[half], xt], [xt], out=xt.t[:, cs], in0=pos[half].t[:], in1=xt.t[:, cs], op=ALU.add)
            P.dma("sp", y[sl, :], xt.t[:], [xt], [db["y"]])
        run_window((xtile(t) for t in range(NT)), 2)
        fw.barrier()


def phase_M(ctx, l):
    P = ctx["P"]; nc = P.nc; fw = P.fw; W = ctx["W"]; db = P.dbuf; dr = P.dram
    identb = ctx["identb"]; onesb = ctx["onesb"]; psr = ctx["psr"]; y = ctx["y"]; C = ctx["C"]; cst = ctx["cst"]
    misc = ctx["misc"]
    XBd = dr["XB"]; YBd = dr["YB"]
    if "breg" not in ctx:
        ctx["breg"] = nc.gpsimd.to_reg(XB_ROWS - 1)
    breg = ctx["breg"]
    with ExitStack() as s:
        DST = P.sb(s, "DST", [128, NT, 4], I32)
        GTE = P.sb(s, "GTE", [128, NT, 4])
        with ExitStack() as s1:
            g32 = P.sb(s1, "g32m", [128, D])
            P.dma("sp", g32.t[:], bcast_rows(W["norm_ffn"][l], D), [db["norm_ffn"]], [g32])
            P.v("dve", "tensor_scalar", [g32], [g32], out=g32.t[:], in0=g32.t[:], scalar1=float(np.sqrt(D)), scalar2=None,
                op0=ALU.mult)
            RW = P.sb(s1, "RW", [128, 8, NE])
            P.dma("sp", RW.t[:], W["router_w"][l].rearrange("(k p) e -> p k e", p=128), [db["router_w"]], [RW])
            rb = P.sb(s1, "rb", [1, NE])
            P.dma("sp", rb.t[:], W["router_b"][l:l + 1, :], [db["router_b"]], [rb])
            ltb = P.sb(s1, "ltb", [128, 128], BF16)
            P.dma("pool", ltb.t[:], P.dram["carr"][:, CNAMES.index("lt"), :], [db["carr"]], [ltb])
            carry = P.sb(s1, "carry", [128, NE])
            P.v("dve", "memset", [], [carry], carry.t[:], 0.0)
            xr = P.sbring(s1, "xm", [128, D], F32, 4); jr = P.sbring(s1, "jm", [128, D], F32, 4)
            hfr = P.sbring(s1, "hfm", [128, D], F32, 4); hbr = P.sbring(s1, "hbm", [128, D], BF16, 5)
            sqr = Ring([P.sb(s1, f"ssqm{i}", [128, 1]) for i in range(6)])
            hTr = P.sbring(s1, "hTf", [128, 8, 128], F32, 4)
            lgr = P.sbring(s1, "lg", [128, NE], F32, 4); t8r = P.sbring(s1, "top8", [128, 8], F32, 4)
            smr = P.sbring(s1, "smm", [128, 16], F32, 4)
            mkr = P.sbring(s1, "mk", [128, NE], F32, 4); mbr = P.sbring(s1, "mkb", [128, NE], BF16, 4)
            pfr = P.sbring(s1, "posf", [128, NE], F32, 4); ovr = P.sbring(s1, "ovf", [128, NE], F32, 4)
            ohr = P.sbring(s1, "oh", [128, NE], F32, 16)
            def rtile(t):
                sl = slice(t * 128, (t + 1) * 128)
                xt = xr.next(); jk = jr.next(); hf = hfr.next(); hb = hbr.next(); ssq = sqr.next()
                P.dma("sp" if t % 2 == 0 else "act", xt.t[:], y[sl, :], [db["y"]], [xt])
                P.act(jk.t[:], xt.t[:], AF.Square, [xt], [jk, ssq], accum_out=ssq.t[:])
                P.act(ssq.t[:], ssq.t[:], AF.Sqrt, [ssq], [ssq], bias=P.constcol(D * EPS))
                yield
                P.v("dve", "reciprocal", [ssq], [ssq], out=ssq.t[:], in_=ssq.t[:])
                yield
                P.act(jk.t[:], xt.t[:], AF.Copy, [xt, ssq], [jk], scale=ssq.t[:, 0:1])
                yield
                P.v("dve", "tensor_tensor", [jk, g32], [hf], out=hf.t[:], in0=jk.t[:], in1=g32.t[:], op=ALU.mult)
                yield
                P.v("pool", "tensor_copy", [hf], [hb], out=hb.t[:], in_=hf.t[:])
                hTf = hTr.next()
                pss = []
                for half in range(2):
                    ps2 = psr.next(); pss.append(ps2)
                    for k in range(4):
                        kk = half * 4 + k
                        P.tr(ps2.t[:, k * 128:(k + 1) * 128], hf.t[:, kk * 128:(kk + 1) * 128], C["ident"], [hf, cst], [ps2],
                             inc=(k == 3))
                yield
                P.act(hTf.t[:, 0:4, :], pss[0].t[:].rearrange("p (k n) -> p k n", k=4), AF.Copy, [pss[0]], [hTf])
                P.v("dve", "tensor_copy", [pss[1]], [hTf], out=hTf.t[:, 4:8, :], in_=pss[1].t[:].rearrange("p (k n) -> p k n", k=4))
                yield
                pl = psr.next()
                for k in range(8):
                    P.mm(pl.t[:, 0:NE], hTf.t[:, k, :], RW.t[:, k, :], [hTf, RW], [pl], start=(k == 0), stop=False)
                P.mm(pl.t[:, 0:NE], C["ones"][0:1, :], rb.t[0:1, :], [cst, rb], [pl], start=False, stop=True)
                yield
                lg = lgr.next(); t8 = t8r.next(); sm = smr.next(); mk = mkr.next(); mkb = mbr.next()
                P.act(lg.t[:], pl.t[:, 0:NE], AF.Copy, [pl], [lg])
                yield
                P.v("dve", "max", [lg], [t8], out=t8.t[:], in_=lg.t[:])
                P.v("dve", "tensor_scalar", [t8], [sm], out=sm.t[:, 0:1], in0=t8.t[:, 0:1], scalar1=-1.0, scalar2=None, op0=ALU.mult)
                P.v("dve", "tensor_scalar", [lg, t8], [mk], out=mk.t[:], in0=lg.t[:], scalar1=t8.t[:, 3:4], scalar2=None, op0=ALU.is_ge)
                yield
                P.act(sm.t[:, 4:8], t8.t[:, 0:4], AF.Exp, [t8, sm], [sm], bias=sm.t[:, 0:1], accum_out=sm.t[:, 1:2])
                P.v("pool", "tensor_copy", [mk], [mkb], out=mkb.t[:], in_=mk.t[:])
                yield
                P.v("dve", "reciprocal", [sm], [sm], out=sm.t[:, 2:3], in_=sm.t[:, 1:2])
                pp = psr.next()
                P.mm(pp.t[:, 0:NE], ltb.t[:], mkb.t[:], [ltb, mkb], [pp])
                P.mm(pp.t[:, NE:2 * NE], onesb.t[:], mkb.t[:], [onesb, mkb], [pp])
                yield
                posf = pfr.next(); ovf = ovr.next()
                P.v("dve", "tensor_tensor", [pp, carry], [posf], out=posf.t[:], in0=pp.t[:, 0:NE], in1=carry.t[:], op=ALU.add)
                P.v("dve", "tensor_tensor", [pp, carry], [carry], out=carry.t[:], in0=pp.t[:, NE:2 * NE], in1=carry.t[:], op=ALU.add)
                P.v("dve", "tensor_scalar", [posf], [ovf], out=ovf.t[:], in0=posf.t[:], scalar1=float(CAP), scalar2=1e7,
                    op0=ALU.is_ge, op1=ALU.mult)
                P.v("dve", "tensor_tensor", [posf, misc], [posf], out=posf.t[:], in0=posf.t[:], in1=misc.t[:, 32:64], op=ALU.add)
                P.v("dve", "tensor_tensor", [posf, ovf], [posf], out=posf.t[:], in0=posf.t[:], in1=ovf.t[:], op=ALU.add)
                yield
                for j in range(4):
                    oh = ohr.next()
                    P.v("dve", "tensor_scalar", [lg, t8], [oh], out=oh.t[:], in0=lg.t[:], scalar1=t8.t[:, j:j + 1], scalar2=None,
                        op0=ALU.is_equal)
                    P.v("dve", "tensor_tensor", [oh, posf], [oh], out=oh.t[:], in0=oh.t[:], in1=posf.t[:], op=ALU.mult)
                    P.v("dve", "tensor_reduce", [oh], [sm], out=sm.t[:, 8 + j:9 + j], in_=oh.t[:], axis=AX.X, op=ALU.add)
                    if j % 2 == 1:
                        yield
                P.v("dve", "tensor_scalar", [sm], [sm], out=sm.t[:, 12:16], in0=sm.t[:, 8:12], scalar1=float(XB_ROWS), scalar2=None,
                    op0=ALU.is_lt)
                P.v("dve", "scalar_tensor_tensor", [sm], [GTE], out=GTE.t[:, t, :], in0=sm.t[:, 4:8], scalar=sm.t[:, 2:3],
                    in1=sm.t[:, 12:16], op0=ALU.mult, op1=ALU.mult)
                P.v("dve", "tensor_copy", [sm], [DST], out=DST.t[:, t, :], in_=sm.t[:, 8:12])
                yield
                for j in range(4):
                    fw.idma(XBd, bass.IndirectOffsetOnAxis(ap=DST.t[:, t, j:j + 1], axis=0), hb.t[:, :], None,
                            reads=[hb.b, DST.b], writes=[db["XB"]], bounds_check=breg, oob_is_err=False)
            run_window((rtile(t) for t in range(NT)), 4)
            fw.barrier()
        with ExitStack() as s2:
            bins = P.sb(s2, "bins", [128, NE, 16])
            P.dma("sp", bins.t[:], W["moe_b_in"][l], [db["moe_b_in"]], [bins])
            WIr = P.sbring(s2, "WI", [128, 8, 2 * D], BF16, 2)
            WOr = P.sbring(s2, "WO2", [128, 8, D], BF16, 2)
            bor = P.sbring(s2, "bo", [1, D], BF16, 2)
            xbr = P.sbring(s2, "xbt", [128, D], BF16, 3)
            xTr = P.sbring(s2, "xTe", [128, 8, CAP], BF16, 2)
            aTr = P.sbring(s2, "actT", [128, 8, CAP], BF16, 2)
            gr = P.sbring(s2, "eg", [128, CAP], F32, 2); sgr = P.sbring(s2, "esg", [128, CAP], F32, 2)
            lr = P.sbring(s2, "el", [128, CAP], F32, 2)
            yor = P.sbring(s2, "yo", [128, D], F32, 2)
            def front(e):
                WI = WIr.next(); WO2 = WOr.next(); bo = bor.next()
                for kq in range(4):
                    P.dma("pool", WI.t[:, 2 * kq:2 * kq + 2, :],
                          W["moe_w_in"][l, e, kq * 256:(kq + 1) * 256, :].rearrange("(k p) n -> p k n", p=128), [db["moe_w_in"]], [WI])
                for kq in range(2):
                    P.dma("pool", WO2.t[:, 4 * kq:4 * kq + 4, :],
                          W["moe_w_out"][l, e, kq * 512:(kq + 1) * 512, :].rearrange("(k p) n -> p k n", p=128), [db["moe_w_out"]], [WO2])
                P.dma("pool", bo.t[:], W["moe_b_out"][l, e:e + 1, :], [db["moe_b_out"]], [bo])
                xT = xTr.next(); aT = aTr.next()
                for ct in range(NCAPT):
                    xb_ = xbr.next()
                    r0 = e * CAP + ct * 128
                    P.dma("sp", xb_.t[:], XBd[r0:r0 + 128, :], [db["XB"]], [xb_])
                    pt = psr.next(); pvt = pt.t[:].bitcast(BF16)
                    for k in range(8):
                        P.tr(pvt[:, k * 128:(k + 1) * 128], xb_.t[:, k * 128:(k + 1) * 128], identb.t[:], [xb_, identb], [pt], inc=(k == 7))
                    if ct % 2 == 0:
                        P.act(xT.t[:, :, ct * 128:(ct + 1) * 128], pvt.rearrange("p (k n) -> p k n", k=8), AF.Copy, [pt], [xT])
                    else:
                        P.v("dve", "tensor_copy", [pt], [xT], out=xT.t[:, :, ct * 128:(ct + 1) * 128],
                            in_=pvt.rearrange("p (k n) -> p k n", k=8))
                for fb in range(8):
                    pg0 = psr.next(); pl0 = psr.next(); prm = psr.next()
                    for (pt_, col, f0, n0, n1) in ((pg0, 0, fb, 0, 512), (pl0, 0, fb + 8, 0, 512), (prm, 0, fb, 512, CAP),
                                                    (prm, 128, fb + 8, 512, CAP)):
                        for k in range(8):
                            P.mm(pt_.t[:, col:col + (n1 - n0)], WI.t[:, k, f0 * 128:(f0 + 1) * 128], xT.t[:, k, n0:n1], [WI, xT], [pt_],
                                 start=(k == 0), stop=(k == 7))
                    g_ = gr.next(); sg = sgr.next(); l_ = lr.next()
                    bg = bins.t[:, e, fb:fb + 1]; bl = bins.t[:, e, fb + 8:fb + 9]
                    for (src, c0, c1, d0) in ((pg0, 0, 512, 0), (prm, 0, CAP - 512, 512)):
                        P.v("dve", "tensor_scalar", [src, bins], [g_], out=g_.t[:, d0:d0 + (c1 - c0)], in0=src.t[:, c0:c1], scalar1=bg,
                            scalar2=7.0, op0=ALU.add, op1=ALU.min)
                    for (src, c0, c1, d0) in ((pl0, 0, 512, 0), (prm, 128, 128 + CAP - 512, 512)):
                        P.v("dve", "tensor_scalar", [src, bins], [l_], out=l_.t[:, d0:d0 + (c1 - c0)], in0=src.t[:, c0:c1], scalar1=bl,
                            scalar2=7.0, op0=ALU.add, op1=ALU.min)
                    P.act(sg.t[:], g_.t[:], AF.Sigmoid, [g_], [sg], scale=1.702)
                    P.v("dve", "tensor_scalar", [l_], [l_], out=l_.t[:], in0=l_.t[:], scalar1=-7.0, scalar2=1.0, op0=ALU.max, op1=ALU.add)
                    P.v("dve", "tensor_tensor", [g_, sg], [g_], out=g_.t[:], in0=g_.t[:], in1=sg.t[:], op=ALU.mult)
                    P.v("dve", "tensor_tensor", [g_, l_], [aT], out=aT.t[:, fb, :], in0=g_.t[:], in1=l_.t[:], op=ALU.mult)
                return aT, WO2, bo

            def back(e, aT, WO2, bo):
                for ct in range(NCAPT):
                    yo = yor.next()
                    for half in range(2):
                        cs = slice(half * 512, (half + 1) * 512)
                        po = psr.next()
                        for k in range(8):
                            P.mm(po.t[:], aT.t[:, k, ct * 128:(ct + 1) * 128], WO2.t[:, k, cs], [aT, WO2], [po], start=(k == 0), stop=False)
                        P.mm(po.t[:], onesb.t[0:1, :], bo.t[0:1, cs], [onesb, bo], [po], start=False, stop=True)
                        P.act(yo.t[:, cs], po.t[:], AF.Copy, [po], [yo])
                    r0 = e * CAP + ct * 128
                    P.dma("sp", YBd[r0:r0 + 128, :], yo.t[:], [yo], [db["YB"]])

            st = front(0)
            for e in range(1, NE):
                st2 = front(e)
                back(e - 1, *st)
                st = st2
            back(NE - 1, *st)
            fw.barrier()
        with ExitStack() as s3:
            xr = P.sbring(s3, "xc", [128, D], F32, 4)
            gr_ = P.sbring(s3, "gth", [128, D], F32, 16)
            for it in gr_.items:
                P.v("dve", "memset", [], [it], it.t[:], 0.0)
            for t in range(NT):
                sl = slice(t * 128, (t + 1) * 128)
                xt = xr.next()
                P.dma("sp", xt.t[:], y[sl, :], [db["y"]], [xt])
                for j in range(4):
                    gt_ = gr_.next()
                    fw.idma(gt_.t[:, :], None, YBd, bass.IndirectOffsetOnAxis(ap=DST.t[:, t, j:j + 1], axis=0),
                            reads=[db["YB"], DST.b], writes=[gt_.b], bounds_check=breg, oob_is_err=False)
                    P.v("dve", "scalar_tensor_tensor", [gt_, GTE, xt], [xt], out=xt.t[:], in0=gt_.t[:], scalar=GTE.t[:, t, j:j + 1],
                        in1=xt.t[:], op0=ALU.mult, op1=ALU.add)
                P.dma("sp", y[sl, :], xt.t[:], [xt], [db["y"]])
            fw.barrier()


def make_in_maps(inputs, n_layers=DEPTH, cores=range(8)):
    f = lambda a: np.ascontiguousarray(np.asarray(a))
    shared = {"carr": CARR, "swamask": SWAMASK, "misc": MISC}
    for k, v in inputs.items():
        if k in ("x", "mem", "positions"):
            continue
        a = np.asarray(v)[:n_layers]
        if k == "gdn_conv":
            shared["gdn_convT"] = f(a.transpose(0, 2, 1))
        elif k == "moe_b_in":
            shared[k] = f(a.reshape(n_layers, NE, 16, 128).transpose(0, 3, 1, 2))
        elif k == "gate_bias":
            shared["gate_bias"] = f(a.reshape(n_layers, 3 * D))
        else:
            shared[k] = f(a)
    maps = []
    for c in cores:
        m = dict(shared)
        m["x"] = f(inputs["x"][c])
        m["mem"] = f(inputs["mem"][c])
        m["pos"] = f(np.asarray(inputs["positions"][c]).astype(np.int32).reshape(NT, 128).T)
        maps.append(m)
    return maps


_CACHE = {}


def kernel(**inputs):
    if "prog" not in _CACHE:
        _CACHE["prog"] = build_program()
    P = _CACHE["prog"]
    maps = make_in_maps(inputs)
    res = run_bass_kernel_spmd(P.nc, maps, core_ids=list(range(8)))
    return np.stack([np.asarray(r["y"]).reshape(S, D) for r in res.results], axis=0).astype(np.float32)
```

```python
import numpy as np
from contextlib import ExitStack
import concourse.bass as bass
import concourse.mybir as mybir
from concourse.bass_utils import run_bass_kernel_spmd

F32 = mybir.dt.float32
BF16 = mybir.dt.bfloat16
I32 = mybir.dt.int32
AF = mybir.ActivationFunctionType
ALU = mybir.AluOpType
AX = mybir.AxisListType

D = 1024
S = 4096
NT = S // 128
DEPTH = 4
N_IN = 8984
MEM = 256
NE = 32
CAP = 640
NCAPT = CAP // 128
XB_ROWS = NE * CAP
EPS = 1e-6


class Buf:
    __slots__ = ("name", "writer", "readers")

    def __init__(self, name=""):
        self.name = name
        self.writer = None
        self.readers = {}


class FW:
    def __init__(self, nc, es, n_dma_sems=10):
        self.nc = nc
        self.es = es
        self.eng = {"pe": nc.tensor, "act": nc.scalar, "dve": nc.vector, "pool": nc.gpsimd, "sp": nc.sync}
        self.sem = {}
        self.cnt = {}
        for k in ("pe", "act", "dve", "pool"):
            self.sem[k] = es.enter_context(nc.semaphore("S_" + k))
            self.cnt[k] = 0
        self.dq = {}
        for q in ("sp", "act", "pool"):
            ring = []
            for i in range(n_dma_sems):
                key = f"d_{q}{i}"
                self.sem[key] = es.enter_context(nc.semaphore("S_" + key))
                self.cnt[key] = 0
                ring.append(key)
            self.dq[q] = [ring, 0]
        self.known = {k: {} for k in ("pe", "act", "dve", "pool", "sp")}
        self.n_inst = 0
        self.n_wait = 0

    def _wait(self, stream, dep):
        key, val = dep
        if stream == "pe" and key == "pe":
            return
        if self.known[stream].get(key, 0) >= val:
            return
        self.eng[stream].wait_ge(self.sem[key], val)
        self.known[stream][key] = val
        self.n_wait += 1

    def _deps(self, stream, reads, writes):
        for b in reads:
            if b.writer is not None:
                self._wait(stream, b.writer)
        for b in writes:
            if b.writer is not None:
                self._wait(stream, b.writer)
            for k, v in b.readers.items():
                self._wait(stream, (k, v))

    def _commit(self, done, reads, writes):
        k, v = done
        for b in writes:
            b.writer = done
            b.readers = {}
        for b in reads:
            if b.readers.get(k, 0) < v:
                b.readers[k] = v

    def op(self, stream, fn, reads=(), writes=(), inc=True):
        self._deps(stream, reads, writes)
        ins = fn()
        self.n_inst += 1
        if inc:
            self.cnt[stream] += 1
            ins.then_inc(self.sem[stream], 1)
            done = (stream, self.cnt[stream])
        else:
            done = (stream, self.cnt[stream] + 1)
        self._commit(done, reads, writes)
        return ins

    def _dma_common(self, q, issue, reads, writes):
        ring, idx = self.dq[q]
        key = ring[idx % len(ring)]
        self.dq[q][1] = idx + 1
        if self.cnt[key] > 0:
            self._wait(q, (key, self.cnt[key]))
        self._deps(q, reads, writes)
        ins = issue()
        self.cnt[key] += 16
        ins.then_inc(self.sem[key], 16)
        self.n_inst += 1
        done = (key, self.cnt[key])
        self._commit(done, reads, writes)
        return done

    def dma(self, q, out, in_, reads=(), writes=(), **kw):
        return self._dma_common(q, lambda: self.eng[q].dma_start(out=out, in_=in_, **kw), reads, writes)

    def idma(self, out, out_off, in_, in_off, reads=(), writes=(), **kw):
        return self._dma_common(
            "pool", lambda: self.nc.gpsimd.indirect_dma_start(out, out_off, in_, in_off, **kw), reads, writes)

    def barrier(self):
        for stream in ("pe", "act", "dve", "pool", "sp"):
            for key, c in self.cnt.items():
                if c > 0:
                    self._wait(stream, (key, c))


class T:
    __slots__ = ("t", "b", "ps", "tb")

    def __init__(self, t, name="", b=None, ps=False, tb=None):
        self.t = t
        self.b = b if b is not None else Buf(name)
        self.ps = ps
        self.tb = tb


class Ring:
    def __init__(self, items):
        self.items = items
        self.i = 0

    def next(self):
        it = self.items[self.i % len(self.items)]
        self.i += 1
        return it


def _consts():
    i = np.arange(128)
    same = (i[:, None] // 64) == (i[None, :] // 64)
    c = {}
    c["ident"] = np.eye(128, dtype=np.float32)
    c["ones"] = np.ones((128, 128), np.float32)
    c["mbt"] = (same & (i[:, None] <= i[None, :])).astype(np.float32)
    c["mrev"] = (same & (i[:, None] > i[None, :])).astype(np.float32)
    c["sel0"] = np.zeros((128, 128), np.float32); c["sel0"][:64, :] = 1.0
    c["sel1"] = np.zeros((128, 128), np.float32); c["sel1"][64:, :] = 1.0
    c["selc"] = same.astype(np.float32)
    c["bigls"] = np.where(same & (i[None, :] < i[:, None]), 0.0, 1e4).astype(np.float32)
    c["negu"] = np.where(same & (i[:, None] <= i[None, :]), 0.0, -1e4).astype(np.float32)
    c["lt"] = (i[:, None] < i[None, :]).astype(np.float32)
    names = list(c.keys())
    arr = np.stack([c[n] for n in names], axis=1)
    m = np.zeros((128, 2, 256), np.float32)
    m[:, 0, :128] = (i[:, None] >= i[None, :]); m[:, 0, 128:] = (i[:, None] <= i[None, :])
    m[:, 1, 128:] = (i[:, None] <= i[None, :])
    misc = np.zeros((128, 128), np.float32)
    misc[:, 0:32] = (10000.0 ** (-np.arange(0, 64, 2, dtype=np.float32) / 64))[None, :]
    misc[:, 32:64] = (np.arange(32, dtype=np.float32) * CAP)[None, :]
    misc[:, 64] = (i // 64 == 0); misc[:, 65] = (i // 64 == 1)
    return names, np.ascontiguousarray(arr), m, misc


CNAMES, CARR, SWAMASK, MISC = _consts()
NCONST = len(CNAMES)

OFF = {}
_o = 0
for _n, _w in (("a_q", 512), ("a_k", 512), ("a_v", 512), ("a_z", 512), ("a_ab", 8), ("b_q", 768), ("b_k", 768),
               ("b_v", 768), ("c_q", 256), ("c_k", 256), ("c_v", 512), ("c_r", 512), ("c_low", 16), ("gates", 3072)):
    OFF[_n] = _o
    _o += _w
assert _o == N_IN


class Prog:
    def __init__(self, n_layers=DEPTH, debug=(), stop_after=None):
        self.L = n_layers
        self.debug = set(debug)
        self.stop_after = stop_after
        self.nc = nc = bass.Bass("TRN2", target_bir_lowering=False)
        self.es = ExitStack()
        self.fw = FW(nc, self.es)
        self.dram = {}
        self.dbuf = {}
        self.uid = 0
        self._cc = {}

    def din(self, name, shape, dt=F32):
        self.dram[name] = self.nc.dram_tensor(name, list(shape), dt, kind="ExternalInput").ap()
        self.dbuf[name] = Buf(name)
        return self.dram[name]

    def dscratch(self, name, shape, dt=F32):
        kind = "ExternalOutput" if name in self.debug else "Internal"
        self.dram[name] = self.nc.dram_tensor(name, list(shape), dt, kind=kind).ap()
        self.dbuf[name] = Buf(name)
        return self.dram[name]

    def sb(self, es, name, shape, dt=F32):
        self.uid += 1
        return T(es.enter_context(self.nc.sbuf_tensor(f"{name}_{self.uid}", list(shape), dt)), name)

    def sbring(self, es, name, shape, dt, n):
        return Ring([self.sb(es, f"{name}{i}", shape, dt) for i in range(n)])

    @staticmethod
    def _b(xs):
        return [x.b if isinstance(x, T) else x for x in xs]

    def mm(self, out, lhsT, rhs, r, w, start=True, stop=True):
        nc = self.nc
        return self.fw.op("pe", lambda: nc.tensor.matmul(out, lhsT=lhsT, rhs=rhs, start=start, stop=stop),
                          self._b(r), self._b(w), inc=stop)

    def tr(self, out, in_, ident, r, w, inc=True):
        nc = self.nc
        return self.fw.op("pe", lambda: nc.tensor.transpose(out=out, in_=in_, identity=ident),
                          self._b(r), self._b(w), inc=inc)

    @staticmethod
    def _psw(r, w):
        return list(w) + [x for x in r if isinstance(x, T) and x.ps]

    def act(self, out, in_, func, r, w, **kw):
        nc = self.nc
        return self.fw.op("act", lambda: nc.scalar.activation(out=out, in_=in_, func=func, **kw),
                          self._b(r), self._b(self._psw(r, w)))

    def v(self, eng, method, r, w, *a, **kw):
        e = self.nc.vector if eng == "dve" else self.nc.gpsimd
        return self.fw.op(eng, lambda: getattr(e, method)(*a, **kw), self._b(r), self._b(self._psw(r, w)))

    def dma(self, q, out, in_, r, w, **kw):
        return self.fw.dma(q, out, in_, self._b(r), self._b(w), **kw)

    def rsqrt(self, out, in_, addc, r, w):
        self.act(out, in_, AF.Sqrt, r, w, bias=self.constcol(addc))
        self.v("dve", "reciprocal", w, w, out=out, in_=out)

    def constcol(self, val):
        key = float(val)
        if key not in self._cc:
            t = self.sb(self.es, "cc", [128, 1])
            self.v("pool", "memset", [], [t], t.t[:], key)
            self._cc[key] = t
        return self._cc[key].t[:, 0:1]


def run_window(gen_iter, window):
    active = []

    def step_all():
        for a in list(active):
            try:
                next(a)
            except StopIteration:
                active.remove(a)
    for g in gen_iter:
        if g is None:
            continue
        active.append(g)
        while len(active) >= window:
            step_all()
    while active:
        step_all()


def bcast_rows(ap, n, parts=128):
    return bass.AP(ap.tensor, ap.offset, [[0, parts], [1, n]])


def build_program(n_layers=DEPTH, debug=(), stop_after=None, moe_decl=NE):
    P = Prog(n_layers, debug, stop_after)
    nc, fw = P.nc, P.fw
    L = n_layers
    x_in = P.din("x", [S, D])
    mem_in = P.din("mem", [MEM, D])
    pos_in = P.din("pos", [128, NT], I32)
    carr_in = P.din("carr", [128, NCONST, 128])
    swam_in = P.din("swamask", [128, 2, 256])
    misc_in = P.din("misc", [128, 128])
    W = {}
    for name, shape in (
        ("norm_mix", [L, D]), ("w_in", [L, D, N_IN]), ("gate_bias", [L, 3 * D]), ("gdn_convT", [L, 1536, 4]),
        ("gdn_a_log", [L, 4]), ("gdn_dt_bias", [L, 4]), ("gdn_norm", [L, 128]), ("swa_q_norm", [L, 64]),
        ("swa_k_norm", [L, 64]), ("gla_gate_up", [L, 16, 256]), ("gla_gate_bias", [L, 256]),
        ("gla_norm", [L, 128]), ("w_branch_a", [L, 512, D]), ("w_branch_b", [L, 256, D]),
        ("w_branch_c", [L, 512, D]), ("w_mix_out", [L, D, D]), ("norm_cross", [L, D]), ("norm_mem", [L, D]),
        ("xa_wq", [L, D, 512]), ("xa_wkv", [L, D, 1024]), ("xa_q_norm", [L, 128]), ("xa_k_norm", [L, 128]),
        ("xa_wo", [L, 512, D]), ("norm_ffn", [L, D]), ("router_w", [L, D, NE]), ("router_b", [L, NE]),
        ("moe_w_in", [L, moe_decl, D, 2 * D]), ("moe_b_in", [L, 128, NE, 16]), ("moe_w_out", [L, moe_decl, D, D]),
        ("moe_b_out", [L, NE, D]),
    ):
        W[name] = P.din(name, shape)
    y = P.nc.dram_tensor("y", [S, D], F32, kind="ExternalOutput").ap()
    P.dram["y"] = y
    P.dbuf["y"] = Buf("y")
    GQKV = P.dscratch("GQKV", [12, 128, S], BF16)
    ZS = P.dscratch("ZS", [S, 512], BF16)
    SQ = P.dscratch("SQ", [S, 768], BF16)
    SK = P.dscratch("SK", [S, 768], BF16)
    SV = P.dscratch("SV", [S, 768], BF16)
    CQ = P.dscratch("CQ", [S, 256])
    CK = P.dscratch("CK", [S, 256])
    CV = P.dscratch("CV", [S, 512], BF16)
    RS = P.dscratch("RS", [S, 512], BF16)
    LA = P.dscratch("LA", [S, 256])
    GT = P.dscratch("GT", [S, 3 * D], BF16)
    GA = P.dscratch("GA", [S, 512], BF16)
    GC = P.dscratch("GC", [S, 512], BF16)
    NUM = P.dscratch("NUM", [3, S, 260])
    H3 = P.dscratch("H3", [S, D], BF16)
    XB = P.dscratch("XB", [XB_ROWS, D], BF16)
    YB = P.dscratch("YB", [XB_ROWS, D])
    if "HT" in P.debug:
        P.dscratch("HT", [128, 8, S], BF16)
    db = P.dbuf

    es = P.es
    cst = P.sb(es, "cst", [128, NCONST, 128])
    identb = P.sb(es, "identb", [128, 128], BF16)
    onesb = P.sb(es, "onesb", [128, 128], BF16)
    swam = P.sb(es, "swam", [128, 2, 256], BF16)
    misc = P.sb(es, "misc", [128, 128])
    cosT = P.sb(es, "cosT", [128, NT, 32])
    sinT = P.sb(es, "sinT", [128, NT, 32])
    gbs = P.sb(es, "gbs", [128, NT, 8])
    C = {n: cst.t[:, i, :] for i, n in enumerate(CNAMES)}
    ident = C["ident"]
    PS = [T(es.enter_context(nc.psum_tensor(f"ps{i}", [128, 512], F32)), f"ps{i}", ps=True) for i in range(8)]
    psr = Ring(PS)

    for cv in (1.0, D * EPS, 64 * EPS, 128 * EPS, 1e-6):
        P.constcol(cv)
    P.dma("sp", cst.t[:], carr_in, [db["carr"]], [cst])
    P.dma("sp", misc.t[:], misc_in, [db["misc"]], [misc])
    P.dma("pool", swam.t[:], swam_in, [db["swamask"]], [swam])
    P.dma("pool", identb.t[:], carr_in[:, CNAMES.index("ident"), :], [db["carr"]], [identb])
    P.dma("pool", onesb.t[:], carr_in[:, CNAMES.index("ones"), :], [db["carr"]], [onesb])

    with ExitStack() as s0:
        posi = P.sb(s0, "posi", [128, NT], I32)
        posf = P.sb(s0, "posf", [128, NT])
        ang = P.sb(s0, "ang", [128, NT, 32])
        red = P.sb(s0, "red", [128, NT, 32])
        P.dma("sp", posi.t[:], pos_in, [db["pos"]], [posi])
        P.v("dve", "tensor_copy", [posi], [posf], out=posf.t[:], in_=posi.t[:])
        TWO_PI = 2.0 * np.pi
        for t in range(NT):
            P.v("dve", "tensor_scalar", [posf, misc], [ang], out=ang.t[:, t, :], in0=misc.t[:, 0:32],
                scalar1=posf.t[:, t:t + 1], scalar2=None, op0=ALU.mult)
        ki = P.sb(s0, "ki", [128, NT, 32], I32)
        kf = P.sb(s0, "kf", [128, NT, 32])
        for dst, shift in ((sinT, 0.0), (cosT, 0.5 * np.pi)):
            P.v("dve", "tensor_scalar", [ang], [red], out=red.t[:], in0=ang.t[:], scalar1=float(shift),
                scalar2=float(1.0 / TWO_PI), op0=ALU.add, op1=ALU.mult)
            P.v("dve", "tensor_copy", [red], [ki], out=ki.t[:], in_=red.t[:])
            P.v("dve", "tensor_copy", [ki], [kf], out=kf.t[:], in_=ki.t[:])
            P.v("dve", "tensor_scalar", [ang], [red], out=red.t[:], in0=ang.t[:], scalar1=float(shift),
                scalar2=None, op0=ALU.add)
            P.v("dve", "scalar_tensor_tensor", [kf, red], [red], out=red.t[:], in0=kf.t[:], scalar=float(-TWO_PI),
                in1=red.t[:], op0=ALU.mult, op1=ALU.add)
            P.v("dve", "tensor_scalar", [red], [kf], out=kf.t[:], in0=red.t[:], scalar1=float(np.pi),
                scalar2=float(-TWO_PI), op0=ALU.is_gt, op1=ALU.mult)
            P.v("dve", "tensor_tensor", [red, kf], [red], out=red.t[:], in0=red.t[:], in1=kf.t[:], op=ALU.add)
            P.v("dve", "tensor_scalar", [red], [red], out=red.t[:], in0=red.t[:], scalar1=float(-np.pi),
                scalar2=float(np.pi), op0=ALU.max, op1=ALU.min)
            P.act(dst.t[:], red.t[:], AF.Sin, [red], [dst])
        fw.barrier()

    ctx = dict(P=P, W=W, C=C, cst=cst, identb=identb, onesb=onesb, swam=swam, misc=misc, cosT=cosT, sinT=sinT,
               gbs=gbs, psr=psr, PS=PS, y=y, x_in=x_in, mem_in=mem_in)
    for l in range(L):
        xsrc, xb = (x_in, db["x"]) if l == 0 else (y, db["y"])
        phase_A(ctx, l, xsrc, xb)
        if stop_after == ("A", l):
            break
        phase_G(ctx, l)
        if stop_after == ("G", l):
            break
        phase_O(ctx, l, xsrc, xb)
        if stop_after == ("O", l):
            break
        phase_X(ctx, l)
        if stop_after == ("X", l):
            break
        phase_M(ctx, l)
        if stop_after == ("M", l):
            break
    fw.barrier()
    es.close()
    return P


def rms_rows(P, s, xt, rows, ncols, scratch, tag):
    ssq = P.sb(s, "ssq" + tag, [128, 1])
    P.act(scratch.t[:rows, :ncols], xt.t[:rows, :ncols], AF.Square, [xt], [scratch, ssq], accum_out=ssq.t[:rows, :])
    P.rsqrt(ssq.t[:rows, :], ssq.t[:rows, :], ncols * EPS, [ssq], [ssq])
    return ssq


def norm_transpose(ctx, s, src_ap, src_buf, gain_ap, gain_buf, hT):
    P = ctx["P"]; nc = P.nc
    psr = ctx["psr"]; identb = ctx["identb"]
    ntile = src_ap.shape[0] // 128
    with ExitStack() as s1:
        g32 = P.sb(s1, "g32", [128, D])
        P.dma("sp", g32.t[:], bcast_rows(gain_ap, D), [gain_buf], [g32])
        P.v("dve", "tensor_scalar", [g32], [g32], out=g32.t[:], in0=g32.t[:], scalar1=float(np.sqrt(D)), scalar2=None,
            op0=ALU.mult)
        xr = P.sbring(s1, "xr", [128, D], F32, 4)
        jr = P.sbring(s1, "junk", [128, D], F32, 4)
        hr = P.sbring(s1, "hr", [128, D], BF16, 4)
        sr = Ring([P.sb(s1, f"ssqA{i}", [128, 1]) for i in range(6)])

        def tile(t):
            xt = xr.next(); jk = jr.next(); hb = hr.next(); ssq = sr.next()
            P.dma("sp" if t % 2 == 0 else "act", xt.t[:], src_ap[t * 128:(t + 1) * 128, :], [src_buf], [xt])
            P.act(jk.t[:], xt.t[:], AF.Square, [xt], [jk, ssq], accum_out=ssq.t[:])
            P.act(ssq.t[:], ssq.t[:], AF.Sqrt, [ssq], [ssq], bias=P.constcol(D * EPS))
            yield
            P.v("dve", "reciprocal", [ssq], [ssq], out=ssq.t[:], in_=ssq.t[:])
            yield
            P.act(jk.t[:], xt.t[:], AF.Copy, [xt, ssq], [jk], scale=ssq.t[:, 0:1])
            yield
            P.v("dve", "tensor_tensor", [jk, g32], [hb], out=hb.t[:], in0=jk.t[:], in1=g32.t[:], op=ALU.mult)
            yield
            ps = psr.next()
            pv = ps.t[:].bitcast(BF16)
            for k in range(8):
                P.tr(pv[:, k * 128:(k + 1) * 128], hb.t[:, k * 128:(k + 1) * 128], identb.t[:], [hb, identb], [ps],
                     inc=(k == 7))
            yield
            P.act(hT.t[:, :, t * 128:(t + 1) * 128], pv.rearrange("p (k n) -> p k n", k=8), AF.Copy, [ps], [hT])
        run_window((tile(t) for t in range(ntile)), 3)
        P.fw.barrier()


def phase_A(ctx, l, xsrc, xbuf):
    P = ctx["P"]; nc = P.nc; fw = P.fw; W = ctx["W"]; db = P.dbuf; dr = P.dram
    psr = ctx["psr"]; C = ctx["C"]; cst = ctx["cst"]; onesb = ctx["onesb"]; gbs = ctx["gbs"]
    cosT, sinT = ctx["cosT"], ctx["sinT"]
    w_in = W["w_in"][l]
    with ExitStack() as s:
        hT = P.sb(s, "hT", [128, 8, S], BF16)
        norm_transpose(ctx, s, xsrc, xbuf, W["norm_mix"][l], db["norm_mix"], hT)
        if "HT" in P.debug:
            P.dma("sp", dr["HT"], hT.t[:], [hT], [db["HT"]])
        if P.stop_after == ("A0", l):
            fw.barrier()
            return
        wr = P.sbring(s, "wt", [128, 8, 512], BF16, 2)

        def load_w(c0, n):
            wt = wr.next()
            P.dma("pool", wt.t[:, :, :n], w_in[:, c0:c0 + n].rearrange("(k p) n -> p k n", p=128), [db["w_in"]], [wt])
            return wt

        def tjob(c0, n, epi, extra=None):
            wt = load_w(c0, n)

            def tiles():
                for t in range(NT):
                    ps = psr.next()
                    for k in range(8):
                        P.mm(ps.t[:, :n], hT.t[:, k, t * 128:(t + 1) * 128], wt.t[:, k, :n], [hT, wt], [ps],
                             start=(k == 0), stop=(k == 7 and extra is None))
                    if extra is not None:
                        extra(ps, n)
                    r = epi(t, ps, n)
                    yield r if hasattr(r, "__next__") else None
            run_window(tiles(), 3)

        def fjob(c0, n, epi):
            wt = load_w(c0, n)
            for tb in range(S // 512):
                ps = psr.next()
                for k in range(8):
                    P.mm(ps.t[:n, :], wt.t[:, k, :n], hT.t[:, k, tb * 512:(tb + 1) * 512], [hT, wt], [ps],
                         start=(k == 0), stop=(k == 7))
                epi(tb, ps, n)

        with ExitStack() as s2:
          if 'SKIPGDN' not in P.debug:
              cw = P.sb(s2, "cw", [128, 12, 4])
              P.dma("sp", cw.t[:], W["gdn_convT"][l].rearrange("(b p) j -> p b j", p=128), [db["gdn_convT"]], [cw])
              prer = P.sbring(s2, "pre", [128, S + 3], F32, 2)
              accr = P.sbring(s2, "acc", [128, S], F32, 2)
              accbr = P.sbring(s2, "accb", [128, S], BF16, 2)
              sqr = P.sbring(s2, "sq", [128, 512], BF16, 8)
              rnr = P.sbring(s2, "rn", [128, 512], F32, 4)
              for fb in range(12):
                  pre = prer.next(); acc = accr.next()
                  P.v("pool", "memset", [], [pre], pre.t[:, 0:3], 0.0)

                  def epi(tb, ps, n, pre=pre):
                      P.act(pre.t[:, 3 + tb * 512:3 + (tb + 1) * 512], ps.t[:, :], AF.Copy, [ps], [pre])
                  fjob(OFF["a_q"] + fb * 128, 128, epi)
                  eng = "dve"
                  P.v(eng, "tensor_scalar", [pre, cw], [acc], out=acc.t[:], in0=pre.t[:, 3:3 + S],
                      scalar1=cw.t[:, fb, 3:4], scalar2=None, op0=ALU.mult)
                  for j in range(3):
                      P.v(eng, "scalar_tensor_tensor", [pre, cw, acc], [acc], out=acc.t[:], in0=pre.t[:, j:j + S],
                          scalar=cw.t[:, fb, j:j + 1], in1=acc.t[:], op0=ALU.mult, op1=ALU.add)
                  accb = accbr.next()
                  if fb < 8:
                      P.act(acc.t[:], acc.t[:], AF.Silu, [acc], [acc])
                      qs = (128.0 ** -0.5) if fb < 4 else 1.0
                      nb_ = S // 512
                      sqs = [sqr.next() for _ in range(nb_)]; rns = {}
                      for tb in range(nb_):
                          P.act(sqs[tb].t[:], acc.t[:, tb * 512:(tb + 1) * 512], AF.Square, [acc], [sqs[tb]])
                      for hb_ in range(2):
                          pss = []
                          for tb in range(hb_ * 4, hb_ * 4 + 4):
                              rns[tb] = rnr.next()
                              ps = psr.next(); pss.append(ps)
                              P.mm(ps.t[:], onesb.t[:], sqs[tb].t[:], [onesb, sqs[tb]], [ps])
                          for i, tb in enumerate(range(hb_ * 4, hb_ * 4 + 4)):
                              P.act(rns[tb].t[:], pss[i].t[:], AF.Sqrt, [pss[i]], [rns[tb]], bias=P.constcol(1e-6))
                          for tb in range(hb_ * 4, hb_ * 4 + 4):
                              sl = slice(tb * 512, (tb + 1) * 512)
                              P.v("dve", "reciprocal", [rns[tb]], [rns[tb]], out=rns[tb].t[:], in_=rns[tb].t[:])
                              P.v("dve", "scalar_tensor_tensor", [acc, rns[tb]], [accb], out=accb.t[:, sl], in0=acc.t[:, sl],
                                  scalar=float(qs), in1=rns[tb].t[:], op0=ALU.mult, op1=ALU.mult)
                  else:
                      P.act(accb.t[:], acc.t[:], AF.Silu, [acc], [accb])
                  P.dma("sp", dr["GQKV"][fb], accb.t[:], [accb], [db["GQKV"]])
          fw.barrier()

        with ExitStack() as s2:
            o16r = P.sbring(s2, "o16", [128, 512], BF16, 5)
            o32r = P.sbring(s2, "o32", [128, 512], F32, 4)

            def store(dst, c0, dt16, func=AF.Copy):
                def epi(t, ps, n):
                    o = (o16r if dt16 else o32r).next()
                    P.act(o.t[:, :n], ps.t[:, :n], func, [ps], [o])
                    P.dma("sp", dr[dst][t * 128:(t + 1) * 128, c0:c0 + n], o.t[:, :n], [o], [db[dst]])
                return epi

            if "ONLYCQ" in P.debug:
                tjob(OFF["c_q"], 256, store("CQ", 0, False))
                fw.barrier()
                return
            tjob(OFF["a_z"], 512, store("ZS", 0, True, AF.Silu))
            par = P.sb(s2, "par", [128, 8])
            P.dma("sp", par.t[:, 0:4], bcast_rows(W["gdn_a_log"][l], 4), [db["gdn_a_log"]], [par])
            P.dma("sp", par.t[:, 4:8], bcast_rows(W["gdn_dt_bias"][l], 4), [db["gdn_dt_bias"]], [par])
            P.act(par.t[:, 0:4], par.t[:, 0:4], AF.Exp, [par], [par])
            t8r = P.sbring(s2, "t8", [128, 8], F32, 2)

            def epi_ab(t, ps, n):
                t8 = t8r.next()
                P.v("dve", "tensor_tensor", [ps, par], [t8], out=t8.t[:, 0:4], in0=ps.t[:, 0:4], in1=par.t[:, 4:8],
                    op=ALU.add)
                P.act(t8.t[:, 0:4], t8.t[:, 0:4], AF.Exp, [t8], [t8])
                P.act(t8.t[:, 0:4], t8.t[:, 0:4], AF.Ln, [t8], [t8], bias=P.constcol(1.0))
                P.v("dve", "scalar_tensor_tensor", [t8, par], [gbs], out=gbs.t[:, t, 0:4], in0=t8.t[:, 0:4],
                    scalar=-1.0, in1=par.t[:, 0:4], op0=ALU.mult, op1=ALU.mult)
                P.act(gbs.t[:, t, 4:8], ps.t[:, 4:8], AF.Sigmoid, [ps], [gbs])
            tjob(OFF["a_ab"], 8, epi_ab)

            gq = P.sb(s2, "gq", [128, 2, 64])
            P.dma("sp", gq.t[:, 0, :], bcast_rows(W["swa_q_norm"][l], 64), [db["swa_q_norm"]], [gq])
            P.dma("sp", gq.t[:, 1, :], bcast_rows(W["swa_k_norm"][l], 64), [db["swa_k_norm"]], [gq])
            P.v("dve", "tensor_scalar", [gq], [gq], out=gq.t[:], in0=gq.t[:], scalar1=8.0, scalar2=None, op0=ALU.mult)
            sqr = P.sbring(s2, "sq2", [128, 512], F32, 4)
            xnr = P.sbring(s2, "xn", [128, 512], F32, 4)
            tmr = P.sbring(s2, "tm", [128, 256], F32, 16)
            ssr = P.sbring(s2, "ss", [128, 8], F32, 4)

            def qk_epi(dst, c0, which):
                def epi(t, ps, n):
                    nh = n // 64
                    sq = sqr.next(); xn = xnr.next(); ss = ssr.next(); o = o16r.next(); tm = tmr.next(); tm2 = tmr.next()
                    cc = tmr.next(); cc2 = tmr.next()
                    v3 = lambda tt, w: tt.t[:, :nh * w].rearrange("p (h d) -> p h d", d=w)
                    P.act(sq.t[:, :n], ps.t[:, :n], AF.Square, [ps], [sq])
                    yield
                    P.v("dve", "tensor_reduce", [sq], [ss], out=ss.t[:, :nh], in_=v3(sq, 64), axis=AX.X, op=ALU.add)
                    yield
                    P.act(ss.t[:, :nh], ss.t[:, :nh], AF.Sqrt, [ss], [ss], bias=P.constcol(64 * EPS))
                    yield
                    P.v("dve", "reciprocal", [ss], [ss], out=ss.t[:, :nh], in_=ss.t[:, :nh])
                    x3 = v3(xn, 64)
                    P.v("dve", "tensor_tensor", [ps, ss], [xn], out=x3, in0=ps.t[:, :n].rearrange("p (h d) -> p h d", d=64),
                        in1=ss.t[:, :nh].unsqueeze(2).to_broadcast([128, nh, 64]), op=ALU.mult)
                    yield
                    P.v("pool", "tensor_tensor", [xn, gq], [xn], out=x3, in0=x3,
                        in1=gq.t[:, which:which + 1, :].to_broadcast([128, nh, 64]), op=ALU.mult)
                    yield
                    o3 = o.t[:, :n].rearrange("p (h d) -> p h d", d=64)
                    cb = cosT.t[:, t:t + 1, :].to_broadcast([128, nh, 32])
                    sb_ = sinT.t[:, t:t + 1, :].to_broadcast([128, nh, 32])
                    x1 = x3[:, :, 0:32]; x2 = x3[:, :, 32:64]
                    P.v("dve", "tensor_tensor", [xn, sinT], [tm], out=v3(tm, 32), in0=x2, in1=sb_, op=ALU.mult)
                    P.v("pool", "tensor_tensor", [xn, cosT], [cc], out=v3(cc, 32), in0=x1, in1=cb, op=ALU.mult)
                    P.v("dve", "tensor_tensor", [xn, sinT], [tm2], out=v3(tm2, 32), in0=x1, in1=sb_, op=ALU.mult)
                    P.v("pool", "tensor_tensor", [xn, cosT], [cc2], out=v3(cc2, 32), in0=x2, in1=cb, op=ALU.mult)
                    yield
                    P.v("dve", "tensor_tensor", [cc, tm], [o], out=o3[:, :, 0:32], in0=v3(cc, 32), in1=v3(tm, 32), op=ALU.subtract)
                    P.v("dve", "tensor_tensor", [cc2, tm2], [o], out=o3[:, :, 32:64], in0=v3(cc2, 32), in1=v3(tm2, 32), op=ALU.add)
                    P.dma("sp", dr[dst][t * 128:(t + 1) * 128, c0:c0 + n], o.t[:, :n], [o], [db[dst]])
                return epi

            tjob(OFF["b_q"], 512, qk_epi("SQ", 0, 0))
            tjob(OFF["b_q"] + 512, 256, qk_epi("SQ", 512, 0))
            tjob(OFF["b_k"], 512, qk_epi("SK", 0, 1))
            tjob(OFF["b_k"] + 512, 256, qk_epi("SK", 512, 1))
            tjob(OFF["b_v"], 512, store("SV", 0, True))
            tjob(OFF["b_v"] + 512, 256, store("SV", 512, True))
            tjob(OFF["c_q"], 256, store("CQ", 0, False))
            tjob(OFF["c_k"], 256, store("CK", 0, False))
            tjob(OFF["c_v"], 512, store("CV", 0, True))
            tjob(OFF["c_r"], 512, store("RS", 0, True, AF.Silu))
            clT = P.sb(s2, "clT", [32, S])
            gu = P.sb(s2, "gu", [32, 256])
            P.v("pool", "memset", [], [clT], clT.t[:], 1.0)
            P.dma("sp", gu.t[0:16, :], W["gla_gate_up"][l], [db["gla_gate_up"]], [gu])
            P.dma("sp", gu.t[16:17, :], W["gla_gate_bias"][l:l + 1, :], [db["gla_gate_bias"]], [gu])

            def epi_cl(tb, ps, n):
                P.act(clT.t[0:16, tb * 512:(tb + 1) * 512], ps.t[0:16, :], AF.Copy, [ps], [clT])
            fjob(OFF["c_low"], 16, epi_cl)
            for t in range(NT):
                ps = psr.next(); o = o32r.next()
                P.mm(ps.t[:, :256], clT.t[0:17, t * 128:(t + 1) * 128], gu.t[0:17, :], [clT, gu], [ps])
                P.act(o.t[:, :256], ps.t[:, :256], AF.Exp, [ps], [o], scale=-1.0)
                P.act(o.t[:, :256], o.t[:, :256], AF.Ln, [o], [o], bias=P.constcol(1.0))
                P.v("dve", "tensor_scalar", [o], [o], out=o.t[:, :256], in0=o.t[:, :256], scalar1=-1.0 / 16.0,
                    scalar2=None, op0=ALU.mult)
                P.dma("sp", dr["LA"][t * 128:(t + 1) * 128, :], o.t[:, :256], [o], [db["LA"]])
            gbias = P.sb(s2, "gbias", [1, 3 * D], BF16)
            P.dma("pool", gbias.t[:], W["gate_bias"][l:l + 1, :], [db["gate_bias"]], [gbias])
            for j in range(6):
                def extra(ps, n, j=j):
                    P.mm(ps.t[:, :n], onesb.t[0:1, :], gbias.t[0:1, j * 512:(j + 1) * 512], [onesb, gbias], [ps],
                         start=False, stop=True)
                tjob(OFF["gates"] + j * 512, 512, store("GT", j * 512, True, AF.Sigmoid), extra=extra)
        fw.barrier()


def head_norm_gate(P, s, rings, o_t, gain, gate, dst, dst_buf, t):
    jk = rings["jk"].next(); ss = rings["ss"].next(); ob = rings["ob"].next()
    P.act(jk.t[:], o_t.t[:], AF.Square, [o_t], [jk])
    P.v("dve", "tensor_reduce", [jk], [ss], out=ss.t[:], in_=jk.t[:].rearrange("p (h d) -> p h d", d=128), axis=AX.X,
        op=ALU.add)
    P.rsqrt(ss.t[:], ss.t[:], 128 * EPS, [ss], [ss])
    o3 = o_t.t[:].rearrange("p (h d) -> p h d", d=128)
    j3 = jk.t[:].rearrange("p (h d) -> p h d", d=128)
    P.v("dve", "tensor_tensor", [o_t, ss], [jk], out=j3, in0=o3, in1=ss.t[:].unsqueeze(2).to_broadcast([128, 4, 128]),
        op=ALU.mult)
    P.v("pool", "tensor_tensor", [jk, gain], [jk], out=j3, in0=j3, in1=gain.t[:].unsqueeze(1).to_broadcast([128, 4, 128]),
        op=ALU.mult)
    P.v("dve", "tensor_tensor", [jk, gate], [ob], out=ob.t[:], in0=jk.t[:], in1=gate.t[:], op=ALU.mult)
    P.dma("act", dst[t * 128:(t + 1) * 128, :], ob.t[:], [ob], [dst_buf])


def phase_G(ctx, l):
    P = ctx["P"]; nc = P.nc; fw = P.fw; W = ctx["W"]; db = P.dbuf; dr = P.dram
    C = ctx["C"]; cst = ctx["cst"]; gbs = ctx["gbs"]; misc = ctx["misc"]; PS = ctx["PS"]; identb = ctx["identb"]
    ident = C["ident"]
    with ExitStack() as s:
        def quarters(b):
            bf = PS[b].t[:].bitcast(BF16)
            return Ring([T(PS[b].t[:, q * 128:(q + 1) * 128], b=PS[b].b, ps=True, tb=bf[:, q * 256:q * 256 + 128])
                         for q in range(4)])
        PQh = [quarters(h) for h in range(4)]
        PQc = Ring([T(PS[b].t[:, 0:128], b=PS[b].b, ps=True, tb=PS[b].t[:].bitcast(BF16)[:, 0:128]) for b in (4, 5, 6)])
        PW = Ring([PS[7]])
        Sg = [P.sb(s, f"Sg{h}", [128, 128]) for h in range(4)]
        Sc = [P.sb(s, f"Sc{h}", [128, 128]) for h in range(4)]
        Sgb = [P.sb(s, f"Sgb{h}", [128, 128], BF16) for h in range(4)]
        Scb = [P.sb(s, f"Scb{h}", [128, 128], BF16) for h in range(4)]
        for h in range(4):
            for st in (Sg[h], Sc[h], Sgb[h], Scb[h]):
                P.v("pool", "memset", [], [st], st.t[:], 0.0)
        gn_a = P.sb(s, "gn_a", [128, 128]); gn_c = P.sb(s, "gn_c", [128, 128])
        for g_, nm in ((gn_a, "gdn_norm"), (gn_c, "gla_norm")):
            P.dma("sp", g_.t[:], bcast_rows(W[nm][l], 128), [db[nm]], [g_])
            P.v("dve", "tensor_scalar", [g_], [g_], out=g_.t[:], in0=g_.t[:], scalar1=float(np.sqrt(128.0)), scalar2=None,
                op0=ALU.mult)
        qkvr = P.sbring(s, "qkv", [128, 12, 128], BF16, 2)
        zr = P.sbring(s, "zt", [128, 512], BF16, 2)
        rsr = P.sbring(s, "rst", [128, 512], BF16, 2)
        lar = P.sbring(s, "la", [128, 256], F32, 2)
        cqr = P.sbring(s, "cq", [128, 256], F32, 2)
        ckr = P.sbring(s, "ck", [128, 256], F32, 2)
        cvr = P.sbring(s, "cv", [128, 512], BF16, 2)
        gsr = P.sbring(s, "gs", [128, 16], F32, 2)
        exr = P.sbring(s, "ex", [128, 12], F32, 2)
        smr = P.sbring(s, "sm", [128, 16], F32, 2)
        HB = []
        for h in range(4):
            d = {}
            for nm in ("dg", "tl", "tu", "dl", "du", "tq"):
                d[nm] = P.sb(s, f"{nm}{h}", [128, 128], F32)
            for nm in ("aq", "rt", "bv", "kd", "r2", "vn", "scT"):
                d[nm] = P.sb(s, f"{nm}{h}", [128, 128], BF16)
            d["xm"] = P.sbring(s, f"xm{h}", [128, 128], BF16, 3)
            d["xt"] = P.sbring(s, f"xt{h}", [128, 128], BF16, 3)
            HB.append(d)
        oar = P.sbring(s, "oa", [128, 512], F32, 2)
        ocr = P.sbring(s, "oc", [128, 512], F32, 2)
        rings = dict(jk=P.sbring(s, "jkh", [128, 512], F32, 2), ss=P.sbring(s, "ssh", [128, 4], F32, 2),
                     ob=P.sbring(s, "obh", [128, 512], BF16, 2))
        ebr = P.sbring(s, "eb", [128, 256], F32, 2); enr = P.sbring(s, "enb", [128, 256], F32, 2)
        err = P.sbring(s, "erev", [128, 256], F32, 2)
        qtr = P.sbring(s, "qt", [128, 256], BF16, 2); ktr = P.sbring(s, "kt", [128, 256], BF16, 2)
        kcr = P.sbring(s, "kdc", [128, 256], BF16, 2)
        qTr = P.sbring(s, "qtT", [128, 2, 128], BF16, 2); kTr = P.sbring(s, "ktT", [128, 2, 128], BF16, 2)
        eblr = P.sbring(s, "ebl", [128, 4], F32, 2)
        mbtb = P.sb(s, "mbtb", [128, 128], BF16)
        P.dma("pool", mbtb.t[:], P.dram["carr"][:, CNAMES.index("mbt"), :], [db["carr"]], [mbtb])
        bgen = phase_B_gen(ctx, l)
        next(bgen)
        b_alive = True

        def gdn_head(h, t, qkv, gs, ex, sm, oa):
            PQ = PQh[h]; B_ = HB[h]
            qT = qkv.t[:, h, :]; kT = qkv.t[:, 4 + h, :]; vT = qkv.t[:, 8 + h, :]
            gc_h = gs.t[:, h:h + 1]
            dg, tl, tu, dl, du, tq = (B_[k] for k in ("dg", "tl", "tu", "dl", "du", "tq"))
            aq, rt, bv, kd, r2, vn = (B_[k] for k in ("aq", "rt", "bv", "kd", "r2", "vn"))
            xmr, xtr = B_["xm"], B_["xt"]
            P.v("dve", "tensor_scalar", [cst, gs], [dg], out=dg.t[:], in0=ident, scalar1=gc_h, scalar2=None, op0=ALU.mult)
            yield
            pB = PQ.next(); pV = PQ.next(); pK = PQ.next()
            P.mm(pB.t[:], C["ones"], dg.t[:], [cst, dg], [pB])
            P.tr(pV.tb, vT, identb.t[:], [qkv, identb], [pV])
            P.tr(pK.tb, kT, identb.t[:], [qkv, identb], [pK])
            yield
            P.v("dve", "scalar_tensor_tensor", [pB, gs, cst], [tl], out=tl.t[:], in0=pB.t[:], scalar=gc_h,
                in1=C["bigls"], op0=ALU.subtract, op1=ALU.max)
            P.v("dve", "scalar_tensor_tensor", [pB, gs, cst], [tu], out=tu.t[:], in0=pB.t[:], scalar=gc_h,
                in1=C["negu"], op0=ALU.subtract, op1=ALU.min)
            P.act(bv.t[:], pV.tb, AF.Copy, [pV, gbs], [bv], scale=gbs.t[:, t, 4 + h:5 + h])
            P.act(kd.t[:], pK.tb, AF.Copy, [pK, sm], [kd], scale=sm.t[:, h:h + 1])
            P.act(dl.t[:], tl.t[:], AF.Exp, [tl], [dl], scale=-1.0)
            P.act(du.t[:], tu.t[:], AF.Exp, [tu], [du])
            yield
            pKK = PQ.next(); pKQ = PQ.next()
            P.mm(pKK.t[:], kT, kT, [qkv], [pKK])
            P.mm(pKQ.t[:], kT, qT, [qkv], [pKQ])
            yield
            xm = xmr.next(); xt = xtr.next()
            P.v("dve", "scalar_tensor_tensor", [pKK, sm, dl], [xm], out=xm.t[:], in0=pKK.t[:], scalar=sm.t[:, 4 + h:5 + h],
                in1=dl.t[:], op0=ALU.mult, op1=ALU.mult)
            P.v("dve", "tensor_tensor", [pKQ, du], [aq], out=aq.t[:], in0=pKQ.t[:], in1=du.t[:], op=ALU.mult)
            yield
            pXT = PQ.next()
            P.tr(pXT.tb, xm.t[:], identb.t[:], [xm, identb], [pXT])
            yield
            P.act(xt.t[:], pXT.tb, AF.Copy, [pXT], [xt])
            P.v("dve", "tensor_tensor", [pXT, identb], [rt], out=rt.t[:], in0=pXT.tb, in1=identb.t[:], op=ALU.add)
            yield
            Pm, PTm = xm, xt
            for lev in range(5):
                p1 = PQ.next()
                P.mm(p1.t[:], PTm.t[:], Pm.t[:], [PTm, Pm], [p1])
                if lev < 4:
                    p2 = PQ.next()
                    P.mm(p2.t[:], Pm.t[:], PTm.t[:], [PTm, Pm], [p2])
                yield
                n1 = xmr.next()
                P.act(n1.t[:], p1.t[:], AF.Copy, [p1], [n1])
                if lev < 4:
                    n2 = xtr.next()
                    P.v("dve", "tensor_copy", [p2], [n2], out=n2.t[:], in_=p2.t[:])
                yield
                p3 = PQ.next()
                P.mm(p3.t[:], n1.t[:], rt.t[:], [n1, rt], [p3])
                yield
                P.v("dve", "tensor_tensor", [p3, rt], [rt], out=rt.t[:], in0=rt.t[:], in1=p3.t[:], op=ALU.add)
                yield
                Pm = n1
                if lev < 4:
                    PTm = n2
            for c in range(2):
                r = slice(64 * c, 64 * c + 64)
                pKS = PQ.next(); pQS = PQ.next()
                P.mm(pKS.t[:], kT, Sgb[h].t[:], [qkv, Sgb[h]], [pKS])
                P.mm(pQS.t[:], qT, Sgb[h].t[:], [qkv, Sgb[h]], [pQS])
                yield
                P.v("dve", "scalar_tensor_tensor", [pKS, sm, bv], [r2], out=r2.t[r, :], in0=pKS.t[r, :],
                    scalar=sm.t[r, 8 + h:9 + h], in1=bv.t[r, :], op0=ALU.mult, op1=ALU.add)
                P.act(tq.t[r, :], pQS.t[r, :], AF.Copy, [pQS, ex], [tq], scale=ex.t[r, h:h + 1])
                yield
                pVN = PQ.next()
                P.mm(pVN.t[:], rt.t[r, :], r2.t[r, :], [rt, r2], [pVN])
                yield
                P.act(vn.t[r, :], pVN.t[r, :], AF.Copy, [pVN], [vn])
                yield
                pAV = PQ.next(); pSU = PQ.next()
                P.mm(pAV.t[:], aq.t[r, :], vn.t[r, :], [aq, vn], [pAV])
                P.mm(pSU.t[:], kd.t[r, :], vn.t[r, :], [kd, vn], [pSU])
                yield
                egl = ex.t[:, 4 + 4 * c + h:5 + 4 * c + h]
                P.v("dve", "scalar_tensor_tensor", [Sg[h], ex, pSU], [Sg[h]], out=Sg[h].t[:], in0=Sg[h].t[:], scalar=egl,
                    in1=pSU.t[:], op0=ALU.mult, op1=ALU.add)
                P.v("dve", "tensor_tensor", [pAV, tq], [oa], out=oa.t[r, h * 128:(h + 1) * 128], in0=pAV.t[r, :],
                    in1=tq.t[r, :], op=ALU.add)
                P.v("pool", "tensor_copy", [Sg[h]], [Sgb[h]], out=Sgb[h].t[:], in_=Sg[h].t[:])
                yield

        def gla_tile(t, la, cq, ck, cv, oc):
            PQ = PQc
            eb = ebr.next(); enb = enr.next(); erev = err.next(); qt = qtr.next(); kt = ktr.next(); kdc = kcr.next()
            pb = PW.next()
            P.mm(pb.t[:, 0:256], C["mbt"], la.t[:], [cst, la], [pb])
            P.mm(pb.t[:, 256:512], C["mrev"], la.t[:], [cst, la], [pb])
            pe_ = PQ.next()
            for p in range(2):
                P.mm(pe_.t[:, 2 * p:2 * p + 2], la.t[:, p * 128:(p + 1) * 128], misc.t[:, 64:66], [la, misc], [pe_])
            yield
            ebl = eblr.next()
            P.act(eb.t[:], pb.t[:, 0:256], AF.Exp, [pb], [eb])
            P.act(enb.t[:], pb.t[:, 0:256], AF.Exp, [pb], [enb], scale=-1.0)
            P.act(erev.t[:], pb.t[:, 256:512], AF.Exp, [pb], [erev])
            P.act(ebl.t[:], pe_.t[:, 0:4], AF.Exp, [pe_], [ebl])
            yield
            P.v("dve", "scalar_tensor_tensor", [cq, eb], [qt], out=qt.t[:], in0=cq.t[:], scalar=0.125, in1=eb.t[:],
                op0=ALU.mult, op1=ALU.mult)
            P.v("pool", "tensor_tensor", [ck, enb], [kt], out=kt.t[:], in0=ck.t[:], in1=enb.t[:], op=ALU.mult)
            P.v("pool", "tensor_tensor", [ck, erev], [kdc], out=kdc.t[:], in0=ck.t[:], in1=erev.t[:], op=ALU.mult)
            yield
            qtT = qTr.next(); ktT = kTr.next()
            for p in range(2):
                pq_ = PQ.next(); pk_ = PQ.next()
                P.tr(pq_.tb, qt.t[:, p * 128:(p + 1) * 128], identb.t[:], [qt, identb], [pq_])
                P.tr(pk_.tb, kt.t[:, p * 128:(p + 1) * 128], identb.t[:], [kt, identb], [pk_])
                yield
                P.act(qtT.t[:, p, :], pq_.tb, AF.Copy, [pq_], [qtT])
                P.v("dve", "tensor_copy", [pk_], [ktT], out=ktT.t[:, p, :], in_=pk_.tb)
                yield
            for h in range(4):
                p = h // 2; o = 64 * (h % 2); fo = slice(o, o + 64)
                scT = HB[h]["scT"]
                pS = PQ.next()
                P.mm(pS.t[:], ktT.t[fo, p, :], qtT.t[fo, p, :], [ktT, qtT], [pS])
                yield
                P.v("dve", "tensor_tensor", [pS, mbtb], [scT], out=scT.t[:], in0=pS.t[:], in1=mbtb.t[:], op=ALU.mult)
                yield
                for c in range(2):
                    r = slice(64 * c, 64 * c + 64)
                    pO = PQ.next(); pSU = PQ.next()
                    P.mm(pO.t[:], qtT.t[:, p, :], Scb[h].t[:, :], [qtT, Scb[h]], [pO], start=True, stop=False)
                    P.mm(pO.t[:], scT.t[:, :], cv.t[:, h * 128:(h + 1) * 128], [scT, cv], [pO], start=False, stop=True)
                    P.mm(pSU.t[:], kdc.t[r, p * 128:(p + 1) * 128], cv.t[r, h * 128:(h + 1) * 128], [kdc, cv], [pSU])
                    yield
                    P.act(oc.t[r, h * 128:(h + 1) * 128], pO.t[r, :], AF.Copy, [pO], [oc])
                    P.v("dve", "scalar_tensor_tensor", [Sc[h], ebl, pSU], [Sc[h]], out=Sc[h].t[fo, :], in0=Sc[h].t[fo, :],
                        scalar=ebl.t[fo, 2 * p + c:2 * p + c + 1], in1=pSU.t[fo, :], op0=ALU.mult, op1=ALU.add)
                    P.v("pool", "tensor_copy", [Sc[h]], [Scb[h]], out=Scb[h].t[fo, :], in_=Sc[h].t[fo, :])
                    yield

        def load_tile(t):
            sl = slice(t * 128, (t + 1) * 128)
            qkv = qkvr.next(); zt = zr.next(); rst = rsr.next(); la = lar.next(); cq = cqr.next(); ck = ckr.next()
            cv = cvr.next()
            for g3 in range(3):
                P.dma("sp", qkv.t[:, 4 * g3:4 * g3 + 4, :], dr["GQKV"][4 * g3:4 * g3 + 4, :, sl].rearrange("f p n -> p f n"),
                      [db["GQKV"]], [qkv])
            P.dma("sp", zt.t[:], dr["ZS"][sl, :], [db["ZS"]], [zt])
            P.dma("sp", rst.t[:], dr["RS"][sl, :], [db["RS"]], [rst])
            P.dma("sp", la.t[:], dr["LA"][sl, :], [db["LA"]], [la])
            P.dma("sp", cq.t[:], dr["CQ"][sl, :], [db["CQ"]], [cq])
            P.dma("sp", ck.t[:], dr["CK"][sl, :], [db["CK"]], [ck])
            P.dma("sp", cv.t[:], dr["CV"][sl, :], [db["CV"]], [cv])
            return qkv, zt, rst, la, cq, ck, cv
        n_g = P.gnt if hasattr(P, 'gnt') else NT
        nxt = load_tile(0)
        for t in range(n_g):
            sl = slice(t * 128, (t + 1) * 128)
            qkv, zt, rst, la, cq, ck, cv = nxt
            if t + 1 < n_g:
                nxt = load_tile(t + 1)
            gs = gsr.next(); ex = exr.next(); sm = smr.next()
            pg = PQc.next()
            for i, nm in enumerate(("mbt", "sel0", "sel1", "selc")):
                P.mm(pg.t[:, i * 4:(i + 1) * 4], C[nm], gbs.t[:, t, 0:4], [cst, gbs], [pg])
            P.v("dve", "tensor_copy", [pg], [gs], out=gs.t[:], in_=pg.t[:, 0:16])
            P.act(ex.t[:], gs.t[:, 0:12], AF.Exp, [gs], [ex])
            P.v("dve", "tensor_tensor", [gs], [sm], out=sm.t[:, 12:16], in0=gs.t[:, 12:16], in1=gs.t[:, 0:4],
                op=ALU.subtract)
            P.act(sm.t[:, 0:4], sm.t[:, 12:16], AF.Exp, [sm], [sm])
            P.v("dve", "tensor_scalar", [gbs], [sm], out=sm.t[:, 4:8], in0=gbs.t[:, t, 4:8], scalar1=-1.0, scalar2=None,
                op0=ALU.mult)
            P.v("dve", "tensor_tensor", [sm, ex], [sm], out=sm.t[:, 8:12], in0=sm.t[:, 4:8], in1=ex.t[:, 0:4],
                op=ALU.mult)
            oa = oar.next(); oc = ocr.next()
            gens = []
            if 'NOGDN' not in P.debug:
                gens += [gdn_head(h, t, qkv, gs, ex, sm, oa) for h in range(4)]
            if 'NOGLA' not in P.debug:
                gens.append(gla_tile(t, la, cq, ck, cv, oc))
            while gens:
                for g_ in list(gens):
                    try:
                        next(g_)
                    except StopIteration:
                        gens.remove(g_)
            if 'NOGDN' not in P.debug:
                head_norm_gate(P, s, rings, oa, gn_a, zt, dr["GA"], db["GA"], t)
            if 'NOGLA' not in P.debug:
                head_norm_gate(P, s, rings, oc, gn_c, rst, dr["GC"], db["GC"], t)
            for _ in range(3):
                if b_alive:
                    try:
                        next(bgen)
                    except StopIteration:
                        b_alive = False
        while b_alive:
            try:
                next(bgen)
            except StopIteration:
                b_alive = False
        fw.barrier()


def phase_B_gen(ctx, l):
    P = ctx["P"]; nc = P.nc; fw = P.fw; db = P.dbuf; dr = P.dram
    identb = ctx["identb"]; swam = ctx["swam"]; PS = ctx["PS"]
    psr = ctx["psr"]
    with ExitStack() as s:
        qr = P.sbring(s, "bq", [128, 256], BF16, 2)
        kr = P.sbring(s, "bk", [128, 256], BF16, 2)
        vr = P.sbring(s, "bv", [128, 256], BF16, 2)
        ver = P.sbring(s, "vext", [128, 4, 65], BF16, 3)
        qkr = P.sbring(s, "qkT", [128, 4, 128], BF16, 3)
        per = P.sbring(s, "pexp", [128, 256], BF16, 4)
        pmr = P.sbring(s, "pm", [128, 256], BF16, 4)
        nor = P.sbring(s, "numsb", [128, 260], F32, 2)
        for it in ver.items:
            P.v("pool", "memset", [], [it], it.t[:], 1.0)
        for it in qkr.items:
            P.v("pool", "memset", [], [it], it.t[:], 0.0)
        prev_qk = qkr.items[-1]; prev_ve = ver.items[-1]
        yield
        for g, dil in enumerate((1, 4, 16)):
            Lg = S // dil
            for res in range(dil):
                for n in range(Lg // 128):
                    row0 = res + dil * 128 * n
                    def rows(name, width, c0):
                        a = dr[name]
                        return bass.AP(a.tensor, a.offset + row0 * width + c0, [[dil * width, 128], [1, 256 if width == 768 else 260]])
                    qt = qr.next(); kt = kr.next(); vt = vr.next(); ve = ver.next(); qk = qkr.next()
                    P.dma("sp", qt.t[:], rows("SQ", 768, g * 256), [db["SQ"]], [qt])
                    P.dma("act", kt.t[:], rows("SK", 768, g * 256), [db["SK"]], [kt])
                    P.dma("sp", vt.t[:], rows("SV", 768, g * 256), [db["SV"]], [vt])
                    P.v("pool", "tensor_copy", [vt], [ve], out=ve.t[:, :, 0:64], in_=vt.t[:].rearrange("p (h d) -> p h d", d=64))
                    pt = psr.next()
                    pv = pt.t[:].bitcast(BF16)
                    for i, src in enumerate((qt, qt, kt, kt)):
                        P.tr(pv[:, i * 128:(i + 1) * 128], src.t[:, (i % 2) * 128:(i % 2 + 1) * 128], identb.t[:], [src, identb], [pt],
                             inc=(i == 3))
                    P.act(qk.t[:], pv[:, 0:512].rearrange("p (a n) -> p a n", a=4), AF.Copy, [pt], [qk])
                    pms = []
                    for h in range(4):
                        p = h // 2; fo = slice(64 * (h % 2), 64 * (h % 2) + 64)
                        sc = psr.next()
                        P.mm(sc.t[:, 0:128], prev_qk.t[fo, 2 + p, :], qk.t[fo, p, :], [prev_qk, qk], [sc])
                        P.mm(sc.t[:, 128:256], qk.t[fo, 2 + p, :], qk.t[fo, p, :], [qk], [sc])
                        pe = per.next(); pm = pmr.next()
                        P.act(pe.t[:], sc.t[:, 0:256], AF.Exp, [sc], [pe], scale=0.125)
                        P.v("dve" if h % 2 == 0 else "pool", "tensor_tensor", [pe, swam], [pm], out=pm.t[:], in0=pe.t[:],
                            in1=swam.t[:, 1 if n == 0 else 0, :], op=ALU.mult)
                        pms.append(pm)
                    nu = psr.next()
                    for h in range(4):
                        P.mm(nu.t[:, h * 65:(h + 1) * 65], pms[h].t[:, 0:128], prev_ve.t[:, h, :], [pms[h], prev_ve], [nu],
                             start=True, stop=False)
                        P.mm(nu.t[:, h * 65:(h + 1) * 65], pms[h].t[:, 128:256], ve.t[:, h, :], [pms[h], ve], [nu],
                             start=False, stop=True)
                    no = nor.next()
                    P.act(no.t[:], nu.t[:, 0:260], AF.Copy, [nu], [no])
                    a = dr["NUM"]
                    dst = bass.AP(a.tensor, a.offset + (g * S + row0) * 260, [[dil * 260, 128], [1, 260]])
                    P.dma("sp", dst, no.t[:], [no], [db["NUM"]])
                    prev_qk = qk; prev_ve = ve
                    yield


def phase_O(ctx, l, xsrc, xbuf):
    P = ctx["P"]; nc = P.nc; fw = P.fw; W = ctx["W"]; db = P.dbuf; dr = P.dram
    identb = ctx["identb"]; psr = ctx["psr"]; y = ctx["y"]
    with ExitStack() as s:
        WA = P.sb(s, "WA", [128, 4, D], BF16); WB = P.sb(s, "WB", [128, 2, D], BF16)
        WC = P.sb(s, "WC", [128, 4, D], BF16); WM = P.sb(s, "WM", [128, 8, D], BF16)
        for wt, nm in ((WA, "w_branch_a"), (WB, "w_branch_b"), (WC, "w_branch_c"), (WM, "w_mix_out")):
            P.dma("pool", wt.t[:], W[nm][l].rearrange("(k p) n -> p k n", p=128), [db[nm]], [wt])
        gar = P.sbring(s, "ga", [128, 512], BF16, 3); gcr = P.sbring(s, "gc", [128, 512], BF16, 3)
        nmr = P.sbring(s, "nm", [128, 3, 260], F32, 3)
        gtr = P.sbring(s, "gt", [128, 3 * D], BF16, 3)
        xr = P.sbring(s, "xo", [128, D], F32, 3)
        rdr = P.sbring(s, "rden", [128, 4], F32, 3)
        obr = P.sbring(s, "obb", [128, 256], BF16, 3)
        aTr = P.sbring(s, "aT", [128, 8, 128], BF16, 3); bTr = P.sbring(s, "bT", [128, 2, 128], BF16, 3)
        t1r = P.sbring(s, "t1", [128, 512], F32, 4); t2r = P.sbring(s, "t2", [128, 512], F32, 8)
        ybr = P.sbring(s, "yb", [128, D], BF16, 3); yTr = P.sbring(s, "yT", [128, 8, 128], BF16, 3)
        PSsub = [Ring(ctx["PS"][0:4]), Ring(ctx["PS"][4:8])]

        def otile(t):
            psr = PSsub[t % 2]
            sl = slice(t * 128, (t + 1) * 128)
            ga = gar.next(); gc = gcr.next(); nm = nmr.next(); gt = gtr.next(); xt = xr.next()
            P.dma("sp", ga.t[:], dr["GA"][sl, :], [db["GA"]], [ga])
            P.dma("sp", gc.t[:], dr["GC"][sl, :], [db["GC"]], [gc])
            P.dma("sp", nm.t[:], dr["NUM"][:, sl, :].rearrange("g p n -> p g n"), [db["NUM"]], [nm])
            P.dma("sp", gt.t[:], dr["GT"][sl, :], [db["GT"]], [gt])
            P.dma("sp", xt.t[:], xsrc[sl, :], [xbuf], [xt])
            yield
            P.v("dve", "tensor_tensor", [nm], [nm], out=nm.t[:, 0, :], in0=nm.t[:, 0, :], in1=nm.t[:, 1, :], op=ALU.add)
            P.v("dve", "tensor_tensor", [nm], [nm], out=nm.t[:, 0, :], in0=nm.t[:, 0, :], in1=nm.t[:, 2, :], op=ALU.add)
            rd = rdr.next(); ob = obr.next()
            n3 = nm.t[:, 0, :].rearrange("p (h d) -> p h d", d=65)
            P.v("dve", "reciprocal", [nm], [rd], out=rd.t[:].unsqueeze(2), in_=n3[:, :, 64:65])
            P.v("dve", "tensor_tensor", [nm, rd], [ob], out=ob.t[:].rearrange("p (h d) -> p h d", d=64), in0=n3[:, :, 0:64],
                in1=rd.t[:].unsqueeze(2).to_broadcast([128, 4, 64]), op=ALU.mult)
            yield
            aT = aTr.next(); bT = bTr.next()
            pa_ = psr.next(); pva = pa_.t[:].bitcast(BF16)
            for k in range(8):
                src = ga if k < 4 else gc
                P.tr(pva[:, k * 128:(k + 1) * 128], src.t[:, (k % 4) * 128:(k % 4 + 1) * 128], identb.t[:], [src, identb], [pa_],
                     inc=(k == 7))
            pb_ = psr.next(); pvb = pb_.t[:].bitcast(BF16)
            for k in range(2):
                P.tr(pvb[:, k * 128:(k + 1) * 128], ob.t[:, k * 128:(k + 1) * 128], identb.t[:], [ob, identb], [pb_], inc=(k == 1))
            yield
            P.act(aT.t[:], pva.rearrange("p (k n) -> p k n", k=8), AF.Copy, [pa_], [aT])
            P.v("dve", "tensor_copy", [pb_], [bT], out=bT.t[:], in_=pvb[:, 0:256].rearrange("p (k n) -> p k n", k=2))
            yield
            yb = ybr.next()
            for half in range(2):
                cs = slice(half * 512, (half + 1) * 512)
                pA = psr.next(); pB = psr.next(); pC = psr.next()
                for k in range(4):
                    P.mm(pA.t[:], aT.t[:, k, :], WA.t[:, k, cs], [aT, WA], [pA], start=(k == 0), stop=(k == 3))
                for k in range(2):
                    P.mm(pB.t[:], bT.t[:, k, :], WB.t[:, k, cs], [bT, WB], [pB], start=(k == 0), stop=(k == 1))
                for k in range(4):
                    P.mm(pC.t[:], aT.t[:, 4 + k, :], WC.t[:, k, cs], [aT, WC], [pC], start=(k == 0), stop=(k == 3))
                yield
                t1 = t1r.next(); t2 = t2r.next(); t3 = t2r.next()
                P.v("dve", "tensor_tensor", [pA, gt], [t1], out=t1.t[:], in0=pA.t[:], in1=gt.t[:, half * 512:(half + 1) * 512],
                    op=ALU.mult)
                P.v("dve", "tensor_tensor", [pB, gt], [t2], out=t2.t[:], in0=pB.t[:], in1=gt.t[:, D + half * 512:D + (half + 1) * 512],
                    op=ALU.mult)
                P.v("dve", "tensor_tensor", [pC, gt], [t3], out=t3.t[:], in0=pC.t[:],
                    in1=gt.t[:, 2 * D + half * 512:2 * D + (half + 1) * 512], op=ALU.mult)
                yield
                P.v("pool", "tensor_tensor", [t1, t2], [t1], out=t1.t[:], in0=t1.t[:], in1=t2.t[:], op=ALU.add)
                P.v("pool", "tensor_tensor", [t1, t3], [yb], out=yb.t[:, cs], in0=t1.t[:], in1=t3.t[:], op=ALU.add)
                yield
            yT = yTr.next()
            py = psr.next(); pvy = py.t[:].bitcast(BF16)
            for k in range(8):
                P.tr(pvy[:, k * 128:(k + 1) * 128], yb.t[:, k * 128:(k + 1) * 128], identb.t[:], [yb, identb], [py], inc=(k == 7))
            yield
            P.act(yT.t[:], pvy.rearrange("p (k n) -> p k n", k=8), AF.Copy, [py], [yT])
            yield
            pos = []
            for half in range(2):
                cs = slice(half * 512, (half + 1) * 512)
                po = psr.next(); pos.append(po)
                for k in range(8):
                    P.mm(po.t[:], yT.t[:, k, :], WM.t[:, k, cs], [yT, WM], [po], start=(k == 0), stop=(k == 7))
            yield
            for half in range(2):
                cs = slice(half * 512, (half + 1) * 512)
                P.v("dve", "tensor_tensor", [pos[half], xt], [xt], out=xt.t[:, cs], in0=pos[half].t[:], in1=xt.t[:, cs], op=ALU.add)
            P.dma("act", y[sl, :], xt.t[:], [xt], [db["y"]])
        run_window((otile(t) for t in range(NT)), 2)
        fw.barrier()


def head_rms(P, rings, src_ps, gain, out_bf):
    jk = rings["jk"].next(); ss = rings["ss"].next()
    P.act(jk.t[:], src_ps.t[:], AF.Square, [src_ps], [jk])
    P.v("dve", "tensor_reduce", [jk], [ss], out=ss.t[:], in_=jk.t[:].rearrange("p (h d) -> p h d", d=128), axis=AX.X,
        op=ALU.add)
    P.rsqrt(ss.t[:], ss.t[:], 128 * EPS, [ss], [ss])
    j3 = jk.t[:].rearrange("p (h d) -> p h d", d=128)
    P.v("dve", "tensor_tensor", [src_ps, ss], [jk], out=j3, in0=src_ps.t[:].rearrange("p (h d) -> p h d", d=128),
        in1=ss.t[:].unsqueeze(2).to_broadcast([128, 4, 128]), op=ALU.mult)
    P.v("pool", "tensor_tensor", [jk, gain], [out_bf], out=out_bf.t[:].rearrange("p (h d) -> p h d", d=128), in0=j3,
        in1=gain.t[:].unsqueeze(1).to_broadcast([128, 4, 128]), op=ALU.mult)


def phase_X(ctx, l):
    P = ctx["P"]; nc = P.nc; fw = P.fw; W = ctx["W"]; db = P.dbuf; dr = P.dram
    identb = ctx["identb"]; psr = ctx["psr"]; y = ctx["y"]
    with ExitStack() as s:
        hT = P.sb(s, "hTx", [128, 8, S], BF16)
        mT = P.sb(s, "mT", [128, 8, MEM], BF16)
        norm_transpose(ctx, s, y, db["y"], W["norm_cross"][l], db["norm_cross"], hT)
        norm_transpose(ctx, s, ctx["mem_in"], db["mem"], W["norm_mem"][l], db["norm_mem"], mT)
        WQ = P.sb(s, "WQ", [128, 8, 512], BF16); WKV = P.sb(s, "WKV", [128, 8, D], BF16); WO = P.sb(s, "WO", [128, 4, D], BF16)
        for wt, nm in ((WQ, "xa_wq"), (WKV, "xa_wkv"), (WO, "xa_wo")):
            P.dma("pool", wt.t[:], W[nm][l].rearrange("(k p) n -> p k n", p=128), [db[nm]], [wt])
        gq = P.sb(s, "xgq", [128, 128]); gk = P.sb(s, "xgk", [128, 128])
        for g_, nm in ((gq, "xa_q_norm"), (gk, "xa_k_norm")):
            P.dma("sp", g_.t[:], bcast_rows(W[nm][l], 128), [db[nm]], [g_])
            P.v("dve", "tensor_scalar", [g_], [g_], out=g_.t[:], in0=g_.t[:], scalar1=float(np.sqrt(128.0)), scalar2=None,
                op0=ALU.mult)
        rings = dict(jk=P.sbring(s, "jkx", [128, 512], F32, 3), ss=P.sbring(s, "ssx", [128, 4], F32, 3))
        kT = P.sb(s, "kTx", [128, 4, MEM], BF16)
        vext = P.sb(s, "vxx", [128, 2, 4, 129], BF16)
        P.v("pool", "memset", [], [vext], vext.t[:], 1.0)
        khr = P.sbring(s, "khat", [128, 512], BF16, 2)
        for mt in range(2):
            pk = psr.next(); pv_ = psr.next()
            for k in range(8):
                P.mm(pk.t[:], mT.t[:, k, mt * 128:(mt + 1) * 128], WKV.t[:, k, 0:512], [mT, WKV], [pk], start=(k == 0), stop=(k == 7))
            for k in range(8):
                P.mm(pv_.t[:], mT.t[:, k, mt * 128:(mt + 1) * 128], WKV.t[:, k, 512:1024], [mT, WKV], [pv_], start=(k == 0),
                     stop=(k == 7))
            kh = khr.next()
            head_rms(P, rings, pk, gk, kh)
            P.act(vext.t[:, mt, :, 0:128], pv_.t[:].rearrange("p (h d) -> p h d", d=128), AF.Copy, [pv_], [vext])
            pt = psr.next(); pvt = pt.t[:].bitcast(BF16)
            for h in range(4):
                P.tr(pvt[:, h * 128:(h + 1) * 128], kh.t[:, h * 128:(h + 1) * 128], identb.t[:], [kh, identb], [pt], inc=(h == 3))
            P.act(kT.t[:, :, mt * 128:(mt + 1) * 128], pvt[:, 0:512].rearrange("p (h n) -> p h n", h=4), AF.Copy, [pt], [kT])
        qhr = P.sbring(s, "qhat", [128, 512], BF16, 3)
        qTr = P.sbring(s, "qTx", [128, 4, 128], BF16, 3)
        per = P.sbring(s, "pex", [128, 256], BF16, 12)
        nsr = P.sbring(s, "nsx", [128, 4, 129], F32, 3)
        rdr = P.sbring(s, "rdx", [128, 4], F32, 3)
        obr = P.sbring(s, "obx", [128, 512], BF16, 3)
        oTr = P.sbring(s, "oTx", [128, 4, 128], BF16, 3)
        xr = P.sbring(s, "xx", [128, D], F32, 3)
        PSsub = [Ring(ctx["PS"][0:4]), Ring(ctx["PS"][4:8])]

        def xtile(t):
            psr = PSsub[t % 2]
            sl = slice(t * 128, (t + 1) * 128)
            xt = xr.next()
            P.dma("sp", xt.t[:], y[sl, :], [db["y"]], [xt])
            pq = psr.next()
            for k in range(8):
                P.mm(pq.t[:], hT.t[:, k, sl], WQ.t[:, k, :], [hT, WQ], [pq], start=(k == 0), stop=(k == 7))
            yield
            qh = qhr.next(); qT = qTr.next()
            jk = rings["jk"].next(); ss = rings["ss"].next()
            P.act(jk.t[:], pq.t[:], AF.Square, [pq], [jk])
            yield
            P.v("dve", "tensor_reduce", [jk], [ss], out=ss.t[:], in_=jk.t[:].rearrange("p (h d) -> p h d", d=128), axis=AX.X,
                op=ALU.add)
            yield
            P.act(ss.t[:], ss.t[:], AF.Sqrt, [ss], [ss], bias=P.constcol(128 * EPS))
            yield
            P.v("dve", "reciprocal", [ss], [ss], out=ss.t[:], in_=ss.t[:])
            j3 = jk.t[:].rearrange("p (h d) -> p h d", d=128)
            P.v("dve", "tensor_tensor", [pq, ss], [jk], out=j3, in0=pq.t[:].rearrange("p (h d) -> p h d", d=128),
                in1=ss.t[:].unsqueeze(2).to_broadcast([128, 4, 128]), op=ALU.mult)
            yield
            P.v("pool", "tensor_tensor", [jk, gq], [qh], out=qh.t[:].rearrange("p (h d) -> p h d", d=128), in0=j3,
                in1=gq.t[:].unsqueeze(1).to_broadcast([128, 4, 128]), op=ALU.mult)
            yield
            pt = psr.next(); pvt = pt.t[:].bitcast(BF16)
            for h in range(4):
                P.tr(pvt[:, h * 128:(h + 1) * 128], qh.t[:, h * 128:(h + 1) * 128], identb.t[:], [qh, identb], [pt], inc=(h == 3))
            yield
            P.act(qT.t[:], pvt[:, 0:512].rearrange("p (h n) -> p h n", h=4), AF.Copy, [pt], [qT])
            yield
            scs = []
            for h in range(4):
                sc = psr.next(); scs.append(sc)
                for mt in range(2):
                    P.mm(sc.t[:, mt * 128:(mt + 1) * 128], kT.t[:, h, mt * 128:(mt + 1) * 128], qT.t[:, h, :], [kT, qT], [sc])
            yield
            pes = []
            for h in range(4):
                pe = per.next()
                P.act(pe.t[:], scs[h].t[:, 0:256], AF.Exp, [scs[h]], [pe], scale=float(128.0 ** -0.5))
                pes.append(pe)
            yield
            ns = nsr.next()
            nus = []
            for hp in range(2):
                nu = psr.next(); nus.append(nu)
                for hh in range(2):
                    h = hp * 2 + hh
                    for mt in range(2):
                        P.mm(nu.t[:, hh * 129:(hh + 1) * 129], pes[h].t[:, mt * 128:(mt + 1) * 128], vext.t[:, mt, h, :],
                             [pes[h], vext], [nu], start=(mt == 0), stop=(mt == 1))
            yield
            P.act(ns.t[:, 0:2, :], nus[0].t[:, 0:258].rearrange("p (h d) -> p h d", d=129), AF.Copy, [nus[0]], [ns])
            P.v("dve", "tensor_copy", [nus[1]], [ns], out=ns.t[:, 2:4, :], in_=nus[1].t[:, 0:258].rearrange("p (h d) -> p h d", d=129))
            yield
            rd = rdr.next(); ob = obr.next(); oT = oTr.next()
            P.v("dve", "reciprocal", [ns], [rd], out=rd.t[:].unsqueeze(2), in_=ns.t[:, :, 128:129])
            P.v("dve", "tensor_tensor", [ns, rd], [ob], out=ob.t[:].rearrange("p (h d) -> p h d", d=128), in0=ns.t[:, :, 0:128],
                in1=rd.t[:].unsqueeze(2).to_broadcast([128, 4, 128]), op=ALU.mult)
            yield
            pt2 = psr.next(); pvt2 = pt2.t[:].bitcast(BF16)
            for h in range(4):
                P.tr(pvt2[:, h * 128:(h + 1) * 128], ob.t[:, h * 128:(h + 1) * 128], identb.t[:], [ob, identb], [pt2], inc=(h == 3))
            yield
            P.act(oT.t[:], pvt2[:, 0:512].rearrange("p (h n) -> p h n", h=4), AF.Copy, [pt2], [oT])
            yield
            pos = []
            for half in range(2):
                cs = slice(half * 512, (half + 1) * 512)
                po = psr.next(); pos.append(po)
                for k in range(4):
                    P.mm(po.t[:], oT.t[:, k, :], WO.t[:, k, cs], [oT, WO], [po], start=(k == 0), stop=(k == 3))
            yield
            for half in range(2):
                cs = slice(half * 512, (half + 1) * 512)
                P.v("dve", "tensor_tensor", [pos[half], xt], [xt], out=xt.t[:, cs], in0=pos[half].t[:], in1=xt.t[:, cs], op=ALU.add)
            P.dma("act", y[sl, :], xt.t[:], [xt], [db["y"]])
        run_window((xtile(t) for t in range(NT)), 2)
        fw.barrier()


def phase_M(ctx, l):
    P = ctx["P"]; nc = P.nc; fw = P.fw; W = ctx["W"]; db = P.dbuf; dr = P.dram
    identb = ctx["identb"]; onesb = ctx["onesb"]; psr = ctx["psr"]; y = ctx["y"]; C = ctx["C"]; cst = ctx["cst"]
    misc = ctx["misc"]
    XBd = dr["XB"]; YBd = dr["YB"]
    if "breg" not in ctx:
        ctx["breg"] = nc.gpsimd.to_reg(XB_ROWS - 1)
    breg = ctx["breg"]
    with ExitStack() as s:
        DST = P.sb(s, "DST", [128, NT, 4], I32)
        GTE = P.sb(s, "GTE", [128, NT, 4])
        with ExitStack() as s1:
            g32 = P.sb(s1, "g32m", [128, D])
            P.dma("sp", g32.t[:], bcast_rows(W["norm_ffn"][l], D), [db["norm_ffn"]], [g32])
            P.v("dve", "tensor_scalar", [g32], [g32], out=g32.t[:], in0=g32.t[:], scalar1=float(np.sqrt(D)), scalar2=None,
                op0=ALU.mult)
            RW = P.sb(s1, "RW", [128, 8, NE])
            P.dma("sp", RW.t[:], W["router_w"][l].rearrange("(k p) e -> p k e", p=128), [db["router_w"]], [RW])
            rb = P.sb(s1, "rb", [1, NE])
            P.dma("sp", rb.t[:], W["router_b"][l:l + 1, :], [db["router_b"]], [rb])
            ltb = P.sb(s1, "ltb", [128, 128], BF16)
            P.dma("pool", ltb.t[:], P.dram["carr"][:, CNAMES.index("lt"), :], [db["carr"]], [ltb])
            carry = P.sb(s1, "carry", [128, NE])
            P.v("dve", "memset", [], [carry], carry.t[:], 0.0)
            xr = P.sbring(s1, "xm", [128, D], F32, 4); jr = P.sbring(s1, "jm", [128, D], F32, 4)
            hfr = P.sbring(s1, "hfm", [128, D], F32, 4); hbr = P.sbring(s1, "hbm", [128, D], BF16, 5)
            sqr = Ring([P.sb(s1, f"ssqm{i}", [128, 1]) for i in range(6)])
            hTr = P.sbring(s1, "hTf", [128, 8, 128], F32, 4)
            lgr = P.sbring(s1, "lg", [128, NE], F32, 4); t8r = P.sbring(s1, "top8", [128, 8], F32, 4)
            smr = P.sbring(s1, "smm", [128, 16], F32, 4)
            mkr = P.sbring(s1, "mk", [128, NE], F32, 4); mbr = P.sbring(s1, "mkb", [128, NE], BF16, 4)
            pfr = P.sbring(s1, "posf", [128, NE], F32, 4); ovr = P.sbring(s1, "ovf", [128, NE], F32, 4)
            ohr = P.sbring(s1, "oh", [128, NE], F32, 16)
            def rtile(t):
                sl = slice(t * 128, (t + 1) * 128)
                xt = xr.next(); jk = jr.next(); hf = hfr.next(); hb = hbr.next(); ssq = sqr.next()
                P.dma("sp" if t % 2 == 0 else "act", xt.t[:], y[sl, :], [db["y"]], [xt])
                P.act(jk.t[:], xt.t[:], AF.Square, [xt], [jk, ssq], accum_out=ssq.t[:])
                P.act(ssq.t[:], ssq.t[:], AF.Sqrt, [ssq], [ssq], bias=P.constcol(D * EPS))
                yield
                P.v("dve", "reciprocal", [ssq], [ssq], out=ssq.t[:], in_=ssq.t[:])
                yield
                P.act(jk.t[:], xt.t[:], AF.Copy, [xt, ssq], [jk], scale=ssq.t[:, 0:1])
                yield
                P.v("dve", "tensor_tensor", [jk, g32], [hf], out=hf.t[:], in0=jk.t[:], in1=g32.t[:], op=ALU.mult)
                yield
                P.v("pool", "tensor_copy", [hf], [hb], out=hb.t[:], in_=hf.t[:])
                hTf = hTr.next()
                pss = []
                for half in range(2):
                    ps2 = psr.next(); pss.append(ps2)
                    for k in range(4):
                        kk = half * 4 + k
                        P.tr(ps2.t[:, k * 128:(k + 1) * 128], hf.t[:, kk * 128:(kk + 1) * 128], C["ident"], [hf, cst], [ps2],
                             inc=(k == 3))
                yield
                P.act(hTf.t[:, 0:4, :], pss[0].t[:].rearrange("p (k n) -> p k n", k=4), AF.Copy, [pss[0]], [hTf])
                P.v("dve", "tensor_copy", [pss[1]], [hTf], out=hTf.t[:, 4:8, :], in_=pss[1].t[:].rearrange("p (k n) -> p k n", k=4))
                yield
                pl = psr.next()
                for k in range(8):
                    P.mm(pl.t[:, 0:NE], hTf.t[:, k, :], RW.t[:, k, :], [hTf, RW], [pl], start=(k == 0), stop=False)
                P.mm(pl.t[:, 0:NE], C["ones"][0:1, :], rb.t[0:1, :], [cst, rb], [pl], start=False, stop=True)
                yield
                lg = lgr.next(); t8 = t8r.next(); sm = smr.next(); mk = mkr.next(); mkb = mbr.next()
                P.act(lg.t[:], pl.t[:, 0:NE], AF.Copy, [pl], [lg])
                yield
                P.v("dve", "max", [lg], [t8], out=t8.t[:], in_=lg.t[:])
                P.v("dve", "tensor_scalar", [t8], [sm], out=sm.t[:, 0:1], in0=t8.t[:, 0:1], scalar1=-1.0, scalar2=None, op0=ALU.mult)
                P.v("dve", "tensor_scalar", [lg, t8], [mk], out=mk.t[:], in0=lg.t[:], scalar1=t8.t[:, 3:4], scalar2=None, op0=ALU.is_ge)
                yield
                P.act(sm.t[:, 4:8], t8.t[:, 0:4], AF.Exp, [t8, sm], [sm], bias=sm.t[:, 0:1], accum_out=sm.t[:, 1:2])
                P.v("pool", "tensor_copy", [mk], [mkb], out=mkb.t[:], in_=mk.t[:])
                yield
                P.v("dve", "reciprocal", [sm], [sm], out=sm.t[:, 2:3], in_=sm.t[:, 1:2])
                pp = psr.next()
                P.mm(pp.t[:, 0:NE], ltb.t[:], mkb.t[:], [ltb, mkb], [pp])
                P.mm(pp.t[:, NE:2 * NE], onesb.t[:], mkb.t[:], [onesb, mkb], [pp])
                yield
                posf = pfr.next(); ovf = ovr.next()
                P.v("dve", "tensor_tensor", [pp, carry], [posf], out=posf.t[:], in0=pp.t[:, 0:NE], in1=carry.t[:], op=ALU.add)
                P.v("dve", "tensor_tensor", [pp, carry], [carry], out=carry.t[:], in0=pp.t[:, NE:2 * NE], in1=carry.t[:], op=ALU.add)
                P.v("dve", "tensor_scalar", [posf], [ovf], out=ovf.t[:], in0=posf.t[:], scalar1=float(CAP), scalar2=1e7,
                    op0=ALU.is_ge, op1=ALU.mult)
                P.v("dve", "tensor_tensor", [posf, misc], [posf], out=posf.t[:], in0=posf.t[:], in1=misc.t[:, 32:64], op=ALU.add)
                P.v("dve", "tensor_tensor", [posf, ovf], [posf], out=posf.t[:], in0=posf.t[:], in1=ovf.t[:], op=ALU.add)
                yield
                for j in range(4):
                    oh = ohr.next()
                    P.v("dve", "tensor_scalar", [lg, t8], [oh], out=oh.t[:], in0=lg.t[:], scalar1=t8.t[:, j:j + 1], scalar2=None,
                        op0=ALU.is_equal)
                    P.v("dve", "tensor_tensor", [oh, posf], [oh], out=oh.t[:], in0=oh.t[:], in1=posf.t[:], op=ALU.mult)
                    P.v("dve", "tensor_reduce", [oh], [sm], out=sm.t[:, 8 + j:9 + j], in_=oh.t[:], axis=AX.X, op=ALU.add)
                    if j % 2 == 1:
                        yield
                P.v("dve", "tensor_scalar", [sm], [sm], out=sm.t[:, 12:16], in0=sm.t[:, 8:12], scalar1=float(XB_ROWS), scalar2=None,
                    op0=ALU.is_lt)
                P.v("dve", "scalar_tensor_tensor", [sm], [GTE], out=GTE.t[:, t, :], in0=sm.t[:, 4:8], scalar=sm.t[:, 2:3],
                    in1=sm.t[:, 12:16], op0=ALU.mult, op1=ALU.mult)
                P.v("dve", "tensor_copy", [sm], [DST], out=DST.t[:, t, :], in_=sm.t[:, 8:12])
                yield
                for j in range(4):
                    fw.idma(XBd, bass.IndirectOffsetOnAxis(ap=DST.t[:, t, j:j + 1], axis=0), hb.t[:, :], None,
                            reads=[hb.b, DST.b], writes=[db["XB"]], bounds_check=breg, oob_is_err=False)
            run_window((rtile(t) for t in range(NT)), 4)
            fw.barrier()
        with ExitStack() as s2:
            bins = P.sb(s2, "bins", [128, NE, 16])
            P.dma("sp", bins.t[:], W["moe_b_in"][l], [db["moe_b_in"]], [bins])
            WIr = P.sbring(s2, "WI", [128, 8, 2 * D], BF16, 2)
            WOr = P.sbring(s2, "WO2", [128, 8, D], BF16, 2)
            bor = P.sbring(s2, "bo", [1, D], BF16, 2)
            xbr = P.sbring(s2, "xbt", [128, D], BF16, 3)
            xTr = P.sbring(s2, "xTe", [128, 8, CAP], BF16, 2)
            aTr = P.sbring(s2, "actT", [128, 8, CAP], BF16, 2)
            gr = P.sbring(s2, "eg", [128, CAP], F32, 2); sgr = P.sbring(s2, "esg", [128, CAP], F32, 2)
            lr = P.sbring(s2, "el", [128, CAP], F32, 2)
            yor = P.sbring(s2, "yo", [128, D], F32, 2)
            def front(e):
                WI = WIr.next(); WO2 = WOr.next(); bo = bor.next()
                for kq in range(4):
                    P.dma("pool", WI.t[:, 2 * kq:2 * kq + 2, :],
                          W["moe_w_in"][l, e, kq * 256:(kq + 1) * 256, :].rearrange("(k p) n -> p k n", p=128), [db["moe_w_in"]], [WI])
                for kq in range(2):
                    P.dma("pool", WO2.t[:, 4 * kq:4 * kq + 4, :],
                          W["moe_w_out"][l, e, kq * 512:(kq + 1) * 512, :].rearrange("(k p) n -> p k n", p=128), [db["moe_w_out"]], [WO2])
                P.dma("pool", bo.t[:], W["moe_b_out"][l, e:e + 1, :], [db["moe_b_out"]], [bo])
                xT = xTr.next(); aT = aTr.next()
                for ct in range(NCAPT):
                    xb_ = xbr.next()
                    r0 = e * CAP + ct * 128
                    P.dma("sp", xb_.t[:], XBd[r0:r0 + 128, :], [db["XB"]], [xb_])
                    pt = psr.next(); pvt = pt.t[:].bitcast(BF16)
                    for k in range(8):
                        P.tr(pvt[:, k * 128:(k + 1) * 128], xb_.t[:, k * 128:(k + 1) * 128], identb.t[:], [xb_, identb], [pt], inc=(k == 7))
                    if ct % 2 == 0:
                        P.act(xT.t[:, :, ct * 128:(ct + 1) * 128], pvt.rearrange("p (k n) -> p k n", k=8), AF.Copy, [pt], [xT])
                    else:
                        P.v("dve", "tensor_copy", [pt], [xT], out=xT.t[:, :, ct * 128:(ct + 1) * 128],
                            in_=pvt.rearrange("p (k n) -> p k n", k=8))
                for fb in range(8):
                    pg0 = psr.next(); pl0 = psr.next(); prm = psr.next()
                    for (pt_, col, f0, n0, n1) in ((pg0, 0, fb, 0, 512), (pl0, 0, fb + 8, 0, 512), (prm, 0, fb, 512, CAP),
                                                    (prm, 128, fb + 8, 512, CAP)):
                        for k in range(8):
                            P.mm(pt_.t[:, col:col + (n1 - n0)], WI.t[:, k, f0 * 128:(f0 + 1) * 128], xT.t[:, k, n0:n1], [WI, xT], [pt_],
                                 start=(k == 0), stop=(k == 7))
                    g_ = gr.next(); sg = sgr.next(); l_ = lr.next()
                    bg = bins.t[:, e, fb:fb + 1]; bl = bins.t[:, e, fb + 8:fb + 9]
                    for (src, c0, c1, d0) in ((pg0, 0, 512, 0), (prm, 0, CAP - 512, 512)):
                        P.v("dve", "tensor_scalar", [src, bins], [g_], out=g_.t[:, d0:d0 + (c1 - c0)], in0=src.t[:, c0:c1], scalar1=bg,
                            scalar2=7.0, op0=ALU.add, op1=ALU.min)
                    for (src, c0, c1, d0) in ((pl0, 0, 512, 0), (prm, 128, 128 + CAP - 512, 512)):
                        P.v("dve", "tensor_scalar", [src, bins], [l_], out=l_.t[:, d0:d0 + (c1 - c0)], in0=src.t[:, c0:c1], scalar1=bl,
                            scalar2=7.0, op0=ALU.add, op1=ALU.min)
                    P.act(sg.t[:], g_.t[:], AF.Sigmoid, [g_], [sg], scale=1.702)
                    P.v("dve", "tensor_scalar", [l_], [l_], out=l_.t[:], in0=l_.t[:], scalar1=-7.0, scalar2=1.0, op0=ALU.max, op1=ALU.add)
                    P.v("dve", "tensor_tensor", [g_, sg], [g_], out=g_.t[:], in0=g_.t[:], in1=sg.t[:], op=ALU.mult)
                    P.v("dve", "tensor_tensor", [g_, l_], [aT], out=aT.t[:, fb, :], in0=g_.t[:], in1=l_.t[:], op=ALU.mult)
                return aT, WO2, bo

            def back(e, aT, WO2, bo):
                for ct in range(NCAPT):
                    yo = yor.next()
                    for half in range(2):
                        cs = slice(half * 512, (half + 1) * 512)
                        po = psr.next()
                        for k in range(8):
                            P.mm(po.t[:], aT.t[:, k, ct * 128:(ct + 1) * 128], WO2.t[:, k, cs], [aT, WO2], [po], start=(k == 0), stop=False)
                        P.mm(po.t[:], onesb.t[0:1, :], bo.t[0:1, cs], [onesb, bo], [po], start=False, stop=True)
                        P.act(yo.t[:, cs], po.t[:], AF.Copy, [po], [yo])
                    r0 = e * CAP + ct * 128
                    P.dma("act", YBd[r0:r0 + 128, :], yo.t[:], [yo], [db["YB"]])

            st = front(0)
            for e in range(1, NE):
                st2 = front(e)
                back(e - 1, *st)
                st = st2
            back(NE - 1, *st)
            fw.barrier()
        with ExitStack() as s3:
            xr = P.sbring(s3, "xc", [128, D], F32, 4)
            gr_ = P.sbring(s3, "gth", [128, D], F32, 16)
            for it in gr_.items:
                P.v("dve", "memset", [], [it], it.t[:], 0.0)
            for t in range(NT):
                sl = slice(t * 128, (t + 1) * 128)
                xt = xr.next()
                P.dma("sp", xt.t[:], y[sl, :], [db["y"]], [xt])
                for j in range(4):
                    gt_ = gr_.next()
                    fw.idma(gt_.t[:, :], None, YBd, bass.IndirectOffsetOnAxis(ap=DST.t[:, t, j:j + 1], axis=0),
                            reads=[db["YB"], DST.b], writes=[gt_.b], bounds_check=breg, oob_is_err=False)
                    P.v("dve", "scalar_tensor_tensor", [gt_, GTE, xt], [xt], out=xt.t[:], in0=gt_.t[:], scalar=GTE.t[:, t, j:j + 1],
                        in1=xt.t[:], op0=ALU.mult, op1=ALU.add)
                P.dma("act", y[sl, :], xt.t[:], [xt], [db["y"]])
            fw.barrier()


def make_in_maps(inputs, n_layers=DEPTH, cores=range(8)):
    f = lambda a: np.ascontiguousarray(np.asarray(a))
    shared = {"carr": CARR, "swamask": SWAMASK, "misc": MISC}
    for k, v in inputs.items():
        if k in ("x", "mem", "positions"):
            continue
        a = np.asarray(v)[:n_layers]
        if k == "gdn_conv":
            shared["gdn_convT"] = f(a.transpose(0, 2, 1))
        elif k == "moe_b_in":
            shared[k] = f(a.reshape(n_layers, NE, 16, 128).transpose(0, 3, 1, 2))
        elif k == "gate_bias":
            shared["gate_bias"] = f(a.reshape(n_layers, 3 * D))
        else:
            shared[k] = f(a)
    maps = []
    for c in cores:
        m = dict(shared)
        m["x"] = f(inputs["x"][c])
        m["mem"] = f(inputs["mem"][c])
        m["pos"] = f(np.asarray(inputs["positions"][c]).astype(np.int32).reshape(NT, 128).T)
        maps.append(m)
    return maps


_CACHE = {}


def kernel(**inputs):
    if "prog" not in _CACHE:
        _CACHE["prog"] = build_program()
    P = _CACHE["prog"]
    maps = make_in_maps(inputs)
    res = run_bass_kernel_spmd(P.nc, maps, core_ids=list(range(8)))
    return np.stack([np.asarray(r["y"]).reshape(S, D) for r in res.results], axis=0).astype(np.float32)
```
